# Optimizing a Trainium2 kernel written in Bass

```python
import math
import jax
import jax.numpy as jnp
from jax import lax
import numpy as np


D_MODEL = 1024
BATCH = 8
SEQ = 4096
DEPTH = 4

HEAD_DIM = 64
ROPE_THETA = 10000.0
NEG_INF = -1e30
BIG_SCORE = 1e30
LN_EPS = 1e-5
Q_BLOCK = 128

A_HEADS = 4
A_VDIM = 2 * HEAD_DIM
B_HEADS = 8
B_KV_HEADS = 2
IDX_HEADS = 4
IDX_DIM = 64
DSA_TOPK = 256
DSA_Q_CHUNK = 64
C_HEADS = 8
MOBA_BLOCK = 256
MOBA_TOPK = 3
MOBA_Q_CHUNK = 16
D_HEADS = 8
D_KV_HEADS = 2
NSA_CMP_LEN = 32
NSA_CMP_STRIDE = 16
NSA_SLC_BLOCK = 64
NSA_SLC_TOPK = 16
NSA_WINDOW = 512
NSA_PHI_HIDDEN = 256
NSA_Q_CHUNK = 32

F_DENSE = 2816
N_EXPERTS = 8
TOP_K = 2
F_EXPERT = 3584
MOE_BLOCK = 256

DN_ALPHA = (2 * DEPTH) ** 0.25
DN_BETA = (8 * DEPTH) ** -0.25
N_EVEN = (DEPTH + 1) // 2
N_ODD = DEPTH // 2

A_QK = A_HEADS * 2 * HEAD_DIM
A_V = A_HEADS * A_VDIM
B_Q = B_HEADS * HEAD_DIM
B_KV = B_KV_HEADS * HEAD_DIM
I_Q = IDX_HEADS * IDX_DIM
EVEN_SIZES = (A_QK, A_QK, A_V, B_Q, B_KV, B_KV, I_Q, IDX_DIM, IDX_HEADS)
P_EVEN = sum(EVEN_SIZES)
D_MIX_EVEN = A_V + B_Q
C_QKV = C_HEADS * HEAD_DIM
D_Q = D_HEADS * HEAD_DIM
D_KV = D_KV_HEADS * HEAD_DIM
ODD_SIZES = (C_QKV, C_QKV, C_QKV, D_Q) + (D_KV,) * 6 + (D_HEADS * 3,)
P_ODD = sum(ODD_SIZES)
D_MIX_ODD = C_QKV + D_Q

kernel_name = 'hybrid_diff_dsa_moba_nsa_moe'

F32 = jnp.float32


def split_cols(h, sizes):
    out, off = [], 0
    for s in sizes:
        out.append(h[..., off:off + s])
        off += s
    return out


def layer_norm(x, g, b):
    xf = x.astype(F32)
    mu = jnp.mean(xf, axis=-1, keepdims=True)
    var = jnp.mean(jnp.square(xf - mu), axis=-1, keepdims=True)
    return ((xf - mu) * lax.rsqrt(var + LN_EPS) * g.astype(F32) + b.astype(F32)).astype(x.dtype)


def rope(x, pos):
    d = x.shape[-1]
    inv = ROPE_THETA ** (-jnp.arange(0, d, 2, dtype=F32) / d)
    ang = pos.astype(F32)[:, None] * inv[None, :]
    cos = jnp.cos(ang)[None, :, None, :]
    sin = jnp.sin(ang)[None, :, None, :]
    xf = x.astype(F32)
    x1, x2 = xf[..., : d // 2], xf[..., d // 2:]
    return jnp.concatenate([x1 * cos - x2 * sin, x2 * cos + x1 * sin], axis=-1).astype(x.dtype)


def masked_softmax(s, mask):
    p = jax.nn.softmax(jnp.where(mask, s, NEG_INF), axis=-1)
    return jnp.where(mask, p, 0.0)


def map_query_chunks(fn, seq_len, chunk):
    starts = jnp.arange(seq_len // chunk, dtype=jnp.int32) * chunk
    out = lax.map(fn, starts)
    out = jnp.moveaxis(out, 0, 1)
    return out.reshape(out.shape[0], seq_len, *out.shape[3:])


def swiglu(x, wg, wu, wd):
    return (jax.nn.silu(x @ wg) * (x @ wu)) @ wd


def diff_attention(q, k, v, lam_params, subln_w, lam_init):
    B, S, Ha, _, dh = q.shape
    scale = dh ** -0.5
    lp = lam_params.astype(F32)
    lam = jnp.exp(jnp.sum(lp[0] * lp[1])) - jnp.exp(jnp.sum(lp[2] * lp[3])) + lam_init
    key_pos = jnp.arange(S)

    def block(c0):
        qb = lax.dynamic_slice_in_dim(q, c0, Q_BLOCK, axis=1)
        s = jnp.einsum('bqhjd,bshjd->bhjqs', qb, k, preferred_element_type=F32) * scale
        mask = key_pos[None, :] <= (c0 + jnp.arange(Q_BLOCK))[:, None]
        p = masked_softmax(s, mask)
        w = p[:, :, 0] - lam * p[:, :, 1]
        return jnp.einsum('bhqs,bshe->bqhe', w.astype(v.dtype), v)

    o = map_query_chunks(block, S, Q_BLOCK).astype(F32)
    o = o * lax.rsqrt(jnp.mean(o * o, axis=-1, keepdims=True) + LN_EPS)
    o = o * subln_w.astype(F32) * (1.0 - lam_init)
    return o.astype(v.dtype).reshape(B, S, Ha * v.shape[-1])


def dsa_attention(q, k, v, iq, ik, iw):
    B, S, H, dh = q.shape
    G = k.shape[2]
    R = H // G
    dt = q.dtype
    scale = dh ** -0.5
    q5 = q.reshape(B, S, G, R, dh)
    k_sel = min(DSA_TOPK, S // 4)
    key_pos = jnp.arange(S)
    bi = jnp.arange(B)[:, None, None]

    def chunk(c0):
        n = DSA_Q_CHUNK
        qc = lax.dynamic_slice_in_dim(q5, c0, n, axis=1)
        iqc = lax.dynamic_slice_in_dim(iq, c0, n, axis=1)
        iwc = lax.dynamic_slice_in_dim(iw, c0, n, axis=1)
        tq = c0 + jnp.arange(n)
        logits = jnp.einsum('bqhd,bsd->bqhs', iqc, ik, preferred_element_type=F32)
        score = jnp.einsum('bqh,bqhs->bqs', iwc.astype(F32), jax.nn.relu(logits))
        score = jnp.where(key_pos[None, None, :] <= tq[None, :, None], score, NEG_INF)
        _, idx = lax.top_k(score, k_sel)
        ok = idx <= tq[None, :, None]
        kg = k[bi, idx]
        vg = v[bi, idx]
        s = jnp.einsum('bqgrd,bqkgd->bgrqk', qc, kg, preferred_element_type=F32) * scale
        p = masked_softmax(s, ok[:, None, None]).astype(dt)
        return jnp.einsum('bgrqk,bqkgd->bqgrd', p, vg)

    o = map_query_chunks(chunk, S, DSA_Q_CHUNK)
    return o.reshape(B, S, H * dh)


def moba_attention(q, k, v):
    B, S, H, dh = q.shape
    dt = q.dtype
    scale = dh ** -0.5
    blk_len = MOBA_BLOCK
    nb = -(-S // blk_len)
    pad = nb * blk_len - S
    k_pad = jnp.pad(k, ((0, 0), (0, pad), (0, 0), (0, 0)))
    v_pad = jnp.pad(v, ((0, 0), (0, pad), (0, 0), (0, 0)))
    k_blk = k_pad.reshape(B, nb, blk_len, H, dh)
    k_mean = jnp.mean(k_blk.astype(F32), axis=2).astype(dt)
    k_bt = k_blk.transpose(0, 3, 1, 2, 4)
    v_bt = v_pad.reshape(B, nb, blk_len, H, dh).transpose(0, 3, 1, 2, 4)
    n_sel = max(1, min(MOBA_TOPK, nb - 1))
    bi = jnp.arange(B)[:, None, None, None]
    hi = jnp.arange(H)[None, :, None, None]
    blk_ids = jnp.arange(nb)
    own_off = jnp.arange(blk_len)
    n = MOBA_Q_CHUNK

    def chunk(c0):
        qc = lax.dynamic_slice_in_dim(q, c0, n, axis=1)
        tq = c0 + jnp.arange(n)
        blk = c0 // blk_len
        gate = jnp.einsum('bqhd,bnhd->bhqn', qc, k_mean, preferred_element_type=F32)
        gate = jnp.where(blk_ids < blk, gate, NEG_INF)
        _, idx = lax.top_k(gate, n_sel)
        sel_ok = idx < blk
        kg = k_bt[bi, hi, idx]
        vg = v_bt[bi, hi, idx]
        s_sel = jnp.einsum('bqhd,bhqmkd->bhqmk', qc, kg, preferred_element_type=F32) * scale
        own_k = lax.dynamic_slice_in_dim(k_pad, blk * blk_len, blk_len, axis=1)
        own_v = lax.dynamic_slice_in_dim(v_pad, blk * blk_len, blk_len, axis=1)
        s_own = jnp.einsum('bqhd,bkhd->bhqk', qc, own_k, preferred_element_type=F32) * scale
        own_ok = (blk * blk_len + own_off)[None, :] <= tq[:, None]
        s = jnp.concatenate([s_sel.reshape(B, H, n, n_sel * blk_len), s_own], axis=-1)
        mask = jnp.concatenate([
            jnp.broadcast_to(sel_ok[..., None], (B, H, n, n_sel, blk_len)).reshape(B, H, n, n_sel * blk_len),
            jnp.broadcast_to(own_ok, (B, H, n, blk_len))], axis=-1)
        p = masked_softmax(s, mask).astype(dt)
        p_sel = p[..., : n_sel * blk_len].reshape(B, H, n, n_sel, blk_len)
        p_own = p[..., n_sel * blk_len:]
        return (jnp.einsum('bhqmk,bhqmkd->bqhd', p_sel, vg)
                + jnp.einsum('bhqk,bkhd->bqhd', p_own, own_v))

    o = map_query_chunks(chunk, S, n)
    return o.reshape(B, S, H * dh)


def compress_blocks(blocks, pe, w1, w2):
    B, Nc, L, G, dh = blocks.shape
    h = blocks + pe[:, None, :].astype(blocks.dtype)
    h = jnp.moveaxis(h, 3, 2).reshape(B, Nc, G, L * dh)
    return jax.nn.gelu(h @ w1) @ w2


def nsa_attention(q, kc_tok, vc_tok, ks, vs, kw, vw, gates, pe, phi_w1, phi_w2, pos):
    B, S, H, dh = q.shape
    G = ks.shape[2]
    R = H // G
    dt = q.dtype
    scale = dh ** -0.5
    q_raw = q.reshape(B, S, G, R, dh)
    q_rot = rope(q, pos).reshape(B, S, G, R, dh)
    ks = rope(ks, pos)
    kw = rope(kw, pos)
    n_cmp = (S - NSA_CMP_LEN) // NSA_CMP_STRIDE + 1
    cmp_start = np.arange(n_cmp) * NSA_CMP_STRIDE
    tok_idx = cmp_start[:, None] + np.arange(NSA_CMP_LEN)[None, :]
    k_cmp = compress_blocks(kc_tok[:, tok_idx], pe[0], phi_w1[0], phi_w2[0])
    v_cmp = compress_blocks(vc_tok[:, tok_idx], pe[1], phi_w1[1], phi_w2[1])
    cmp_end = jnp.asarray(cmp_start + NSA_CMP_LEN - 1, dtype=jnp.int32)
    sb = NSA_SLC_BLOCK
    n_sb = S // sb
    sb_start = np.arange(n_sb) * sb
    shares = ((cmp_start[:, None] <= sb_start[None, :] + sb - 1)
              & (cmp_start[:, None] + NSA_CMP_LEN - 1 >= sb_start[None, :]))
    cmp_to_slc = jnp.asarray(shares, dtype=F32)
    n_sel = min(NSA_SLC_TOPK, n_sb)
    ks_blk = ks.reshape(B, n_sb, sb, G, dh).transpose(0, 3, 1, 2, 4)
    vs_blk = vs.reshape(B, n_sb, sb, G, dh).transpose(0, 3, 1, 2, 4)
    win = NSA_WINDOW
    kw_pad = jnp.pad(kw, ((0, 0), (win, 0), (0, 0), (0, 0)))
    vw_pad = jnp.pad(vw, ((0, 0), (win, 0), (0, 0), (0, 0)))
    bi = jnp.arange(B)[:, None, None, None]
    gi = jnp.arange(G)[None, :, None, None]
    sb_ids = jnp.arange(n_sb)
    sb_off = jnp.arange(sb)
    n = NSA_Q_CHUNK
    win_off = jnp.arange(win + n)

    def chunk(c0):
        tq = c0 + jnp.arange(n)
        qr = lax.dynamic_slice_in_dim(q_rot, c0, n, axis=1)
        qraw = lax.dynamic_slice_in_dim(q_raw, c0, n, axis=1)
        g = lax.dynamic_slice_in_dim(gates, c0, n, axis=1)
        s_c = jnp.einsum('bqgrd,bngd->bgrqn', qraw, k_cmp, preferred_element_type=F32) * scale
        p_c = masked_softmax(s_c, cmp_end[None, :] <= tq[:, None])
        o_cmp = jnp.einsum('bgrqn,bngd->bqgrd', p_c.astype(dt), v_cmp)
        imp = jnp.einsum('bgrqn,nj->bgqj', p_c, cmp_to_slc)
        cur = (tq // sb)[:, None]
        causal = sb_ids[None, :] <= cur
        forced = (sb_ids[None, :] == 0) | ((sb_ids[None, :] >= cur - 1) & causal)
        imp = jnp.where(forced, BIG_SCORE, jnp.where(causal, imp, NEG_INF))
        _, idx = lax.top_k(imp, n_sel)
        kg = ks_blk[bi, gi, idx].reshape(B, G, n, n_sel * sb, dh)
        vg = vs_blk[bi, gi, idx].reshape(B, G, n, n_sel * sb, dh)
        key_pos = (idx[..., None] * sb + sb_off).reshape(B, G, n, n_sel * sb)
        s_s = jnp.einsum('bqgrd,bgqmd->bgrqm', qr, kg, preferred_element_type=F32) * scale
        p_s = masked_softmax(s_s, (key_pos <= tq[None, None, :, None])[:, :, None]).astype(dt)
        o_slc = jnp.einsum('bgrqm,bgqmd->bqgrd', p_s, vg)
        kwc = lax.dynamic_slice_in_dim(kw_pad, c0, win + n, axis=1)
        vwc = lax.dynamic_slice_in_dim(vw_pad, c0, win + n, axis=1)
        kpos = c0 - win + win_off
        dist = tq[:, None] - kpos[None, :]
        s_w = jnp.einsum('bqgrd,bkgd->bgrqk', qr, kwc, preferred_element_type=F32) * scale
        p_w = masked_softmax(s_w, (dist >= 0) & (dist < win) & (kpos[None, :] >= 0)).astype(dt)
        o_win = jnp.einsum('bgrqk,bkgd->bqgrd', p_w, vwc)
        return g[..., 0:1] * o_cmp + g[..., 1:2] * o_slc + g[..., 2:3] * o_win

    o = map_query_chunks(chunk, S, n)
    return o.reshape(B, S, H * dh)


def even_mixer(x, w_in, w_out, lam_params, subln_w, lam_init, pos):
    B, S, _ = x.shape
    aq, ak, av, bq, bk, bv, iq, ik, iw = split_cols(x @ w_in, EVEN_SIZES)
    aq = rope(aq.reshape(B, S, 2 * A_HEADS, HEAD_DIM), pos).reshape(B, S, A_HEADS, 2, HEAD_DIM)
    ak = rope(ak.reshape(B, S, 2 * A_HEADS, HEAD_DIM), pos).reshape(B, S, A_HEADS, 2, HEAD_DIM)
    o_a = diff_attention(aq, ak, av.reshape(B, S, A_HEADS, A_VDIM), lam_params, subln_w, lam_init)
    bq = rope(bq.reshape(B, S, B_HEADS, HEAD_DIM), pos)
    bk = rope(bk.reshape(B, S, B_KV_HEADS, HEAD_DIM), pos)
    bv = bv.reshape(B, S, B_KV_HEADS, HEAD_DIM)
    iq = rope(iq.reshape(B, S, IDX_HEADS, IDX_DIM), pos)
    ik = rope(ik[:, :, None, :], pos)[:, :, 0]
    o_b = dsa_attention(bq, bk, bv, iq, ik, iw)
    return jnp.concatenate([o_a, o_b], axis=-1) @ w_out


def odd_mixer(x, w_in, w_out, gate_b, pe, phi_w1, phi_w2, pos):
    B, S, _ = x.shape
    cq, ck, cv, dq, dkc, dvc, dks, dvs, dkw, dvw, dg = split_cols(x @ w_in, ODD_SIZES)
    hs = (B, S, C_HEADS, HEAD_DIM)
    o_c = moba_attention(rope(cq.reshape(hs), pos), rope(ck.reshape(hs), pos), cv.reshape(hs))
    kv = (B, S, D_KV_HEADS, HEAD_DIM)
    gates = jax.nn.sigmoid(dg.astype(F32) + gate_b.astype(F32)).astype(x.dtype)
    gates = gates.reshape(B, S, D_KV_HEADS, D_HEADS // D_KV_HEADS, 3)
    o_d = nsa_attention(dq.reshape(B, S, D_HEADS, HEAD_DIM), dkc.reshape(kv), dvc.reshape(kv),
                        dks.reshape(kv), dvs.reshape(kv), dkw.reshape(kv), dvw.reshape(kv),
                        gates, pe, phi_w1, phi_w2, pos)
    return jnp.concatenate([o_c, o_d], axis=-1) @ w_out


def moe_swiglu(x, w_router, b_router, w_gate, w_up, w_down):
    B, S, D = x.shape
    xt = x.reshape(-1, D)
    N = xt.shape[0]
    logits = jnp.dot(xt, w_router, preferred_element_type=F32) + b_router.astype(F32)
    top_logit, top_e = lax.top_k(logits, TOP_K)
    gate = jax.nn.softmax(top_logit, axis=-1)
    nk = N * TOP_K
    e_flat = top_e.reshape(-1)
    tok_flat = jnp.repeat(jnp.arange(N, dtype=jnp.int32), TOP_K)
    g_flat = gate.reshape(-1)
    order = jnp.argsort(e_flat)
    e_sorted = e_flat[order]
    counts = jnp.bincount(e_flat, length=N_EXPERTS)
    padded = (counts + MOE_BLOCK - 1) // MOE_BLOCK * MOE_BLOCK
    pad_end = jnp.cumsum(padded)
    pad_start = pad_end - padded
    grp_start = jnp.cumsum(counts) - counts
    slot = pad_start[e_sorted] + jnp.arange(nk) - grp_start[e_sorted]
    n_blocks = -(-nk // MOE_BLOCK) + N_EXPERTS
    n_slots = n_blocks * MOE_BLOCK
    slot_tok = jnp.full((n_slots,), N, jnp.int32).at[slot].set(tok_flat[order])
    slot_gate = jnp.zeros((n_slots,), F32).at[slot].set(g_flat[order])
    block_e = jnp.minimum(jnp.searchsorted(pad_end, jnp.arange(n_blocks) * MOE_BLOCK, side='right'),
                          N_EXPERTS - 1)
    x_pad = jnp.concatenate([xt, jnp.zeros((1, D), xt.dtype)], axis=0)

    def expert_block(args):
        toks, e = args
        h = x_pad[toks]
        return (jax.nn.silu(h @ w_gate[e]) * (h @ w_up[e])) @ w_down[e]

    y_slots = lax.map(expert_block, (slot_tok.reshape(n_blocks, MOE_BLOCK), block_e))
    y_slots = y_slots.reshape(n_slots, D).astype(F32) * slot_gate[:, None]
    y = jnp.zeros((N + 1, D), F32).at[slot_tok].add(y_slots)[:N]
    return y.astype(x.dtype).reshape(B, S, D)


def setup_inputs(seed: int = 0) -> dict:
    key = jax.random.key(seed)
    keys = iter(jax.random.split(key, 32))

    def nrm(shape, scale):
        return jax.random.normal(next(keys), shape, F32) * scale

    d = D_MODEL
    return {
        'x': nrm((BATCH, SEQ, d), 1.0),
        'ev_w_in': nrm((N_EVEN, d, P_EVEN), d ** -0.5),
        'ev_w_out': nrm((N_EVEN, D_MIX_EVEN, d), D_MIX_EVEN ** -0.5 * DN_BETA),
        'dif_lambda': nrm((N_EVEN, 4, HEAD_DIM), 0.1),
        'dif_subln': 1.0 + nrm((N_EVEN, A_VDIM), 0.05),
        'ffd_w_gate': nrm((N_EVEN, d, F_DENSE), d ** -0.5),
        'ffd_w_up': nrm((N_EVEN, d, F_DENSE), d ** -0.5),
        'ffd_w_down': nrm((N_EVEN, F_DENSE, d), F_DENSE ** -0.5 * DN_BETA),
        'od_w_in': nrm((N_ODD, d, P_ODD), d ** -0.5),
        'od_w_out': nrm((N_ODD, D_MIX_ODD, d), D_MIX_ODD ** -0.5 * DN_BETA),
        'nsa_gate_b': nrm((N_ODD, D_HEADS * 3), 0.01),
        'nsa_pe': nrm((N_ODD, 2, NSA_CMP_LEN, HEAD_DIM), 0.1),
        'nsa_phi_w1': nrm((N_ODD, 2, NSA_CMP_LEN * HEAD_DIM, NSA_PHI_HIDDEN), (NSA_CMP_LEN * HEAD_DIM) ** -0.5),
        'nsa_phi_w2': nrm((N_ODD, 2, NSA_PHI_HIDDEN, HEAD_DIM), NSA_PHI_HIDDEN ** -0.5),
        'moe_w_router': nrm((N_ODD, d, N_EXPERTS), d ** -0.5),
        'moe_b_router': nrm((N_ODD, N_EXPERTS), 0.01),
        'moe_w_gate': nrm((N_ODD, N_EXPERTS, d, F_EXPERT), d ** -0.5),
        'moe_w_up': nrm((N_ODD, N_EXPERTS, d, F_EXPERT), d ** -0.5),
        'moe_w_down': nrm((N_ODD, N_EXPERTS, F_EXPERT, d), F_EXPERT ** -0.5 * DN_BETA),
        'ln_mix_g': 1.0 + nrm((DEPTH, d), 0.05),
        'ln_mix_b': nrm((DEPTH, d), 0.02),
        'ln_ffn_g': 1.0 + nrm((DEPTH, d), 0.05),
        'ln_ffn_b': nrm((DEPTH, d), 0.02),
    }


def reference(x, ev_w_in, ev_w_out, dif_lambda, dif_subln, ffd_w_gate, ffd_w_up, ffd_w_down,
              od_w_in, od_w_out, nsa_gate_b, nsa_pe, nsa_phi_w1, nsa_phi_w2,
              moe_w_router, moe_b_router, moe_w_gate, moe_w_up, moe_w_down,
              ln_mix_g, ln_mix_b, ln_ffn_g, ln_ffn_b):
    pos = jnp.arange(x.shape[1], dtype=jnp.int32)
    for l in range(DEPTH):
        i = l // 2
        if l % 2 == 0:
            lam_init = 0.8 - 0.6 * math.exp(-0.3 * l)
            mix = even_mixer(x, ev_w_in[i], ev_w_out[i], dif_lambda[i], dif_subln[i], lam_init, pos)
        else:
            mix = odd_mixer(x, od_w_in[i], od_w_out[i], nsa_gate_b[i], nsa_pe[i],
                            nsa_phi_w1[i], nsa_phi_w2[i], pos)
        x = layer_norm(DN_ALPHA * x + mix, ln_mix_g[l], ln_mix_b[l])
        if l % 2 == 0:
            ffn = swiglu(x, ffd_w_gate[i], ffd_w_up[i], ffd_w_down[i])
        else:
            ffn = moe_swiglu(x, moe_w_router[i], moe_b_router[i], moe_w_gate[i], moe_w_up[i], moe_w_down[i])
        x = layer_norm(DN_ALPHA * x + ffn, ln_ffn_g[l], ln_ffn_b[l])
    return x
```

```python
import math
from contextlib import ExitStack

import numpy as np
import ml_dtypes

import concourse.bass as bass
import concourse.mybir as mybir
from concourse.bass_utils import run_bass_kernel_spmd

F32 = mybir.dt.float32
BF16 = mybir.dt.bfloat16
ALU = mybir.AluOpType
AF = mybir.ActivationFunctionType
AX = mybir.AxisListType

D = 1024
SEQ = 4096
DEPTH = 4
NT = SEQ // 128
HD = 64
LN_EPS = 1e-5
DN_ALPHA = (2 * DEPTH) ** 0.25
F_DENSE = 2816
N_EXPERTS = 8
F_EXPERT = 3584
MOE_CAP = 1536
NEGB = -30000.0
SCALE = HD ** -0.5

EV_SIZES = dict(aq=512, ak=512, av=512, bq=512, bk=128, bv=128, iq=256, ik=64, iw=4)
OD_SIZES = dict(cq=512, ck=512, cv=512, dq=512, dkc=128, dvc=128, dks=128, dvs=128, dkw=128, dvw=128, dg=24)


def _offsets(sizes):
    off, o = {}, 0
    for k, v in sizes.items():
        off[k] = o
        o += v
    return off


EV_OFF = _offsets(EV_SIZES)
OD_OFF = _offsets(OD_SIZES)

ENGS = ("tensor", "vector", "scalar", "gpsimd", "sync")
EPOCH = 60000
NDMASEM = 40
NSWSEM = 12
RELAX_SAME_ENGINE = False
NPROG = 10


class View:
    __slots__ = ("buf", "ap")

    def __init__(self, buf, ap):
        self.buf = buf
        self.ap = ap

    def __getitem__(self, idx):
        return View(self.buf, self.ap[idx])

    def rearrange(self, s, **kw):
        return View(self.buf, self.ap.rearrange(s, **kw))

    def bitcast(self, dt):
        return View(self.buf, self.ap.bitcast(dt))

    def broadcast_to(self, shape):
        return View(self.buf, self.ap.broadcast_to(shape))

    def partition_broadcast(self, n):
        return View(self.buf, self.ap.partition_broadcast(n))


class Buf:
    def __init__(self, t, name=""):
        self.t = t
        self.w = None
        self.r = []
        self.name = name
        self.kids = {}
        self.psum = False

    def __getitem__(self, idx):
        return View(self, self.t[idx])

    def at(self, key):
        k = self.kids.get(key)
        if k is None:
            k = Buf(self.t, f"{self.name}.{key}")
            self.kids[key] = k
        return k


class Op:
    __slots__ = ("eng", "fn", "deps", "signal", "is_dma", "dsem", "dval", "sigval", "sigsem", "gen")

    def __init__(self, eng, fn, is_dma=False):
        self.eng = eng
        self.fn = fn
        self.deps = []
        self.signal = False
        self.is_dma = is_dma
        self.dsem = None
        self.dval = 0
        self.sigval = 0
        self.sigsem = None
        self.gen = 0


class Sched:
    def __init__(self, nc, st, same_engine_sync=True):
        self.nc = nc
        self.same_engine_sync = same_engine_sync
        self.ops = {e: [] for e in ENGS}
        self.dma_rr = 0
        self.dma_last = [None] * NDMASEM
        self.dma_cnt = [0] * NDMASEM
        self.sig_cnt = {e: 0 for e in ENGS}
        self.psems = {e: [st.enter_context(nc.semaphore(f"p_{e}_{k}")) for k in range(NPROG)] for e in ENGS}
        self.dsems = [st.enter_context(nc.semaphore(f"d_{k}")) for k in range(NDMASEM)]
        self.final_ops = []
        self.n_ops = 0
        self.gen = 0

    def _track(self, op, reads, writes):
        deps = []
        for b in reads:
            if b.w is not None:
                deps.append((b.w, True))
            if b.psum:
                deps.extend((r, False) for r in b.r if r.eng != op.eng)
        for b in writes:
            if b.w is not None:
                deps.append((b.w, False))
            deps.extend((r, False) for r in b.r)
        seen = {}
        for d, raw in deps:
            if d is op:
                continue
            if RELAX_SAME_ENGINE and (not raw) and (not d.is_dma) and (not op.is_dma) and d.eng == op.eng:
                continue
            if id(d) in seen:
                continue
            seen[id(d)] = True
            op.deps.append(d)
        for b in reads:
            b.r.append(op)
        for b in writes:
            b.w = op
            b.r = []

    def op(self, eng, fn, reads=(), writes=()):
        o = Op(eng, fn)
        o.gen = self.gen
        self._track(o, reads, writes)
        self.ops[eng].append(o)
        self.n_ops += 1
        return o

    def dma_op(self, eng, fn, reads=(), writes=()):
        o = Op(eng, fn, is_dma=True)
        o.gen = self.gen
        self._track(o, reads, writes)
        if eng == "gpsimd":
            self.dma_rr_sw = (getattr(self, "dma_rr_sw", -1) + 1) % NSWSEM
            k = self.dma_rr_sw
        else:
            self.dma_rr = (self.dma_rr + 1) % (NDMASEM - NSWSEM)
            k = NSWSEM + self.dma_rr
        prev = self.dma_last[k]
        if prev is not None:
            o.deps.append(prev)
        self.dma_cnt[k] += 1
        o.dsem = k
        o.dval = 16 * self.dma_cnt[k]
        self.dma_last[k] = o
        self.ops[eng].append(o)
        self.n_ops += 1
        return o

    @staticmethod
    def _bufs(*views):
        out = []
        for v in views:
            if isinstance(v, View) and v.buf not in out:
                out.append(v.buf)
        return out

    @staticmethod
    def _ap(v):
        return v.ap if isinstance(v, View) else v

    def dma(self, out, in_, eng="sync", final=False, **kw):
        o_ap, i_ap = out.ap, in_.ap
        o = self.dma_op(eng, lambda e: e.dma_start(out=o_ap, in_=i_ap, **kw), reads=[in_.buf], writes=[out.buf])
        if final:
            self.final_ops.append(o)
        return o

    def dma_T(self, out, in_, eng="sync"):
        o_ap, i_ap = out.ap, in_.ap
        return self.dma_op(eng, lambda e: e.dma_start_transpose(out=o_ap, in_=i_ap), reads=[in_.buf], writes=[out.buf])

    def matmul(self, out, lhsT, rhs, start=True, stop=True, skip=False):
        o_ap, l_ap, r_ap = out.ap, lhsT.ap, rhs.ap
        return self.op("tensor",
                       lambda e: e.matmul(o_ap, l_ap, r_ap, start=start, stop=stop, skip_group_check=skip),
                       reads=self._bufs(lhsT, rhs), writes=[out.buf])

    def act(self, out, in_, func, bias=None, scale=1.0, accum_out=None, eng="scalar"):
        o_ap, i_ap = out.ap, in_.ap
        kw = {}
        if bias is not None:
            kw["bias"] = self._ap(bias)
        if accum_out is not None:
            kw["accum_out"] = accum_out.ap
        sc = self._ap(scale)
        writes = self._bufs(out, accum_out)
        return self.op("scalar", lambda e: e.activation(o_ap, i_ap, func, scale=sc, **kw),
                       reads=self._bufs(in_, bias, scale, accum_out), writes=writes)

    def tt(self, out, in0, in1, op, eng="vector"):
        o_ap, a_ap, b_ap = out.ap, in0.ap, in1.ap
        return self.op(eng, lambda e: e.tensor_tensor(o_ap, a_ap, b_ap, op),
                       reads=self._bufs(in0, in1), writes=[out.buf])

    def ts(self, out, in0, s1, s2, op0, op1=None, accum_out=None, eng="vector"):
        o_ap, a_ap = out.ap, in0.ap
        s1a, s2a = self._ap(s1), self._ap(s2)
        kw = {}
        if op1 is not None:
            kw["op1"] = op1
        if accum_out is not None:
            kw["accum_out"] = accum_out.ap
        return self.op(eng, lambda e: e.tensor_scalar(o_ap, a_ap, s1a, s2a, op0, **kw),
                       reads=self._bufs(in0, s1, s2, accum_out), writes=self._bufs(out, accum_out))

    def stt(self, out, in0, scalar, in1, op0, op1, eng="vector"):
        o_ap, a_ap, b_ap = out.ap, in0.ap, in1.ap
        sa = self._ap(scalar)
        return self.op(eng, lambda e: e.scalar_tensor_tensor(o_ap, a_ap, sa, b_ap, op0, op1),
                       reads=self._bufs(in0, scalar, in1), writes=[out.buf])

    def copy(self, out, in_, eng="vector"):
        o_ap, i_ap = out.ap, in_.ap
        if eng == "scalar":
            return self.op("scalar", lambda e: e.copy(o_ap, i_ap), reads=[in_.buf], writes=[out.buf])
        return self.op(eng, lambda e: e.tensor_copy(o_ap, i_ap), reads=[in_.buf], writes=[out.buf])

    def memset(self, out, val, eng="vector"):
        o_ap = out.ap
        return self.op(eng, lambda e: e.memset(o_ap, val), writes=[out.buf])

    def reduce(self, out, in_, op, axis=AX.X, eng="vector"):
        o_ap, i_ap = out.ap, in_.ap
        return self.op(eng, lambda e: e.tensor_reduce(o_ap, i_ap, axis, op), reads=[in_.buf], writes=[out.buf])

    def max8(self, out, in_):
        o_ap, i_ap = out.ap, in_.ap
        return self.op("vector", lambda e: e.max(o_ap, i_ap), reads=[in_.buf], writes=[out.buf])

    def match_replace(self, out, to_replace, in_values, imm):
        o_ap, t_ap, i_ap = out.ap, to_replace.ap, in_values.ap
        return self.op("vector", lambda e: e.match_replace(o_ap, t_ap, i_ap, imm),
                       reads=self._bufs(to_replace, in_values), writes=[out.buf])

    def recip(self, out, in_):
        o_ap, i_ap = out.ap, in_.ap
        return self.op("vector", lambda e: e.reciprocal(o_ap, i_ap), reads=[in_.buf], writes=[out.buf])

    def bn_stats(self, out, in_):
        o_ap, i_ap = out.ap, in_.ap
        return self.op("vector", lambda e: e.bn_stats(o_ap, i_ap), reads=[in_.buf], writes=[out.buf])

    def bn_aggr(self, out, in_):
        o_ap, i_ap = out.ap, in_.ap
        return self.op("vector", lambda e: e.bn_aggr(o_ap, i_ap), reads=[in_.buf], writes=[out.buf])

    def flush(self):
        nc = self.nc
        same = self.same_engine_sync
        gen = self.gen
        for e in ENGS:
            for o in self.ops[e]:
                o.deps = [d for d in o.deps if d.gen == gen]
                for d in o.deps:
                    if d.is_dma:
                        continue
                    if d.eng == e and (e == "tensor" or not same):
                        continue
                    d.signal = True
        for e in ENGS:
            for o in self.ops[e]:
                if o.signal and not o.is_dma and o.sigsem is None:
                    c = self.sig_cnt[e]
                    o.sigsem = (e, c // EPOCH)
                    o.sigval = c % EPOCH + 1
                    self.sig_cnt[e] = c + 1
            assert self.sig_cnt[e] < EPOCH * NPROG, "out of progress semaphores"
        psems, dsems = self.psems, self.dsems
        outstanding = [d for d in self.dma_last if d is not None]
        ops_by_eng = self.ops

        def make(e):
            ops = ops_by_eng[e]

            def body(eng):
                waited = {}
                for o in ops:
                    for d in o.deps:
                        if d.is_dma:
                            key, sem, val = ("d", d.dsem), dsems[d.dsem], d.dval
                        else:
                            if d.eng == e and (e == "tensor" or not same):
                                continue
                            key, sem, val = d.sigsem, psems[d.sigsem[0]][d.sigsem[1]], d.sigval
                        if waited.get(key, 0) >= val:
                            continue
                        waited[key] = val
                        eng.wait_ge(sem, val)
                    ins = o.fn(eng)
                    if o.is_dma:
                        ins.then_inc(dsems[o.dsem], 16)
                    elif o.signal:
                        ins.then_inc(psems[o.sigsem[0]][o.sigsem[1]], 1)
                if e == "sync":
                    for d in outstanding:
                        if waited.get(("d", d.dsem), 0) >= d.dval:
                            continue
                        waited[("d", d.dsem)] = d.dval
                        eng.wait_ge(dsems[d.dsem], d.dval)
            return body

        with nc.Block() as block:
            block.tensor(make("tensor"))
            block.vector(make("vector"))
            block.scalar(make("scalar"))
            block.gpsimd(make("gpsimd"))
            block.sync(make("sync"))
        self.ops = {e: [] for e in ENGS}
        self.gen += 1


def _bf(a):
    return np.ascontiguousarray(np.asarray(a, dtype=np.float32).astype(ml_dtypes.bfloat16))


def make_constants():
    c = {}
    pos = np.arange(SEQ, dtype=np.float32)
    inv = (10000.0 ** (-np.arange(0, HD, 2, dtype=np.float32) / HD)).astype(np.float32)
    ang = (pos[None, :] * inv[:, None]).astype(np.float32)
    cos = np.cos(ang).astype(np.float32)
    sin = np.sin(ang).astype(np.float32)
    p = np.arange(128)
    f = p % 32
    sign = np.where((p % 64) < 32, -1.0, 1.0).astype(np.float32)
    c["c_cos"] = np.ascontiguousarray(cos[f])
    c["c_sin"] = np.ascontiguousarray(sin[f] * sign[:, None])
    eye = np.eye(128, dtype=np.float32)
    c["c_ident"] = _bf(eye)
    c["c_ident4"] = _bf(np.tile(eye, (1, 4)))
    c["c_identf"] = eye.copy()
    q = np.arange(128)[:, None]
    k = np.arange(128)[None, :]
    c["c_causal"] = _bf(np.where(k <= q, 0.0, NEGB))
    c["c_causalf"] = np.where(k <= q, 0.0, -1e30).astype(np.float32)
    c["c_winfar"] = _bf(np.where(k > q, 0.0, NEGB))
    e16 = np.zeros((16, 16, 128), np.float32)
    for n in range(16):
        e16[n, n, :] = 1.0
    c["c_e16"] = _bf(e16)
    e64 = np.zeros((64, 32, 128), np.float32)
    for kt in range(32):
        for kk in range(128):
            e64[2 * kt + kk // 64, kt, kk] = 1.0
    c["c_e64"] = _bf(e64)
    n = np.arange(256)[None, None, :]
    tq = (np.arange(32)[:, None, None] * 128 + np.arange(128)[None, :, None])
    valid = (n < 255) & (16 * n + 31 <= tq)
    c["c_cmpb"] = _bf(np.where(valid, 0.0, NEGB))
    j = np.arange(64)[None, None, :]
    cur = tq // 64
    causal = j <= cur
    forced = (j == 0) | ((j >= cur - 1) & causal)
    c["c_seladd"] = np.where(forced, 1e30, np.where(causal, 0.0, -1e30)).astype(np.float32)
    cs = np.arange(255) * 16
    sbs = np.arange(64) * 64
    shares = (cs[:, None] <= sbs[None, :] + 63) & (cs[:, None] + 31 >= sbs[None, :])
    m = np.zeros((256, 64), np.float32)
    m[:255] = shares
    c["c_c2s"] = _bf(m)
    pp = np.arange(128)
    c["c_tri"] = _bf((pp[:, None] < pp[None, :]).astype(np.float32))
    c["c_ones"] = _bf(np.ones((128, 128), np.float32))
    c["c_ebase"] = np.tile((np.arange(8, dtype=np.float32) * MOE_CAP)[None, None, :], (128, 32, 1))
    c["c_pow2"] = np.tile((2.0 ** -(np.arange(24) + 1.0)).astype(np.float32)[None, :], (128, 1))
    return c


def _perm_cols(w):
    d, n = w.shape
    return np.ascontiguousarray(w.reshape(d, n // 64, 2, 32)[:, :, ::-1, :].reshape(d, n))


def layout_weights(inp):
    out = {}
    for i in range(2):
        w = inp["ev_w_in"][i]
        o = EV_OFF
        fm = np.concatenate([w[:, o[k]:o[k] + EV_SIZES[k]] for k in ("aq", "ak", "bq", "bk", "iq", "ik")], axis=1)
        out[f"ev_fm{i}"] = np.ascontiguousarray(fm)
        out[f"ev_fmp{i}"] = _perm_cols(fm)
        out[f"ev_tm{i}"] = np.ascontiguousarray(
            np.concatenate([w[:, o[k]:o[k] + EV_SIZES[k]] for k in ("av", "bv", "iw")], axis=1))
        w = inp["od_w_in"][i]
        o = OD_OFF
        rope = np.concatenate([w[:, o[k]:o[k] + OD_SIZES[k]] for k in ("cq", "ck", "dq", "dks", "dkw")], axis=1)
        rest = np.concatenate([w[:, o[k]:o[k] + OD_SIZES[k]] for k in ("dkc", "dvc")], axis=1)
        out[f"od_fm{i}"] = np.ascontiguousarray(np.concatenate([rope, rest], axis=1))
        out[f"od_fmp{i}"] = _perm_cols(rope)
        out[f"od_tm{i}"] = np.ascontiguousarray(
            np.concatenate([w[:, o[k]:o[k] + OD_SIZES[k]] for k in ("cv", "dvs", "dvw", "dg")], axis=1))
        out[f"nsa_peT{i}"] = np.ascontiguousarray(np.transpose(inp["nsa_pe"][i], (0, 2, 1)))
    return out


PROJ_SKIP = set()
MOE_ROUTED = True
NFILL = 0
EV_FM = 1984
EV_TM = 644
OD_FM = 2048
OD_FMR = 1792
OD_TM = 792


class KB:
    def __init__(self, nc, st, dbg=()):
        self.nc = nc
        self.st = st
        self.S = Sched(nc, st)
        self.d = {}
        self.dbg = set(dbg)

    def din(self, name, shape, dt):
        ap = self.nc.dram_tensor(name, list(shape), dt, kind="ExternalInput").ap()
        self.d[name] = Buf(ap, name)
        return self.d[name]

    def dscr(self, name, shape, dt, out=False):
        if out or name in self.dbg:
            ap = self.nc.dram_tensor(name, list(shape), dt, kind="ExternalOutput").ap()
        else:
            ap = self.nc.dram_tensor(name, list(shape), dt).ap()
        self.d[name] = Buf(ap, name)
        return self.d[name]

    _uid = 0

    def sb(self, ps, name, shape, dt):
        KB._uid += 1
        nm = f"s{KB._uid}_{name}"
        return Buf(ps.enter_context(self.nc.sbuf_tensor(nm, list(shape), dt)), nm)

    def banks(self, ps, n=8):
        out = []
        for k in range(n):
            KB._uid += 1
            nm = f"p{KB._uid}_bank{k}"
            out.append(Buf(ps.enter_context(self.nc.psum_tensor(nm, [128, 512], F32)), nm))
            out[-1].psum = True
        return out

    def const(self, ps, name, shape, dt, src=None, eng="sync"):
        b = self.sb(ps, name, shape, dt)
        src = self.d[name] if src is None else src
        idx = tuple(slice(None) for _ in shape)
        self.S.dma(b[idx], src[idx], eng=eng)
        return b

    @staticmethod
    def run_units(units, depth=2, fill=None, nfill=0):
        n = len(units)
        for i in range(n + depth):
            if i < n:
                units[i][0]()
                if fill is not None:
                    for _ in range(nfill):
                        fill()
            j = i - depth
            if j >= 0:
                units[j][1]()

    def make_fill(self, ps, bank, ident4=None):
        S = self.S
        if ident4 is None:
            ident4 = self.const(ps, "c_ident4", [128, 512], BF16)
        ones = self.const(ps, "c_ones", [128, 128], BF16)
        fb = Buf(bank.t, "fillbank")
        fb.psum = True

        def fill():
            S.matmul(fb[:, :], lhsT=ones[:, :], rhs=ident4[:, :], start=True, stop=True, skip=True)
        return fill

    def convert(self, dst, src_view, rows):
        S = self.S
        for r0 in range(0, rows, 128):
            r1 = min(rows, r0 + 128)
            S.dma(dst.at(r0)[r0:r1, :], src_view[r0:r1, :], eng="gpsimd")

    def conv_add(self, dst_name, src_view, rows, cols):
        if not hasattr(self, "cq"):
            self.cq, self.cdone = [], set()
        self.cq.append((dst_name, src_view, rows, rows * cols * 6))

    def conv_budget(self, nbytes):
        while getattr(self, "cq", None) and nbytes > 0:
            name, src, rows, b = self.cq.pop(0)
            self.convert(self.d[name], src, rows)
            self.cdone.add(name)
            nbytes -= b

    def conv_ensure(self, names):
        if not hasattr(self, "cq"):
            return
        if any(n not in self.cdone for n in names):
            while any(n not in self.cdone for n in names):
                self.conv_budget(1)
            self.S.flush()

    def emit_xT(self, src, t, identf, tbanks, stage, xT_d, xT32=None):
        S = self.S
        for hb in range(2):
            bank = tbanks[hb]
            for cc in range(4):
                c = hb * 4 + cc
                S.matmul(bank[:, cc * 128:(cc + 1) * 128], lhsT=src[:, c * 128:(c + 1) * 128], rhs=identf[:, :],
                         start=True, stop=True, skip=True)
            S.copy(stage[:, hb * 4:(hb + 1) * 4, (t % 4) * 128:(t % 4 + 1) * 128],
                   bank[:, :].rearrange("p (c t) -> p c t", c=4), eng="scalar" if hb == 0 else "vector")
            if xT32 is not None:
                S.copy(xT32[:, hb * 4:(hb + 1) * 4, :], bank[:, :].rearrange("p (c t) -> p c t", c=4),
                       eng="vector" if hb == 0 else "scalar")
        if t % 4 == 3:
            g = t // 4
            for c in range(8):
                S.dma(xT_d.at((c, g))[c * 128:(c + 1) * 128, g * 512:(g + 1) * 512], stage[:, c, :])

    def phase_prologue(self, x_src, xT_d):
        S = self.S
        with ExitStack() as ps:
            identf = self.const(ps, "c_identf", [128, 128], F32)
            xt = [self.sb(ps, f"xt{k}", [128, 1024], F32) for k in range(2)]
            stage = [self.sb(ps, f"stage{k}", [128, 8, 512], BF16) for k in range(2)]
            bk = self.banks(ps, 4)
            for t in range(NT):
                S.dma(xt[t % 2][:, :], x_src[t * 128:(t + 1) * 128, :])
                self.emit_xT(xt[t % 2][:, :], t, identf, bk[(t % 2) * 2:(t % 2) * 2 + 2], stage[(t // 4) % 2], xT_d)
            S.flush()

    def phase_proj(self, L):
        S, d = self.S, self.d
        even = L % 2 == 0
        i = L // 2
        if even:
            fm, fmp, tm = d[f"b_ev_fm{i}"], d[f"b_ev_fmp{i}"], d[f"b_ev_tm{i}"]
            NFM, NR, NTM = EV_FM, EV_FM, EV_TM
        else:
            fm, fmp, tm = d[f"b_od_fm{i}"], d[f"b_od_fmp{i}"], d[f"b_od_tm{i}"]
            NFM, NR, NTM = OD_FM, OD_FMR, OD_TM
        xT_d, qk_d, vt_d = d["xT"], d["qk"], d["vt"]
        with ExitStack() as ps:
            xT = self.sb(ps, "xT", [128, 8, SEQ], BF16)
            for c in range(8):
                S.dma(xT.at(c)[:, c, :], xT_d[c * 128:(c + 1) * 128, :])
            xTb = [xT.at(c) for c in range(8)]
            cos = self.const(ps, "c_cos", [128, SEQ], F32)
            sin = self.const(ps, "c_sin", [128, SEQ], F32)
            wA = [self.sb(ps, f"wA{k}", [128, 8, 512], BF16) for k in range(2)]
            wB = [self.sb(ps, f"wB{k}", [128, 8, 512], BF16) for k in range(2)]
            osb = [self.sb(ps, f"osb{k}", [128, 2048], BF16) for k in range(2)]
            oraw = [self.sb(ps, f"oraw{k}", [128, 2048], BF16) for k in range(2)]
            t1 = [self.sb(ps, f"t1_{k}", [128, 512], F32) for k in range(2)]
            t2 = [self.sb(ps, f"t2_{k}", [128, 512], F32) for k in range(2)]
            tf = [self.sb(ps, f"tf_{k}", [128, 512], F32) for k in range(2)]
            wtm = self.sb(ps, "wtm", [128, 8, NTM], BF16)
            vsb = [self.sb(ps, f"vsb{k}", [128, NTM], BF16) for k in range(2)]
            sm = [self.sb(ps, f"sm{k}", [128, 24], F32) for k in range(2)]
            bk = self.banks(ps, 8)
            fm_v = fm[:, :].rearrange("(c p) n -> p c n", p=128)
            fmp_v = fmp[:, :].rearrange("(c p) n -> p c n", p=128)
            if not even:
                km = self.sb(ps, "km", [128, 4, 16], F32)
                kmb = self.sb(ps, "kmb", [128, 4, 16], BF16)
                gb = self.sb(ps, "gb", [128, 24], F32)
                S.dma(gb[:, :], d["nsa_gate_b"][i:i + 1, :].broadcast_to([128, 24]))
            ntile = (NFM + 127) // 128
            u = 0
            ostage = 0
            SK = PROJ_SKIP
            for ct in range(ntile if "fm" not in SK else 0):
                rows = min(128, NFM - ct * 128)
                grp, cg = ct // 4, ct % 4
                rope = ct * 128 < NR
                A, B = wA[grp % 2], wB[grp % 2]
                if cg == 0:
                    w = min(512, NFM - grp * 512)
                    S.dma(A[:, :, :w], fm_v[:, :, grp * 512:grp * 512 + w])
                    wb = min(512, NR - grp * 512)
                    if wb > 0:
                        S.dma(B[:, :, :wb], fmp_v[:, :, grp * 512:grp * 512 + wb])
                is_ck = (not even) and 4 <= ct < 8
                is_dq = (not even) and 8 <= ct < 12
                for tc in range(8):
                    tok = slice(tc * 512, (tc + 1) * 512)
                    half, hc = tc // 4, tc % 4
                    if hc == 0:
                        ostage += 1
                    ob = osb[ostage % 2]
                    orw = oraw[ostage % 2]
                    ocol = slice(hc * 512, (hc + 1) * 512)
                    pa = bk[(2 * u) % 8]
                    pb = bk[(2 * u + 1) % 8]
                    for c in range(8):
                        S.matmul(pa[:rows, :], lhsT=A[:, c, cg * 128:cg * 128 + rows],
                                 rhs=View(xTb[c], xT.t[:, c, tok]), start=(c == 0), stop=(c == 7))
                    if rope:
                        for c in range(8):
                            S.matmul(pb[:rows, :], lhsT=B[:, c, cg * 128:cg * 128 + rows],
                                     rhs=View(xTb[c], xT.t[:, c, tok]), start=(c == 0), stop=(c == 7))
                        a1, a2 = t1[u % 2], t2[u % 2]
                        S.tt(a1[:rows, :], pa[:rows, :], cos[:rows, tok], ALU.mult)
                        S.tt(a2[:rows, :], pb[:rows, :], sin[:rows, tok], ALU.mult)
                        if is_ck and "km" not in SK:
                            f = tf[u % 2]
                            S.tt(f[:rows, :], a1[:rows, :], a2[:rows, :], ALU.add, eng="gpsimd")
                            S.copy(ob[:rows, ocol], f[:rows, :], eng="scalar")
                            S.reduce(km[:, ct - 4, tc * 2:(tc + 1) * 2],
                                     f[:, :].rearrange("p (b t) -> p b t", b=2), ALU.add)
                        else:
                            S.tt(ob[:rows, ocol], a1[:rows, :], a2[:rows, :], ALU.add, eng="gpsimd")
                        if is_dq:
                            S.copy(orw[:rows, ocol], pa[:rows, :], eng="scalar")
                    else:
                        S.copy(ob[:rows, ocol], pa[:rows, :], eng="scalar")
                    u += 1
                    if hc == 3:
                        S.dma(qk_d.at((ct, half))[ct * 128:ct * 128 + rows, half * 2048:(half + 1) * 2048], ob[:rows, :])
                        if is_dq:
                            r0 = (ct - 8) * 128
                            S.dma(d["qraw"].at((ct, half))[r0:r0 + 128, half * 2048:(half + 1) * 2048], orw[:, :])
            if not even and "km" not in SK and "fm" not in SK:
                S.ts(kmb[:, :, :], km[:, :, :], 1.0 / 256.0, None, ALU.mult)
                for k4 in range(4):
                    S.dma(d["kmean"].at(k4)[k4 * 128:(k4 + 1) * 128, :], kmb[:, k4, :])
            tm_v = tm[:, :].rearrange("(c p) n -> p c n", p=128)
            S.dma(wtm[:, :, :], tm_v)
            n1 = NTM - 512
            for t in range(NT if "tm" not in SK else 0):
                p0, p1 = bk[(2 * t) % 8], bk[(2 * t + 1) % 8]
                tk = slice(t * 128, (t + 1) * 128)
                for c in range(8):
                    S.matmul(p0[:, :], lhsT=View(xTb[c], xT.t[:, c, tk]), rhs=wtm[:, c, 0:512], start=(c == 0), stop=(c == 7))
                for c in range(8):
                    S.matmul(p1[:, :n1], lhsT=View(xTb[c], xT.t[:, c, tk]), rhs=wtm[:, c, 512:NTM], start=(c == 0), stop=(c == 7))
                vb, s = vsb[t % 2], sm[t % 2]
                S.copy(vb[:, 0:512], p0[:, :], eng="scalar")
                if even:
                    S.copy(vb[:, 512:640], p1[:, 0:128])
                    S.copy(s[:, 0:4], p1[:, 128:132])
                    S.dma(vt_d.at(t)[tk, 0:640], vb[:, 0:640])
                    S.dma(d["iw"].at(t)[tk, :], s[:, 0:4])
                else:
                    S.copy(vb[:, 512:768], p1[:, 0:256])
                    S.tt(s[:, 0:24], p1[:, 256:280], gb[:, :], ALU.add)
                    S.act(s[:, 0:24], s[:, 0:24], AF.Sigmoid)
                    S.dma(vt_d.at(t)[tk, 0:768], vb[:, 0:768])
                    S.dma(d["gates"].at(t)[tk, :], s[:, 0:24])
            S.flush()

    def phase_diff(self, L):
        S, d = self.S, self.d
        i = L // 2
        lam_init = 0.8 - 0.6 * math.exp(-0.3 * L)
        qk_d, vt_d, mix_d = d["qk"], d["vt"], d["mix"]
        with ExitStack() as ps:
            ident = self.const(ps, "c_ident", [128, 128], BF16)
            causal = self.const(ps, "c_causal", [128, 128], BF16)
            lamb = self.sb(ps, "lamb", [128, 256], F32)
            S.dma(lamb[:, :], d["dif_lambda"][i:i + 1].rearrange("o a b -> o (a b)").broadcast_to([128, 256]))
            sub = self.sb(ps, "sub", [128, 128], F32)
            S.dma(sub[:, :], d["dif_subln"][i:i + 1, :].broadcast_to([128, 128]))
            prod = self.sb(ps, "prod", [128, 2, 64], F32)
            S.tt(prod[:, 0, :], lamb[:, 0:64], lamb[:, 64:128], ALU.mult)
            S.tt(prod[:, 1, :], lamb[:, 128:192], lamb[:, 192:256], ALU.mult)
            ssum = self.sb(ps, "ssum", [128, 2], F32)
            S.reduce(ssum[:, :], prod[:, :, :], ALU.add)
            ee = self.sb(ps, "ee", [128, 2], F32)
            S.act(ee[:, :], ssum[:, :], AF.Exp)
            neglam = self.sb(ps, "neglam", [128, 1], F32)
            S.tt(neglam[:, :], ee[:, 1:2], ee[:, 0:1], ALU.subtract)
            S.ts(neglam[:, :], neglam[:, :], -lam_init, None, ALU.add)
            sw = self.sb(ps, "sw", [128, 128], F32)
            S.ts(sw[:, :], sub[:, :], 1.0 - lam_init, None, ALU.mult)
            KT = [[self.sb(ps, f"KT{k}{j}", [64, SEQ], BF16) for j in range(2)] for k in range(2)]
            QT = [[self.sb(ps, f"QT{k}{j}", [64, SEQ], BF16) for j in range(2)] for k in range(2)]
            Vaug = [self.sb(ps, f"Vaug{k}", [128, NT, 129], BF16) for k in range(2)]
            for k in range(2):
                S.memset(Vaug[k][:, :, 128:129], 1.0)
            pT = [self.sb(ps, f"pT{k}", [128, 512], BF16) for k in range(3)]
            rs = [self.sb(ps, f"rs{k}", [128, 2], F32) for k in range(2)]
            rr = [self.sb(ps, f"rr{k}", [128, 2], F32) for k in range(2)]
            tt_ = [self.sb(ps, f"tt{k}", [128, 128], F32) for k in range(2)]
            oo = [self.sb(ps, f"oo{k}", [128, 128], F32) for k in range(2)]
            jk = [self.sb(ps, f"jk{k}", [128, 128], F32) for k in range(2)]
            ss = [self.sb(ps, f"ss{k}", [128, 1], F32) for k in range(2)]
            mo = [self.sb(ps, f"mo{k}", [128, 128], BF16) for k in range(2)]
            bk = self.banks(ps, 8)
            scb = bk[0:3]
            accs = [[bk[3], bk[4]], [bk[5], bk[6]]]
            tmp0 = [self.sb(ps, f"tmp0_{k}", [128, 4, 128], F32) for k in range(2)]
            cnt = dict(u=0, pp=0, a=0)
            fill = self.make_fill(ps, bk[7])

            def make_unit(h, c, j, kt, kt_, qt_, va, acc):
                b0 = max(0, kt - 4 * c)
                col0 = b0 * 128
                diag = kt >= 4 * c
                uu = cnt["u"]
                cnt["u"] += 1
                sc = scb[uu % 3]
                p = pT[uu % 3]

                def score():
                    S.matmul(sc[:, col0:512], lhsT=kt_[j][:, kt * 128:(kt + 1) * 128],
                             rhs=qt_[j][:, c * 512 + col0:(c + 1) * 512], start=True, stop=not diag)
                    if diag:
                        S.matmul(sc[:, col0:col0 + 128], lhsT=causal[:, :], rhs=ident[:, :], start=False, stop=True)
                    S.act(p[:, col0:512], sc[:, col0:512], AF.Exp, scale=SCALE)

                def pv():
                    for b in range(b0, 4):
                        bank = acc[b // 2]
                        o0 = (b % 2) * 129
                        S.matmul(bank[:, o0:o0 + 129], lhsT=p[:, b * 128:(b + 1) * 128], rhs=va[:, kt, :],
                                 start=(kt == 0 and b % 2 == 0), stop=(kt == 4 * c + b), skip=True)
                    if kt == 4 * c + 3:
                        post(h, c, j, acc)
                return (score, pv)

            def post(h, c, j, acc):
                t0 = tmp0[c % 2]
                for b in range(4):
                    qt = 4 * c + b
                    o0 = (b % 2) * 129
                    A = acc[b // 2]
                    k2 = cnt["pp"] % 2
                    cnt["pp"] += 1
                    S.ts(rs[k2][:, 0:1], A[:, o0 + 128:o0 + 129], 1e-30, None, ALU.max)
                    S.recip(rr[k2][:, 0:1], rs[k2][:, 0:1])
                    if j == 0:
                        S.ts(t0[:, b, :], A[:, o0:o0 + 128], rr[k2][:, 0:1], None, ALU.mult)
                        continue
                    S.ts(tt_[k2][:, :], A[:, o0:o0 + 128], rr[k2][:, 0:1], neglam[:, 0:1], ALU.mult, ALU.mult)
                    S.tt(oo[k2][:, :], t0[:, b, :], tt_[k2][:, :], ALU.add, eng="gpsimd")
                    S.memset(ss[k2][:, :], 0.0)
                    S.act(jk[k2][:, :], oo[k2][:, :], AF.Square, accum_out=ss[k2][:, 0:1])
                    S.ts(ss[k2][:, :], ss[k2][:, :], 1.0 / 128.0, LN_EPS, ALU.mult, ALU.add)
                    S.act(ss[k2][:, :], ss[k2][:, :], AF.Ln)
                    S.act(ss[k2][:, :], ss[k2][:, :], AF.Exp, scale=-0.5)
                    S.stt(mo[k2][:, :], oo[k2][:, :], ss[k2][:, 0:1], sw[:, :], ALU.mult, ALU.mult)
                    S.dma(mix_d.at(("a", h, qt))[qt * 128:(qt + 1) * 128, h * 128:(h + 1) * 128], mo[k2][:, :])

            for h in range(4):
                kt_, qt_, va = KT[h % 2], QT[h % 2], Vaug[h % 2]
                for j in range(2):
                    r0 = (h * 2 + j) * 64
                    S.dma(qt_[j][:, :], qk_d[r0:r0 + 64, :])
                    S.dma(kt_[j][:, :], qk_d[512 + r0:512 + r0 + 64, :])
                S.dma(va[:, :, 0:128], vt_d[:, h * 128:(h + 1) * 128].rearrange("(kt p) e -> p kt e", p=128))
                units = []
                for c in range(8):
                    for j in range(2):
                        acc = accs[cnt["a"] % 2]
                        cnt["a"] += 1
                        for kt in range(4 * c + 4):
                            units.append(make_unit(h, c, j, kt, kt_, qt_, va, acc))
                self.run_units(units, fill=fill, nfill=NFILL)
            S.flush()

    def phase_dsa(self, L, nit=22):
        S, d = self.S, self.d
        qk_d, vt_d, mix_d = d["qk"], d["vt"], d["mix"]
        R_BQ, R_BK, R_IQ, R_IK = 1024, 1536, 1664, 1920
        with ExitStack() as ps:
            ident4 = self.const(ps, "c_ident4", [128, 512], BF16)
            causalf = self.const(ps, "c_causalf", [128, 128], F32)
            pow2 = self.const(ps, "c_pow2", [128, 24], F32)
            bkT = self.sb(ps, "bkT", [64, 2, SEQ], BF16)
            S.dma(bkT[:, :, :], qk_d[R_BK:R_BK + 128, :].rearrange("(g d) t -> d g t", d=64))
            ikT = self.sb(ps, "ikT", [64, SEQ], BF16)
            S.dma(ikT[:, :], qk_d[R_IK:R_IK + 64, :])
            Vaug = self.sb(ps, "Vaug", [128, NT, 2, 65], BF16)
            S.memset(Vaug[:, :, :, 64:65], 1.0)
            for g in range(2):
                S.dma(Vaug[:, :, g, 0:64], vt_d[:, 512 + g * 64:512 + (g + 1) * 64].rearrange("(kt p) e -> p kt e", p=128))
            iqT = [self.sb(ps, f"iqT{k}", [64, 4, 128], BF16) for k in range(2)]
            bqT = [self.sb(ps, f"bqT{k}", [64, 8 * 128], BF16) for k in range(2)]
            iwt = [self.sb(ps, f"iwt{k}", [128, 4], F32) for k in range(2)]
            score = [self.sb(ps, f"score{k}", [128, SEQ], F32) for k in range(2)]
            biasq = [self.sb(ps, f"biasq{k}", [128, SEQ], BF16) for k in range(2)]
            junk = self.sb(ps, "junk", [128, SEQ], BF16)
            rl = [self.sb(ps, f"rl{k}", [128, 512], F32) for k in range(2)]
            pT = [self.sb(ps, f"pT{k}", [128, 512], BF16) for k in range(3)]
            mn = self.sb(ps, "mn", [128, 1], F32)
            mx = self.sb(ps, "mx", [128, 1], F32)
            w0 = self.sb(ps, "w0", [128, 1], F32)
            halfs = self.sb(ps, "halfs", [128, 24], F32)
            lo = self.sb(ps, "lo", [128, 1], F32)
            mid = self.sb(ps, "mid", [128, 1], F32)
            cnt = self.sb(ps, "cnt", [128, 24], F32)
            c256 = self.sb(ps, "c256", [128, 1], F32)
            S.memset(c256[:, :], 256.0)
            step = self.sb(ps, "step", [128, 1], F32)
            rs = self.sb(ps, "rs", [128, 4, 1], F32)
            rr = self.sb(ps, "rr", [128, 4, 1], F32)
            mo = [self.sb(ps, f"mo{k}", [128, 512], BF16) for k in range(2)]
            bk = self.banks(ps, 8)
            idxb = bk[0:2]
            scb = bk[2:5]
            accb = bk[5:7]
            cnts = dict(u=0, v=0, a=0)
            fill = self.make_fill(ps, bk[7], ident4=ident4)

            def index_tile(qt):
                k2 = qt % 2
                N = 128 * (qt + 1)
                qs = slice(qt * 128, (qt + 1) * 128)
                S.dma(iqT[k2][:, :, :], qk_d[R_IQ:R_IQ + 256, qs].rearrange("(h d) q -> d h q", d=64))
                S.dma(bqT[k2][:, :].rearrange("d (h q) -> d h q", h=8), qk_d[R_BQ:R_BQ + 512, qs].rearrange("(h d) q -> d h q", d=64))
                S.dma(iwt[k2][:, :], d["iw"][qs, :])
                sc = score[k2]
                nch = (N + 511) // 512
                for ch in range(nch):
                    w = min(512, N - ch * 512)
                    ks = slice(ch * 512, ch * 512 + w)
                    for h in range(4):
                        pl = idxb[cnts["v"] % 2]
                        r = rl[cnts["v"] % 2]
                        cnts["v"] += 1
                        S.matmul(pl[:, :w], lhsT=iqT[k2][:, h, :], rhs=ikT[:, ks], start=True, stop=True)
                        S.act(r[:, :w], pl[:, :w], AF.Relu)
                        if h == 0:
                            S.ts(sc[:, ks], r[:, :w], iwt[k2][:, 0:1], None, ALU.mult)
                        else:
                            S.stt(sc[:, ks], r[:, :w], iwt[k2][:, h:h + 1], sc[:, ks], ALU.mult, ALU.add)
                S.reduce(mn[:, :], sc[:, :N], ALU.min)
                S.reduce(mx[:, :], sc[:, :N], ALU.max)
                S.tt(sc[:, N - 128:N], sc[:, N - 128:N], causalf[:, :], ALU.add)
                S.tt(w0[:, :], mx[:, :], mn[:, :], ALU.subtract)
                S.ts(halfs[:, :], pow2[:, :], w0[:, 0:1], None, ALU.mult)
                S.memset(cnt[:, :], 0.0)
                S.copy(lo[:, :], mn[:, :])
                S.tt(mid[:, :], mn[:, :], halfs[:, 0:1], ALU.add)
                for n in range(nit):
                    S.ts(junk[:, :N], sc[:, :N], mid[:, 0:1], 0.0, ALU.is_ge, ALU.add, accum_out=cnt[:, n:n + 1])
                    S.ts(step[:, :], cnt[:, n:n + 1], c256[:, 0:1], halfs[:, n:n + 1], ALU.is_ge, ALU.mult)
                    S.tt(lo[:, :], lo[:, :], step[:, :], ALU.add)
                    if n + 1 < nit:
                        S.tt(mid[:, :], lo[:, :], halfs[:, n + 1:n + 2], ALU.add)
                S.ts(biasq[k2][:, :N], sc[:, :N], lo[:, 0:1], NEGB, ALU.is_lt, ALU.mult)
                if "dbg_lo" in d:
                    dl = self.sb(ps, f"dl{qt}", [128, 4], F32)
                    S.copy(dl[:, 0:1], lo[:, :])
                    S.copy(dl[:, 1:2], cnt[:, nit - 1:nit])
                    S.copy(dl[:, 2:3], mn[:, :])
                    S.copy(dl[:, 3:4], mx[:, :])
                    S.dma(d["dbg_lo"].at(qt)[qs, :], dl[:, :])

            def attend_tile(qt):
                k2 = qt % 2
                m = mo[qt % 2]
                units = []
                for g in range(2):
                    acc = accb[cnts["a"] % 2]
                    cnts["a"] += 1
                    for kt in range(qt + 1):
                        units.append(make_unit(qt, k2, m, g, kt, acc))
                self.run_units(units, fill=fill, nfill=NFILL)
                S.dma(mix_d.at(("b", qt))[qt * 128:(qt + 1) * 128, 512:1024], m[:, :])

            def make_unit(qt, k2, m, g, kt, acc):
                uu = cnts["u"]
                cnts["u"] += 1
                sc = scb[uu % 3]
                p = pT[uu % 3]
                ks = slice(kt * 128, (kt + 1) * 128)

                def score():
                    S.matmul(sc[:, :], lhsT=bkT[:, g, ks], rhs=bqT[k2][:, g * 512:(g + 1) * 512], start=True, stop=False)
                    S.matmul(sc[:, :], lhsT=biasq[k2][:, ks], rhs=ident4[:, :], start=False, stop=True)
                    S.act(p[:, :], sc[:, :], AF.Exp, scale=SCALE)

                def pv():
                    for r in range(4):
                        S.matmul(acc[:, r * 65:(r + 1) * 65], lhsT=p[:, r * 128:(r + 1) * 128], rhs=Vaug[:, kt, g, :],
                                 start=(kt == 0 and r == 0), stop=(kt == qt), skip=True)
                    if kt == qt:
                        a3 = acc[:, 0:260].rearrange("p (r e) -> p r e", e=65)
                        S.ts(rs[:, :, :], a3[:, :, 64:65], 1e-30, None, ALU.max)
                        S.recip(rr[:, :, :], rs[:, :, :])
                        for r in range(4):
                            hcol = (4 * g + r) * 64
                            S.ts(m[:, hcol:hcol + 64], acc[:, r * 65:r * 65 + 64], rr[:, r, :], None, ALU.mult)
                return (score, pv)

            index_tile(0)
            for qt in range(NT):
                if qt + 1 < NT:
                    index_tile(qt + 1)
                attend_tile(qt)
            S.flush()

    def ln_alloc(self, ps, L, which, nbuf=2):
        S, d = self.S, self.d
        r = {"nbuf": nbuf}
        r["g"] = self.sb(ps, "lng", [128, D], F32)
        r["b"] = self.sb(ps, "lnb", [128, D], F32)
        S.dma(r["g"][:, :], d[f"ln_{which}_g"][L:L + 1, :].broadcast_to([128, D]))
        S.dma(r["b"][:, :], d[f"ln_{which}_b"][L:L + 1, :].broadcast_to([128, D]))
        r["xr"] = [self.sb(ps, f"xr{k}", [128, D], F32) for k in range(nbuf)]
        r["z"] = [self.sb(ps, f"z{k}", [128, D], F32) for k in range(nbuf)]
        r["xn"] = [self.sb(ps, f"xn{k}", [128, D], F32) for k in range(nbuf)]
        r["st"] = [self.sb(ps, f"st{k}", [128, 2, 6], F32) for k in range(nbuf)]
        r["mv"] = [self.sb(ps, f"mv{k}", [128, 2], F32) for k in range(nbuf)]
        r["rstd"] = [self.sb(ps, f"rstd{k}", [128, 1], F32) for k in range(nbuf)]
        r["stage"] = [self.sb(ps, f"stage{k}", [128, 8, 512], BF16) for k in range(nbuf)]
        r["identf"] = self.const(ps, "c_identf", [128, 128], F32)
        return r

    def ln_tile(self, r, t, y_views, res_src, dst, tbanks, want_xT=True, xT32=None, defer=False):
        S, d = self.S, self.d
        nb = r["nbuf"]
        k = t % nb
        tk = slice(t * 128, (t + 1) * 128)
        xr, z, xn = r["xr"][k], r["z"][k], r["xn"][k]
        S.dma(xr[:, :], View(res_src.at(t), res_src.t[tk, :]))
        for hh in range(2):
            cs = slice(hh * 512, (hh + 1) * 512)
            S.stt(z[:, cs], xr[:, cs], float(DN_ALPHA), y_views[hh], ALU.mult, ALU.add)
            S.bn_stats(r["st"][k][:, hh, :], z[:, cs])
        S.bn_aggr(r["mv"][k][:, :], r["st"][k][:, :, :])
        rstd = r["rstd"][k]
        S.ts(rstd[:, :], r["mv"][k][:, 1:2], LN_EPS, None, ALU.add)
        S.act(rstd[:, :], rstd[:, :], AF.Ln)
        S.act(rstd[:, :], rstd[:, :], AF.Exp, scale=-0.5)
        S.ts(xn[:, :], z[:, :], r["mv"][k][:, 0:1], rstd[:, 0:1], ALU.subtract, ALU.mult)
        S.tt(xn[:, :], xn[:, :], r["g"][:, :], ALU.mult, eng="gpsimd")
        S.tt(xn[:, :], xn[:, :], r["b"][:, :], ALU.add, eng="gpsimd")
        final = dst.name == "out"
        S.dma(View(dst.at(t), dst.t[tk, :]), xn[:, :], final=final)
        if want_xT:
            def later():
                self.emit_xT(xn[:, :], t, r["identf"], tbanks, r["stage"][(t // 4) % nb], d["xT"], xT32=xT32)
            if defer:
                return later
            later()
        return None

    def phase_outproj(self, L):
        S, d = self.S, self.d
        even = L % 2 == 0
        i = L // 2
        wout_d = d[f"b_ev_wout{i}"] if even else d[f"b_od_wout{i}"]
        res_src = d["x"] if (L == 0 or "force_x" in self.dbg) else d["xres"]
        with ExitStack() as ps:
            r = self.ln_alloc(ps, L, "mix")
            ident = self.const(ps, "c_ident", [128, 128], BF16)
            wout = self.sb(ps, "wout", [128, 8, D], BF16)
            S.dma(wout[:, :, :], wout_d[:, :].rearrange("(c p) n -> p c n", p=128))
            mixt = [self.sb(ps, f"mixt{k}", [128, D], BF16) for k in range(2)]
            mixT = [self.sb(ps, f"mixT{k}", [128, 8, 128], BF16) for k in range(2)]
            bk = self.banks(ps, 8)
            if not even:
                wr = self.sb(ps, "wr", [128, 8, 8], F32)
                S.dma(wr[:, :, :], d["moe_w_router"][i].rearrange("(c p) e -> p c e", p=128))
                br = self.sb(ps, "br", [128, 8], F32)
                S.dma(br[:, :], d["moe_b_router"][i:i + 1, :].broadcast_to([128, 8]))
                xT32 = [self.sb(ps, f"xT32_{k}", [128, 8, 128], F32) for k in range(2)]
                lg = [self.sb(ps, f"lg{k}", [128, 8], F32) for k in range(2)]
                m8 = [self.sb(ps, f"m8{k}", [128, 8], F32) for k in range(2)]
                dd = [self.sb(ps, f"dd{k}", [128, 1], F32) for k in range(2)]
                g12 = [self.sb(ps, f"g12{k}", [128, 2], F32) for k in range(2)]
                G = [self.sb(ps, f"G{k}", [128, 8], F32) for k in range(2)]
                G2 = [self.sb(ps, f"G2{k}", [128, 8], F32) for k in range(2)]
            pends = []
            for t in range(NT):
                k = t % 2
                tk = slice(t * 128, (t + 1) * 128)
                S.dma(mixt[k][:, :], View(d["mix"].at(("r", t)), d["mix"].t[tk, :]))
                for hb in range(2):
                    tb = bk[hb]
                    for cc in range(4):
                        c = hb * 4 + cc
                        S.matmul(tb[:, cc * 128:(cc + 1) * 128], lhsT=mixt[k][:, c * 128:(c + 1) * 128], rhs=ident[:, :],
                                 start=True, stop=True, skip=True)
                    S.copy(mixT[k][:, hb * 4:(hb + 1) * 4, :], tb[:, :].rearrange("p (c t) -> p c t", c=4),
                           eng="scalar" if hb == 0 else "vector")
                yb = [bk[2 + 2 * k], bk[3 + 2 * k]]
                for hh in range(2):
                    for c in range(8):
                        S.matmul(yb[hh][:, :], lhsT=mixT[k][:, c, :], rhs=wout[:, c, hh * 512:(hh + 1) * 512],
                                 start=(c == 0), stop=(c == 7))
                pend = self.ln_tile(r, t, [yb[0][:, :], yb[1][:, :]], res_src, d["xres"], bk[6:8],
                                    xT32=None if even else xT32[k], defer=True)
                pends.append((t, k, pend))
                if len(pends) > 1:
                    self._outproj_tail(*pends.pop(0), even, locals())
            while pends:
                self._outproj_tail(*pends.pop(0), even, locals())
            if False:
                if not even:
                    lp = bk[0]
                    for c in range(8):
                        S.matmul(lp[:, 0:8], lhsT=xT32[k][:, c, :], rhs=wr[:, c, :], start=(c == 0), stop=(c == 7))
                    S.tt(lg[k][:, :], lp[:, 0:8], br[:, :], ALU.add)
                    S.max8(m8[k][:, :], lg[k][:, :])
                    S.tt(dd[k][:, :], m8[k][:, 0:1], m8[k][:, 1:2], ALU.subtract)
                    S.act(g12[k][:, 0:1], dd[k][:, :], AF.Sigmoid)
                    S.act(g12[k][:, 1:2], dd[k][:, :], AF.Sigmoid, scale=-1.0)
                    S.ts(G[k][:, :], lg[k][:, :], m8[k][:, 0:1], g12[k][:, 0:1], ALU.is_equal, ALU.mult)
                    S.ts(G2[k][:, :], lg[k][:, :], m8[k][:, 1:2], g12[k][:, 1:2], ALU.is_equal, ALU.mult)
                    S.tt(G[k][:, :], G[k][:, :], G2[k][:, :], ALU.add)
                    S.dma(d["moeg"].at(t)[tk, :], G[k][:, :])
                    S.ts(G2[k][:, :], lg[k][:, :], m8[k][:, 0:1], None, ALU.is_equal)
                    S.dma(d["moem1"].at(t)[tk, :], G2[k][:, :])
                    S.ts(G[k][:, :], lg[k][:, :], m8[k][:, 1:2], None, ALU.is_equal)
                    S.dma(d["moem2"].at(t)[tk, :], G[k][:, :])
                    S.dma(d["moegv"].at(t)[tk, :], g12[k][:, :])
            S.flush()

    def _outproj_tail(self, t, k, pend, even, L_):
        S, d = self.S, self.d
        if pend is not None:
            pend()
        if even:
            return
        bk, xT32, wr, br = L_["bk"], L_["xT32"], L_["wr"], L_["br"]
        lg, m8, dd, g12, G, G2 = L_["lg"], L_["m8"], L_["dd"], L_["g12"], L_["G"], L_["G2"]
        tk = slice(t * 128, (t + 1) * 128)
        lp = bk[0]
        for c in range(8):
            S.matmul(lp[:, 0:8], lhsT=xT32[k][:, c, :], rhs=wr[:, c, :], start=(c == 0), stop=(c == 7))
        S.tt(lg[k][:, :], lp[:, 0:8], br[:, :], ALU.add)
        S.max8(m8[k][:, :], lg[k][:, :])
        S.tt(dd[k][:, :], m8[k][:, 0:1], m8[k][:, 1:2], ALU.subtract)
        S.act(g12[k][:, 0:1], dd[k][:, :], AF.Sigmoid)
        S.act(g12[k][:, 1:2], dd[k][:, :], AF.Sigmoid, scale=-1.0)
        S.ts(G[k][:, :], lg[k][:, :], m8[k][:, 0:1], g12[k][:, 0:1], ALU.is_equal, ALU.mult)
        S.ts(G2[k][:, :], lg[k][:, :], m8[k][:, 1:2], g12[k][:, 1:2], ALU.is_equal, ALU.mult)
        S.tt(G[k][:, :], G[k][:, :], G2[k][:, :], ALU.add)
        S.dma(d["moeg"].at(t)[tk, :], G[k][:, :])
        S.ts(G2[k][:, :], lg[k][:, :], m8[k][:, 0:1], None, ALU.is_equal)
        S.dma(d["moem1"].at(t)[tk, :], G2[k][:, :])
        S.ts(G[k][:, :], lg[k][:, :], m8[k][:, 1:2], None, ALU.is_equal)
        S.dma(d["moem2"].at(t)[tk, :], G[k][:, :])
        S.dma(d["moegv"].at(t)[tk, :], g12[k][:, :])

    def phase_ffn_dense(self, L):
        S, d = self.S, self.d
        i = L // 2
        NF = F_DENSE // 128
        final = L == DEPTH - 1
        dst = d["out"] if final else d["xres"]
        with ExitStack() as ps:
            r = self.ln_alloc(ps, L, "ffn")
            Wd = self.sb(ps, "Wd", [128, NF, D], BF16)
            wd_v = d[f"b_ffd{i}"][:, :].rearrange("(f p) n -> p f n", p=128)
            for f0 in range(0, NF, 4):
                f1 = min(NF, f0 + 4)
                S.dma(Wd.at(f0)[:, f0:f1, :], wd_v[:, f0:f1, :])
            xTc = [self.sb(ps, f"xTc{k}", [128, 8, 512], BF16) for k in range(2)]
            Wg = [self.sb(ps, f"Wg{k}", [128, 8, 256], BF16) for k in range(2)]
            Wu = [self.sb(ps, f"Wu{k}", [128, 8, 256], BF16) for k in range(2)]
            hT = self.sb(ps, "hT", [128, NF, 512], BF16)
            sg = [self.sb(ps, f"sg{k}", [128, 512], F32) for k in range(2)]
            bk = self.banks(ps, 8)
            g_v = d[f"b_ffg{i}"][:, :].rearrange("(c p) n -> p c n", p=128)
            u_v = d[f"b_ffu{i}"][:, :].rearrange("(c p) n -> p c n", p=128)
            xT_v = d["xT"][:, :].rearrange("(c p) t -> p c t", p=128)
            u = 0
            w = 0
            pend = None
            for tc in range(8):
                xc = xTc[tc % 2]
                S.dma(xc[:, :, :], View(d["xT"].at(("c", tc)), xT_v.ap[:, :, tc * 512:(tc + 1) * 512]))
                for fg in range(NF // 2):
                    wg, wu = Wg[w % 2], Wu[w % 2]
                    w += 1
                    S.dma(wg[:, :, :], g_v[:, :, fg * 256:(fg + 1) * 256])
                    S.dma(wu[:, :, :], u_v[:, :, fg * 256:(fg + 1) * 256])
                    for f2 in range(2):
                        ft = fg * 2 + f2
                        pg, pu = bk[(2 * u) % 4], bk[(2 * u + 1) % 4]
                        for c in range(8):
                            S.matmul(pg[:, :], lhsT=wg[:, c, f2 * 128:(f2 + 1) * 128], rhs=xc[:, c, :], start=(c == 0), stop=(c == 7))
                        for c in range(8):
                            S.matmul(pu[:, :], lhsT=wu[:, c, f2 * 128:(f2 + 1) * 128], rhs=xc[:, c, :], start=(c == 0), stop=(c == 7))
                        S.act(sg[u % 2][:, :], pg[:, :], AF.Silu)
                        S.tt(View(hT.at(ft), hT.t[:, ft, :]), sg[u % 2][:, :], pu[:, :], ALU.mult)
                        u += 1
                for t4 in range(4):
                    t = tc * 4 + t4
                    yb = [bk[4], bk[5]]
                    for hh in range(2):
                        for ft in range(NF):
                            S.matmul(yb[hh][:, :], lhsT=View(hT.at(ft), hT.t[:, ft, t4 * 128:(t4 + 1) * 128]),
                                     rhs=View(Wd.at((ft // 4) * 4), Wd.t[:, ft, hh * 512:(hh + 1) * 512]),
                                     start=(ft == 0), stop=(ft == NF - 1))
                    if pend is not None:
                        pend()
                    pend = self.ln_tile(r, t, [yb[0][:, :], yb[1][:, :]], d["xres"], dst, bk[6:8], want_xT=not final,
                                        defer=True)
            if pend is not None:
                pend()
            S.flush()

    def phase_cmp(self, L):
        S, d = self.S, self.d
        i = L // 2
        qk_d = d["qk"]
        with ExitStack() as ps:
            bk = self.banks(ps, 8)
            w1 = [self.sb(ps, f"w1_{a}", [64, 32, 256], BF16) for a in range(2)]
            w2 = [self.sb(ps, f"w2_{a}", [128, 2, 64], BF16) for a in range(2)]
            peT = [self.sb(ps, f"peT{a}", [64, 32], F32) for a in range(2)]
            for a in range(2):
                S.dma(w1[a][:, :, :], d[f"b_phi1_{i}"][a * 2048:(a + 1) * 2048, :].rearrange("(l d) j -> d l j", d=64))
                S.dma(w2[a][:, :, :], d[f"b_phi2_{i}"][a * 256:(a + 1) * 256, :].rearrange("(jh p) e -> p jh e", p=128))
                S.dma(peT[a][:, :], d[f"nsa_peT{i}"][a])
            src = [self.sb(ps, f"src{k}", [64, SEQ], BF16) for k in range(2)]
            hl = [self.sb(ps, f"hl{k}", [64, 32, 256], BF16) for k in range(2)]
            zs = [self.sb(ps, f"zs{k}", [128, 256], F32) for k in range(2)]
            z2 = [self.sb(ps, f"z2{k}", [128, 256], F32) for k in range(2)]
            sgm = [self.sb(ps, f"sgm{k}", [128, 256], F32) for k in range(2)]
            gz = [self.sb(ps, f"gz{k}", [128, 2, 256], BF16) for k in range(2)]
            ko = [self.sb(ps, f"ko{k}", [64, 256], BF16) for k in range(2)]
            vo = [self.sb(ps, f"vo{k}", [128, 2, 64], BF16) for k in range(2)]
            for k in range(2):
                S.memset(gz[k][:, :, :], 0.0)
                S.memset(ko[k][:, :], 0.0)
            n = 0
            for a in range(2):
                for g in range(2):
                    k = n % 2
                    n += 1
                    r0 = (1792 if a == 0 else 1920) + g * 64
                    S.dma(src[k][:, :], qk_d[r0:r0 + 64, :])
                    for l in range(32):
                        S.ts(hl[k][:, l, 0:255], src[k][:, l:l + 16 * 254 + 1:16], peT[a][:, l:l + 1], None, ALU.add,
                             eng="vector" if l % 2 == 0 else "gpsimd")
                    for jh in range(2):
                        zb = bk[jh]
                        for l in range(32):
                            S.matmul(zb[:, 0:255], lhsT=w1[a][:, l, jh * 128:(jh + 1) * 128], rhs=hl[k][:, l, 0:255],
                                     start=(l == 0), stop=(l == 31))
                        S.copy(zs[jh][:, 0:255], zb[:, 0:255], eng="scalar")
                        S.tt(z2[jh][:, 0:255], zs[jh][:, 0:255], zs[jh][:, 0:255], ALU.mult)
                        S.ts(z2[jh][:, 0:255], z2[jh][:, 0:255], 0.044715, 1.0, ALU.mult, ALU.add)
                        S.tt(z2[jh][:, 0:255], z2[jh][:, 0:255], zs[jh][:, 0:255], ALU.mult)
                        S.act(sgm[jh][:, 0:255], z2[jh][:, 0:255], AF.Sigmoid, scale=1.5957691216057308)
                        S.tt(gz[k][:, jh, 0:255], zs[jh][:, 0:255], sgm[jh][:, 0:255], ALU.mult)
                    if a == 0:
                        ob = bk[2]
                        for jh in range(2):
                            S.matmul(ob[0:64, 0:255], lhsT=w2[a][:, jh, :], rhs=gz[k][:, jh, 0:255], start=(jh == 0), stop=(jh == 1))
                        S.copy(ko[g][:, 0:255], ob[0:64, 0:255])
                        S.dma(d["kcmpT"].at(g)[g], ko[g][:, :])
                    else:
                        for nt in range(2):
                            ob = bk[3 + nt]
                            for jh in range(2):
                                S.matmul(ob[:, 0:64], lhsT=gz[k][:, jh, nt * 128:(nt + 1) * 128], rhs=w2[a][:, jh, :],
                                         start=(jh == 0), stop=(jh == 1))
                            S.copy(vo[g][:, nt, :], ob[:, 0:64])
                        S.dma(d["vcmp"].at(g)[g].rearrange("(nt p) e -> p nt e", p=128), vo[g][:, :, :])
            S.flush()

    def phase_moba(self, L):
        S, d = self.S, self.d
        qk_d, vt_d, mix_d = d["qk"], d["vt"], d["mix"]
        with ExitStack() as ps:
            ident = self.const(ps, "c_ident", [128, 128], BF16)
            causal = self.const(ps, "c_causal", [128, 128], BF16)
            e16 = self.const(ps, "c_e16", [16, 16, 128], BF16)
            QT = [self.sb(ps, f"QT{k}", [64, SEQ], BF16) for k in range(2)]
            KT = [self.sb(ps, f"KT{k}", [64, SEQ], BF16) for k in range(2)]
            Vaug = [self.sb(ps, f"Vaug{k}", [128, NT, 65], BF16) for k in range(2)]
            kmT = [self.sb(ps, f"kmT{k}", [64, 16], BF16) for k in range(2)]
            for k in range(2):
                S.memset(Vaug[k][:, :, 64:65], 1.0)
            gate = [self.sb(ps, f"gate{k}", [128, 2, 16], F32) for k in range(2)]
            m8 = [self.sb(ps, f"m8{k}", [128, 2, 8], F32) for k in range(2)]
            bias = [self.sb(ps, f"bias{k}", [128, 2, 16], BF16) for k in range(2)]
            biasT = [self.sb(ps, f"biasT{k}", [16, 16, 256], BF16) for k in range(2)]
            pT = [self.sb(ps, f"pT{k}", [128, 256], BF16) for k in range(3)]
            rs = [self.sb(ps, f"rs{k}", [128, 2, 1], F32) for k in range(2)]
            rr = [self.sb(ps, f"rr{k}", [128, 2, 1], F32) for k in range(2)]
            mo = [self.sb(ps, f"mo{k}", [128, NT, 64], BF16) for k in range(2)]
            bk = self.banks(ps, 8)
            scb = bk[0:3]
            accb = bk[3:5]
            gpb = [bk[5], bk[5]]
            tpb = bk[7]
            cnt = dict(u=0)
            fill = self.make_fill(ps, bk[6])

            def make_unit(h, hk, B, kt, qt_, kt_, va, acc):
                uu = cnt["u"]
                cnt["u"] += 1
                sc = scb[uu % 3]
                p = pT[uu % 3]
                ks = slice(kt * 128, (kt + 1) * 128)
                col0 = 128 if kt == 2 * B + 1 else 0
                past = kt < 2 * B
                sel = B > 3
                k2 = B % 2

                def score():
                    last_first = not ((past and sel) or (not past))
                    S.matmul(sc[:, col0:256], lhsT=kt_[:, ks], rhs=qt_[:, B * 256 + col0:(B + 1) * 256],
                             start=True, stop=last_first)
                    if past and sel:
                        S.matmul(sc[:, 0:256], lhsT=e16[:, kt // 2, :], rhs=biasT[hk][:, B, :], start=False, stop=True)
                    if not past:
                        c0 = (kt - 2 * B) * 128
                        S.matmul(sc[:, c0:c0 + 128], lhsT=causal[:, :], rhs=ident[:, :], start=False, stop=True)
                    S.act(p[:, col0:256], sc[:, col0:256], AF.Exp, scale=SCALE)

                def pv():
                    for t in range(col0 // 128, 2):
                        S.matmul(acc[:, t * 65:(t + 1) * 65], lhsT=p[:, t * 128:(t + 1) * 128], rhs=va[:, kt, :],
                                 start=(kt == 0 and t == 0), stop=(kt == 2 * B + t), skip=True)
                    if kt == 2 * B + 1:
                        a3 = acc[:, 0:130].rearrange("p (t e) -> p t e", e=65)
                        S.ts(rs[k2][:, :, :], a3[:, :, 64:65], 1e-30, None, ALU.max)
                        S.recip(rr[k2][:, :, :], rs[k2][:, :, :])
                        for t in range(2):
                            S.ts(mo[hk][:, 2 * B + t, :], acc[:, t * 65:t * 65 + 64], rr[k2][:, t, :], None, ALU.mult)
                return (score, pv)

            for h in range(8):
                hk = h % 2
                qt_, kt_, va = QT[hk], KT[hk], Vaug[hk]
                S.dma(qt_[:, :], qk_d[h * 64:(h + 1) * 64, :])
                S.dma(kt_[:, :], qk_d[512 + h * 64:512 + (h + 1) * 64, :])
                S.dma(va[:, :, 0:64], vt_d[:, h * 64:(h + 1) * 64].rearrange("(kt p) e -> p kt e", p=128))
                S.dma(kmT[hk][:, :], d["kmean"][h * 64:(h + 1) * 64, :])
                for B in range(4, 16):
                    k2 = B % 2
                    gp = gpb[k2]
                    for t in range(2):
                        S.matmul(gp[:, t * 16:(t + 1) * 16], lhsT=qt_[:, (2 * B + t) * 128:(2 * B + t + 1) * 128],
                                 rhs=kmT[hk][:, :], start=True, stop=True, skip=True)
                    S.memset(gate[k2][:, :, :], -1e30)
                    S.copy(gate[k2][:, :, 0:B], gp[:, 0:32].rearrange("p (t n) -> p t n", t=2)[:, :, 0:B])
                    for t in range(2):
                        S.max8(m8[k2][:, t, :], gate[k2][:, t, :])
                        S.ts(bias[k2][:, t, :], gate[k2][:, t, :], m8[k2][:, t, 2:3], NEGB, ALU.is_lt, ALU.mult)
                        S.matmul(tpb[0:16, t * 128:(t + 1) * 128], lhsT=bias[k2][:, t, :], rhs=ident[:, :],
                                 start=True, stop=True, skip=True)
                    S.copy(biasT[hk][:, B, :], tpb[0:16, 0:256], eng="scalar")
                units = []
                for B in range(16):
                    acc = accb[B % 2]
                    for kt in range(2 * B + 2):
                        units.append(make_unit(h, hk, B, kt, qt_, kt_, va, acc))
                self.run_units(units, fill=fill, nfill=NFILL)
                mv = mix_d[:, h * 64:(h + 1) * 64].rearrange("(qt p) e -> p qt e", p=128)
                for q4 in range(4):
                    S.dma(View(mix_d.at(("c", h, q4)), mv.ap[:, q4 * 8:(q4 + 1) * 8, :]), mo[hk][:, q4 * 8:(q4 + 1) * 8, :])
            S.flush()

    def phase_nsa(self, L):
        S, d = self.S, self.d
        qk_d, vt_d, mix_d = d["qk"], d["vt"], d["mix"]
        with ExitStack() as ps:
            ident = self.const(ps, "c_ident", [128, 128], BF16)
            ident4 = self.const(ps, "c_ident4", [128, 512], BF16)
            causal = self.const(ps, "c_causal", [128, 128], BF16)
            winfar = self.const(ps, "c_winfar", [128, 128], BF16)
            e64 = self.const(ps, "c_e64", [64, 32, 128], BF16)
            KsT = self.sb(ps, "KsT", [64, SEQ], BF16)
            KwT = self.sb(ps, "KwT", [64, SEQ], BF16)
            Vs = self.sb(ps, "Vs", [128, NT, 65], BF16)
            Vw = self.sb(ps, "Vw", [128, NT, 65], BF16)
            kcT = self.sb(ps, "kcT", [64, 256], BF16)
            Vc = self.sb(ps, "Vc", [128, 2, 129], BF16)
            qraw = [self.sb(ps, f"qraw{k}", [64, 512], BF16) for k in range(2)]
            qrot = [self.sb(ps, f"qrot{k}", [64, 512], BF16) for k in range(2)]
            cmpb = [self.sb(ps, f"cmpb{k}", [128, 256], BF16) for k in range(2)]
            sadd = [self.sb(ps, f"sadd{k}", [128, 64], F32) for k in range(2)]
            gts = [self.sb(ps, f"gts{k}", [128, 4, 3], F32) for k in range(2)]
            pT = [self.sb(ps, f"pT{k}", [128, 512], BF16) for k in range(3)]
            rcp = [self.sb(ps, f"rcp{k}", [128, 4, 3], F32) for k in range(2)]
            sums = [self.sb(ps, f"sums{k}", [128, 4, 3], F32) for k in range(2)]
            coef = [self.sb(ps, f"coef{k}", [128, 4, 3], F32) for k in range(2)]
            imp = [self.sb(ps, f"imp{k}", [128, 64], F32) for k in range(2)]
            imp3 = [self.sb(ps, f"imp3{k}", [128, 64], F32) for k in range(2)]
            m8a = [self.sb(ps, f"m8a{k}", [128, 8], F32) for k in range(2)]
            m8b = [self.sb(ps, f"m8b{k}", [128, 8], F32) for k in range(2)]
            sbias = [self.sb(ps, f"sbias{k}", [128, 64], BF16) for k in range(2)]
            biasT4 = [self.sb(ps, f"biasT4{k}", [64, 512], BF16) for k in range(2)]
            oo = [self.sb(ps, f"oo{k}", [128, 64], F32) for k in range(2)]
            mo = [self.sb(ps, f"mo{k}", [128, 256], BF16) for k in range(2)]
            bk = self.banks(ps, 8)
            scb = bk[0:3]
            accC = bk[3:5]
            accS, accW = bk[5], bk[6]
            cnt = dict(u=0)
            fill = self.make_fill(ps, bk[7], ident4=ident4)
            for g in range(2):
                S.dma(KsT[:, :], qk_d[1536 + g * 64:1536 + (g + 1) * 64, :])
                S.dma(KwT[:, :], qk_d[1664 + g * 64:1664 + (g + 1) * 64, :])
                S.memset(Vs[:, :, 64:65], 1.0)
                S.memset(Vw[:, :, 64:65], 1.0)
                S.memset(Vc[:, :, 64:65], 1.0)
                S.dma(Vs[:, :, 0:64], vt_d[:, 512 + g * 64:512 + (g + 1) * 64].rearrange("(kt p) e -> p kt e", p=128))
                S.dma(Vw[:, :, 0:64], vt_d[:, 640 + g * 64:640 + (g + 1) * 64].rearrange("(kt p) e -> p kt e", p=128))
                S.dma(kcT[:, :], d["kcmpT"][g])
                S.dma(Vc[:, :, 0:64], d["vcmp"][g].rearrange("(nt p) e -> p nt e", p=128))
                S.dma(Vc[:, :, 65:129], d["c_c2s"][:, :].rearrange("(nt p) e -> p nt e", p=128))
                for qt in range(NT):
                    k2 = qt % 2
                    qs = slice(qt * 128, (qt + 1) * 128)
                    S.dma(qraw[k2][:, :].rearrange("d (r q) -> d r q", r=4),
                          d["qraw"][g * 256:(g + 1) * 256, qs].rearrange("(r d) q -> d r q", d=64))
                    S.dma(qrot[k2][:, :].rearrange("d (r q) -> d r q", r=4),
                          qk_d[1024 + g * 256:1024 + (g + 1) * 256, qs].rearrange("(r d) q -> d r q", d=64))
                    S.dma(cmpb[k2][:, :], d["c_cmpb"][qt])
                    S.dma(sadd[k2][:, :], d["c_seladd"][qt])
                    S.dma(gts[k2][:, :, :], d["gates"][qs, g * 12:(g + 1) * 12].rearrange("p (r b) -> p r b", b=3))
                    def mk(kind, kt, k2=k2, qt=qt):
                        uu = cnt["u"]
                        cnt["u"] += 1
                        sc = scb[uu % 3]
                        p = pT[uu % 3]
                        ks = slice(kt * 128, (kt + 1) * 128)

                        def score():
                            if kind == "c":
                                S.matmul(sc[:, :], lhsT=kcT[:, ks], rhs=qraw[k2][:, :], start=True, stop=False)
                                S.matmul(sc[:, :], lhsT=cmpb[k2][:, ks], rhs=ident4[:, :], start=False, stop=True)
                            elif kind == "s":
                                S.matmul(sc[:, :], lhsT=KsT[:, ks], rhs=qrot[k2][:, :], start=True, stop=False)
                                S.matmul(sc[:, :], lhsT=e64[:, kt, :], rhs=biasT4[k2][:, :], start=False, stop=(kt != qt))
                                if kt == qt:
                                    S.matmul(sc[:, :], lhsT=causal[:, :], rhs=ident4[:, :], start=False, stop=True)
                            else:
                                edge = (kt == qt) or (kt == qt - 4)
                                S.matmul(sc[:, :], lhsT=KwT[:, ks], rhs=qrot[k2][:, :], start=True, stop=not edge)
                                if kt == qt:
                                    S.matmul(sc[:, :], lhsT=causal[:, :], rhs=ident4[:, :], start=False, stop=True)
                                elif kt == qt - 4:
                                    S.matmul(sc[:, :], lhsT=winfar[:, :], rhs=ident4[:, :], start=False, stop=True)
                            S.act(p[:, :], sc[:, :], AF.Exp, scale=SCALE)

                        def pv():
                            if kind == "c":
                                for r in range(4):
                                    o0 = (r % 2) * 129
                                    S.matmul(accC[r // 2][:, o0:o0 + 129], lhsT=p[:, r * 128:(r + 1) * 128], rhs=Vc[:, kt, :],
                                             start=(kt == 0 and r % 2 == 0), stop=(kt == 1), skip=True)
                            elif kind == "s":
                                for r in range(4):
                                    S.matmul(accS[:, r * 65:(r + 1) * 65], lhsT=p[:, r * 128:(r + 1) * 128], rhs=Vs[:, kt, :],
                                             start=(kt == 0 and r == 0), stop=(kt == qt), skip=True)
                            else:
                                k0 = max(0, qt - 4)
                                for r in range(4):
                                    S.matmul(accW[:, r * 65:(r + 1) * 65], lhsT=p[:, r * 128:(r + 1) * 128], rhs=Vw[:, kt, :],
                                             start=(kt == k0 and r == 0), stop=(kt == qt), skip=True)
                        return (score, pv)

                    units = [mk("c", 0), mk("c", 1)] + [mk("w", kt) for kt in range(max(0, qt - 4), qt + 1)]
                    self.run_units(units, fill=fill, nfill=NFILL)
                    for r in range(4):
                        o0 = (r % 2) * 129
                        S.ts(sums[k2][:, r, 0:1], accC[r // 2][:, o0 + 64:o0 + 65], 1e-30, None, ALU.max)
                    S.recip(rcp[k2][:, :, 0:1], sums[k2][:, :, 0:1])
                    for r in range(4):
                        o0 = (r % 2) * 129
                        iu = accC[r // 2][:, o0 + 65:o0 + 129]
                        if r == 0:
                            S.ts(imp[k2][:, :], iu, rcp[k2][:, 0, 0:1], None, ALU.mult)
                        else:
                            S.stt(imp[k2][:, :], iu, rcp[k2][:, r, 0:1], imp[k2][:, :], ALU.mult, ALU.add)
                    S.tt(imp[k2][:, :], imp[k2][:, :], sadd[k2][:, :], ALU.add)
                    S.max8(m8a[k2][:, :], imp[k2][:, :])
                    S.match_replace(imp3[k2][:, :], m8a[k2][:, :], imp[k2][:, :], -3.0e38)
                    S.max8(m8b[k2][:, :], imp3[k2][:, :])
                    S.ts(sbias[k2][:, :], imp[k2][:, :], m8b[k2][:, 7:8], NEGB, ALU.is_lt, ALU.mult)
                    tp = scb[cnt["u"] % 3]
                    cnt["u"] += 1
                    S.matmul(tp[0:64, 0:128], lhsT=sbias[k2][:, :], rhs=ident[:, :], start=True, stop=True, skip=True)
                    for r in range(4):
                        S.copy(biasT4[k2][:, r * 128:(r + 1) * 128], tp[0:64, 0:128], eng="scalar" if r % 2 == 0 else "vector")
                    self.run_units([mk("s", kt) for kt in range(qt + 1)], fill=fill, nfill=NFILL)
                    s3 = accS[:, 0:260].rearrange("p (r e) -> p r e", e=65)
                    w3 = accW[:, 0:260].rearrange("p (r e) -> p r e", e=65)
                    S.ts(sums[k2][:, :, 1:2], s3[:, :, 64:65], 1e-30, None, ALU.max)
                    S.ts(sums[k2][:, :, 2:3], w3[:, :, 64:65], 1e-30, None, ALU.max)
                    S.recip(rcp[k2][:, :, 1:3], sums[k2][:, :, 1:3])
                    S.tt(coef[k2][:, :, :], gts[k2][:, :, :], rcp[k2][:, :, :], ALU.mult)
                    for r in range(4):
                        o0 = (r % 2) * 129
                        o = oo[r % 2]
                        S.ts(o[:, :], accC[r // 2][:, o0:o0 + 64], coef[k2][:, r, 0:1], None, ALU.mult)
                        S.stt(o[:, :], accS[:, r * 65:r * 65 + 64], coef[k2][:, r, 1:2], o[:, :], ALU.mult, ALU.add)
                        S.stt(mo[k2][:, r * 64:(r + 1) * 64], accW[:, r * 65:r * 65 + 64], coef[k2][:, r, 2:3], o[:, :],
                              ALU.mult, ALU.add)
                    S.dma(mix_d.at(("d", g, qt))[qs, 512 + g * 256:512 + (g + 1) * 256], mo[k2][:, :])
            S.flush()

    def phase_moe(self, L):
        S, d = self.S, self.d
        i = L // 2
        NF = F_EXPERT // 128
        final = L == DEPTH - 1
        dst = d["out"] if final else d["xres"]
        with ExitStack() as ps:
            r = self.ln_alloc(ps, L, "ffn", nbuf=1)
            xTc = [self.sb(ps, f"xTc{k}", [128, 8, 512], BF16) for k in range(2)]
            Gc = [self.sb(ps, f"Gc{k}", [128, 4, 8], F32) for k in range(2)]
            Wg = [self.sb(ps, f"Wg{k}", [128, 8, 256], BF16) for k in range(2)]
            Wu = [self.sb(ps, f"Wu{k}", [128, 8, 256], BF16) for k in range(2)]
            Wdh = [self.sb(ps, f"Wdh{k}", [128, NF, 512], BF16) for k in range(2)]
            hT = self.sb(ps, "hT", [128, NF, 512], BF16)
            sg = [self.sb(ps, f"sg{k}", [128, 512], F32) for k in range(2)]
            acc = self.sb(ps, "acc", [128, 4, D], F32)
            bk = self.banks(ps, 8)
            g_all = d[f"b_mg{i}"]
            u_all = d[f"b_mu{i}"]
            d_all = d[f"b_md{i}"]
            xT_v = d["xT"][:, :].rearrange("(c p) t -> p c t", p=128)
            u = 0
            w = 0
            wd = 0
            for tc in range(8):
                xc = xTc[tc % 2]
                gc = Gc[tc % 2]
                S.dma(xc[:, :, :], View(d["xT"].at(("c", tc)), xT_v.ap[:, :, tc * 512:(tc + 1) * 512]))
                S.dma(gc[:, :, :], d["moeg"][tc * 512:(tc + 1) * 512, :].rearrange("(t p) e -> p t e", p=128))
                for e in range(N_EXPERTS):
                    g_v = g_all[e * 1024:(e + 1) * 1024, :].rearrange("(c p) n -> p c n", p=128)
                    u_v = u_all[e * 1024:(e + 1) * 1024, :].rearrange("(c p) n -> p c n", p=128)
                    d_v = d_all[e * F_EXPERT:(e + 1) * F_EXPERT, :].rearrange("(f p) n -> p f n", p=128)
                    for fg in range(NF // 2):
                        wg, wu = Wg[w % 2], Wu[w % 2]
                        w += 1
                        S.dma(wg[:, :, :], g_v[:, :, fg * 256:(fg + 1) * 256])
                        S.dma(wu[:, :, :], u_v[:, :, fg * 256:(fg + 1) * 256])
                        for f2 in range(2):
                            ft = fg * 2 + f2
                            pg, pu = bk[(2 * u) % 4], bk[(2 * u + 1) % 4]
                            for c in range(8):
                                S.matmul(pg[:, :], lhsT=wg[:, c, f2 * 128:(f2 + 1) * 128], rhs=xc[:, c, :], start=(c == 0), stop=(c == 7))
                            for c in range(8):
                                S.matmul(pu[:, :], lhsT=wu[:, c, f2 * 128:(f2 + 1) * 128], rhs=xc[:, c, :], start=(c == 0), stop=(c == 7))
                            S.act(sg[u % 2][:, :], pg[:, :], AF.Silu)
                            S.tt(View(hT.at(ft), hT.t[:, ft, :]), sg[u % 2][:, :], pu[:, :], ALU.mult)
                            u += 1
                    for hh in range(2):
                        wdh = Wdh[wd % 2]
                        wd += 1
                        for f0 in range(0, NF, 7):
                            S.dma(View(wdh.at(f0), wdh.t[:, f0:f0 + 7, :]), d_v[:, f0:f0 + 7, hh * 512:(hh + 1) * 512])
                        for t4 in range(4):
                            py = bk[4 + (t4 % 2)]
                            for ft in range(NF):
                                S.matmul(py[:, :], lhsT=View(hT.at(ft), hT.t[:, ft, t4 * 128:(t4 + 1) * 128]),
                                         rhs=View(wdh.at((ft // 7) * 7), wdh.t[:, ft, :]), start=(ft == 0), stop=(ft == NF - 1))
                            av = View(acc.at((t4, hh)), acc.t[:, t4, hh * 512:(hh + 1) * 512])
                            if e == 0:
                                S.ts(av, py[:, :], gc[:, t4, e:e + 1], None, ALU.mult)
                            else:
                                S.stt(av, py[:, :], gc[:, t4, e:e + 1], av, ALU.mult, ALU.add)
                for t4 in range(4):
                    t = tc * 4 + t4
                    yv = [View(acc.at((t4, hh)), acc.t[:, t4, hh * 512:(hh + 1) * 512]) for hh in range(2)]
                    self.ln_tile(r, t, yv, d["xres"], dst, bk[6:8], want_xT=not final)
            S.flush()

    def phase_moe_routed(self, L):
        S, d = self.S, self.d
        i = L // 2
        NF = F_EXPERT // 128
        CAP = MOE_CAP
        NCH = CAP // 512
        ROWS = 8 * CAP
        U32 = mybir.dt.uint32
        final = L == DEPTH - 1
        dst = d["out"] if final else d["xres"]
        xs_d, ys_d = d["xs"], d["ys"]
        with ExitStack() as ps:
            r = self.ln_alloc(ps, L, "ffn", nbuf=1)
            ident = self.const(ps, "c_ident", [128, 128], BF16)
            tri = self.const(ps, "c_tri", [128, 128], BF16)
            ones = self.const(ps, "c_ones", [128, 128], BF16)
            ebase = self.const(ps, "c_ebase", [128, 32, 8], F32)
            bk = self.banks(ps, 8)
            m1 = self.sb(ps, "m1", [128, 32, 8], F32)
            m2 = self.sb(ps, "m2", [128, 32, 8], F32)
            gv = self.sb(ps, "gv", [128, 32, 2], F32)
            S.dma(m1[:, :, :], d["moem1"][:, :].rearrange("(t p) e -> p t e", p=128))
            S.dma(m2[:, :, :], d["moem2"][:, :].rearrange("(t p) e -> p t e", p=128))
            S.dma(gv[:, :, :], d["moegv"][:, :].rearrange("(t p) e -> p t e", p=128))
            selb = self.sb(ps, "selb", [128, 256], BF16)
            S.tt(selb[:, :].rearrange("p (t e) -> p t e", e=8), m1[:, :, :], m2[:, :, :], ALU.add)
            S.matmul(bk[0][:, 0:256], lhsT=tri[:, :], rhs=selb[:, :], start=True, stop=True)
            S.matmul(bk[1][:, 0:256], lhsT=ones[:, :], rhs=selb[:, :], start=True, stop=True)
            tot = self.sb(ps, "tot", [128, 32, 8], F32)
            S.copy(tot[:, :, :], bk[1][:, 0:256].rearrange("p (t e) -> p t e", e=8))
            off = self.sb(ps, "off", [128, 32, 8], F32)
            S.memset(off[:, 0, :], 0.0)
            for t in range(1, 32):
                S.tt(off[:, t, :], off[:, t - 1, :], tot[:, t - 1, :], ALU.add)
            pos = self.sb(ps, "pos", [128, 32, 8], F32)
            S.tt(pos[:, :, :], off[:, :, :], bk[0][:, 0:256].rearrange("p (t e) -> p t e", e=8), ALU.add)
            slot = self.sb(ps, "slot", [128, 32, 8], F32)
            S.tt(slot[:, :, :], pos[:, :, :], ebase[:, :, :], ALU.add)
            S.ts(pos[:, :, :], pos[:, :, :], float(CAP), 1.0e6, ALU.is_ge, ALU.mult)
            S.tt(slot[:, :, :], slot[:, :, :], pos[:, :, :], ALU.add)
            sl_f = self.sb(ps, "sl_f", [128, 2, 32], F32)
            tmp = self.sb(ps, "rtmp", [128, 32, 8], F32)
            S.tt(tmp[:, :, :], slot[:, :, :], m1[:, :, :], ALU.mult)
            S.reduce(sl_f[:, 0, :], tmp[:, :, :], ALU.add)
            S.tt(tmp[:, :, :], slot[:, :, :], m2[:, :, :], ALU.mult)
            S.reduce(sl_f[:, 1, :], tmp[:, :, :], ALU.add)
            sl_i = self.sb(ps, "sl_i", [128, 2, 32], U32)
            S.copy(sl_i[:, :, :], sl_f[:, :, :])
            okf = self.sb(ps, "okf", [128, 2, 32], F32)
            S.ts(okf[:, :, :], sl_f[:, :, :], 1.0e5, None, ALU.is_lt)
            geff = self.sb(ps, "geff", [128, 2, 32], F32)
            S.tt(geff[:, 0, :], gv[:, :, 0], okf[:, 0, :], ALU.mult)
            S.tt(geff[:, 1, :], gv[:, :, 1], okf[:, 1, :], ALU.mult)
            zt = self.sb(ps, "zt", [128, D], BF16)
            S.memset(zt[:, :], 0.0)
            for r0 in range(0, ROWS, 128):
                S.dma(xs_d.at(("z", r0))[r0:r0 + 128, :], zt[:, :])
            S.flush()
            KB._uid += 1
            breg = ps.enter_context(self.nc.gpsimd.register(f"moe_bound{KB._uid}"))
            S.op("gpsimd", lambda e: e.reg_mov(breg, ROWS - 1))
            xf = [self.sb(ps, f"xf{k}", [128, D], F32) for k in range(2)]
            xb = [self.sb(ps, f"xb{k}", [128, D], BF16) for k in range(2)]
            for t in range(NT):
                k = t % 2
                tk = slice(t * 128, (t + 1) * 128)
                S.dma(xf[k][:, :], View(d["xres"].at(t), d["xres"].t[tk, :]))
                S.copy(xb[k][:, :], xf[k][:, :], eng="scalar")
                for j in range(2):
                    idx_ap = sl_i.t[:, j, t:t + 1]
                    o_ap, i_ap = xs_d.t[:, :], xb[k].t[:, :]
                    S.dma_op("gpsimd",
                             lambda e, o_ap=o_ap, i_ap=i_ap, idx_ap=idx_ap: e.indirect_dma_start(
                                 out=o_ap, out_offset=bass.IndirectOffsetOnAxis(ap=idx_ap, axis=0), in_=i_ap, in_offset=None,
                                 bounds_check=breg, oob_is_err=False),
                             reads=[xb[k], sl_i], writes=[xs_d.at(("sc", t, j))])
            S.flush()
            xtm = self.sb(ps, "xtm", [128, 4, D], BF16)
            xTc = self.sb(ps, "xTc", [128, 8, 512], BF16)
            Wg = [self.sb(ps, f"Wg{k}", [128, 8, 256], BF16) for k in range(2)]
            Wu = [self.sb(ps, f"Wu{k}", [128, 8, 256], BF16) for k in range(2)]
            Wdh = [self.sb(ps, f"Wdh{k}", [128, NF, 512], BF16) for k in range(2)]
            hT = self.sb(ps, "hT", [128, NF, 512], BF16)
            sg = [self.sb(ps, f"sg{k}", [128, 512], F32) for k in range(2)]
            yo = [self.sb(ps, f"yo{k}", [128, 512], F32) for k in range(2)]
            g_all, u_all, d_all = d[f"b_mg{i}"], d[f"b_mu{i}"], d[f"b_md{i}"]
            u = 0
            w = 0
            wd = 0
            yy = 0
            for e in range(N_EXPERTS):
                g_v = g_all[e * 1024:(e + 1) * 1024, :].rearrange("(c p) n -> p c n", p=128)
                u_v = u_all[e * 1024:(e + 1) * 1024, :].rearrange("(c p) n -> p c n", p=128)
                d_v = d_all[e * F_EXPERT:(e + 1) * F_EXPERT, :].rearrange("(f p) n -> p f n", p=128)
                for c in range(NCH):
                    r0 = e * CAP + c * 512
                    S.dma(xtm[:, :, :], View(xs_d.at(("ld", r0)), xs_d.t[r0:r0 + 512, :].rearrange("(s p) n -> p s n", p=128)))
                    for s4 in range(4):
                        for hb in range(2):
                            tb = bk[6 + hb]
                            for cc in range(4):
                                dc = hb * 4 + cc
                                S.matmul(tb[:, cc * 128:(cc + 1) * 128], lhsT=xtm[:, s4, dc * 128:(dc + 1) * 128], rhs=ident[:, :],
                                         start=True, stop=True, skip=True)
                            S.copy(xTc[:, hb * 4:(hb + 1) * 4, s4 * 128:(s4 + 1) * 128],
                                   tb[:, :].rearrange("p (c t) -> p c t", c=4), eng="scalar" if hb == 0 else "vector")
                    for fg in range(NF // 2):
                        wg, wu = Wg[w % 2], Wu[w % 2]
                        w += 1
                        S.dma(wg[:, :, :], g_v[:, :, fg * 256:(fg + 1) * 256])
                        S.dma(wu[:, :, :], u_v[:, :, fg * 256:(fg + 1) * 256])
                        for f2 in range(2):
                            ft = fg * 2 + f2
                            pg, pu = bk[(2 * u) % 4], bk[(2 * u + 1) % 4]
                            for cI in range(8):
                                S.matmul(pg[:, :], lhsT=wg[:, cI, f2 * 128:(f2 + 1) * 128], rhs=xTc[:, cI, :], start=(cI == 0), stop=(cI == 7))
                            for cI in range(8):
                                S.matmul(pu[:, :], lhsT=wu[:, cI, f2 * 128:(f2 + 1) * 128], rhs=xTc[:, cI, :], start=(cI == 0), stop=(cI == 7))
                            S.act(sg[u % 2][:, :], pg[:, :], AF.Silu)
                            S.tt(View(hT.at(ft), hT.t[:, ft, :]), sg[u % 2][:, :], pu[:, :], ALU.mult)
                            u += 1
                    for hh in range(2):
                        wdh = Wdh[wd % 2]
                        wd += 1
                        for f0 in range(0, NF, 7):
                            S.dma(View(wdh.at(f0), wdh.t[:, f0:f0 + 7, :]), d_v[:, f0:f0 + 7, hh * 512:(hh + 1) * 512])
                        for t4 in range(4):
                            py = bk[4 + (t4 % 2)]
                            for ft in range(NF):
                                S.matmul(py[:, :], lhsT=View(hT.at(ft), hT.t[:, ft, t4 * 128:(t4 + 1) * 128]),
                                         rhs=View(wdh.at((ft // 7) * 7), wdh.t[:, ft, :]), start=(ft == 0), stop=(ft == NF - 1))
                            y = yo[yy % 2]
                            yy += 1
                            S.copy(y[:, :], py[:, :], eng="scalar" if t4 % 2 == 0 else "vector")
                            rr0 = r0 + t4 * 128
                            S.dma(View(ys_d.at((rr0, hh)), ys_d.t[rr0:rr0 + 128, hh * 512:(hh + 1) * 512]), y[:, :])
            S.flush()
            y1 = self.sb(ps, "y1", [128, D], F32)
            y2 = self.sb(ps, "y2", [128, D], F32)
            acc = self.sb(ps, "acc", [128, D], F32)
            S.memset(y1[:, :], 0.0)
            S.memset(y2[:, :], 0.0)
            S.op("gpsimd", lambda e: e.reg_mov(breg, ROWS - 1))
            for t in range(NT):
                for j, yb in enumerate((y1, y2)):
                    idx_ap = sl_i.t[:, j, t:t + 1]
                    o_ap, i_ap = yb.t[:, :], ys_d.t[:, :]
                    S.dma_op("gpsimd",
                             lambda e, o_ap=o_ap, i_ap=i_ap, idx_ap=idx_ap: e.indirect_dma_start(
                                 out=o_ap, out_offset=None, in_=i_ap, in_offset=bass.IndirectOffsetOnAxis(ap=idx_ap, axis=0),
                                 bounds_check=breg, oob_is_err=False),
                             reads=[sl_i], writes=[yb])
                S.ts(acc[:, :], y1[:, :], geff[:, 0, t:t + 1], None, ALU.mult)
                S.stt(acc[:, :], y2[:, :], geff[:, 1, t:t + 1], acc[:, :], ALU.mult, ALU.add)
                self.ln_tile(r, t, [acc[:, 0:512], acc[:, 512:1024]], d["xres"], dst, bk[6:8], want_xT=not final)
            S.flush()


CONST_SPECS = dict(
    c_cos=([128, SEQ], F32), c_sin=([128, SEQ], F32), c_ident=([128, 128], BF16), c_ident4=([128, 512], BF16),
    c_identf=([128, 128], F32), c_causal=([128, 128], BF16), c_causalf=([128, 128], F32), c_winfar=([128, 128], BF16),
    c_e16=([16, 16, 128], BF16), c_e64=([64, 32, 128], BF16), c_cmpb=([32, 128, 256], BF16),
    c_seladd=([32, 128, 64], F32), c_c2s=([256, 64], BF16), c_pow2=([128, 24], F32),
    c_tri=([128, 128], BF16), c_ones=([128, 128], BF16), c_ebase=([128, 32, 8], F32),
)

PARAM_SPECS = dict(
    ev_w_out=[2, 1024, 1024], dif_lambda=[2, 4, 64], dif_subln=[2, 128],
    ffd_w_gate=[2, 1024, F_DENSE], ffd_w_up=[2, 1024, F_DENSE], ffd_w_down=[2, F_DENSE, 1024],
    od_w_out=[2, 1024, 1024], nsa_gate_b=[2, 24], nsa_phi_w1=[2, 2, 2048, 256], nsa_phi_w2=[2, 2, 256, 64],
    moe_w_router=[2, 1024, 8], moe_b_router=[2, 8],
    moe_w_gate=[2, 8, 1024, F_EXPERT], moe_w_up=[2, 8, 1024, F_EXPERT], moe_w_down=[2, 8, F_EXPERT, 1024],
    ln_mix_g=[4, 1024], ln_mix_b=[4, 1024], ln_ffn_g=[4, 1024], ln_ffn_b=[4, 1024],
)
LAYOUT_SPECS = {}
for _i in range(2):
    LAYOUT_SPECS[f"ev_fm{_i}"] = [1024, EV_FM]
    LAYOUT_SPECS[f"ev_fmp{_i}"] = [1024, EV_FM]
    LAYOUT_SPECS[f"ev_tm{_i}"] = [1024, EV_TM]
    LAYOUT_SPECS[f"od_fm{_i}"] = [1024, OD_FM]
    LAYOUT_SPECS[f"od_fmp{_i}"] = [1024, OD_FMR]
    LAYOUT_SPECS[f"od_tm{_i}"] = [1024, OD_TM]
    LAYOUT_SPECS[f"nsa_peT{_i}"] = [2, 64, 32]


def build_program(dbg=(), phases=None):
    nc = bass.Bass("TRN2", target_bir_lowering=False)
    st = ExitStack()
    kb = KB(nc, st, dbg)
    d = kb.d
    kb.din("x", [SEQ, D], F32)
    for k, (shape, dt) in CONST_SPECS.items():
        kb.din(k, shape, dt)
    for k, shape in PARAM_SPECS.items():
        kb.din(k, shape, F32)
    for k, shape in LAYOUT_SPECS.items():
        kb.din(k, shape, F32)
    kb.dscr("out", [SEQ, D], F32, out=True)
    kb.dscr("xres", [SEQ, D], F32)
    kb.dscr("xT", [D, SEQ], BF16)
    kb.dscr("qk", [2048, SEQ], BF16)
    kb.dscr("qraw", [512, SEQ], BF16)
    kb.dscr("vt", [SEQ, 768], BF16)
    kb.dscr("iw", [SEQ, 4], F32)
    kb.dscr("gates", [SEQ, 24], F32)
    kb.dscr("kmean", [512, 16], BF16)
    kb.dscr("mix", [SEQ, D], BF16)
    kb.dscr("moeg", [SEQ, 8], F32)
    kb.dscr("moem1", [SEQ, 8], F32)
    kb.dscr("moem2", [SEQ, 8], F32)
    kb.dscr("moegv", [SEQ, 2], F32)
    kb.dscr("xs", [8 * MOE_CAP, D], BF16)
    kb.dscr("ys", [8 * MOE_CAP, D], F32)
    if "dbg_lo" in kb.dbg:
        kb.dscr("dbg_lo", [SEQ, 4], F32)
    kb.dscr("kcmpT", [2, 64, 256], BF16)
    kb.dscr("vcmp", [2, 256, 64], BF16)
    for i in range(2):
        for k in ("ev_fm", "ev_fmp", "ev_tm", "od_fm", "od_fmp", "od_tm"):
            kb.dscr(f"b_{k}{i}", LAYOUT_SPECS[f"{k}{i}"], BF16)
        kb.dscr(f"b_ev_wout{i}", [1024, 1024], BF16)
        kb.dscr(f"b_od_wout{i}", [1024, 1024], BF16)
        kb.dscr(f"b_ffg{i}", [1024, F_DENSE], BF16)
        kb.dscr(f"b_ffu{i}", [1024, F_DENSE], BF16)
        kb.dscr(f"b_ffd{i}", [F_DENSE, 1024], BF16)
        kb.dscr(f"b_phi1_{i}", [2 * 2048, 256], BF16)
        kb.dscr(f"b_phi2_{i}", [2 * 256, 64], BF16)
        kb.dscr(f"b_mg{i}", [8 * 1024, F_EXPERT], BF16)
        kb.dscr(f"b_mu{i}", [8 * 1024, F_EXPERT], BF16)
        kb.dscr(f"b_md{i}", [8 * F_EXPERT, 1024], BF16)

    def want(name):
        return phases is None or name in phases

    S = kb.S
    for L in range(DEPTH):
        i = L // 2
        if L % 2 == 0:
            for k, cols in (("ev_fm", EV_FM), ("ev_fmp", EV_FM), ("ev_tm", EV_TM)):
                kb.conv_add(f"b_{k}{i}", d[f"{k}{i}"][:, :], 1024, cols)
            kb.conv_add(f"b_ev_wout{i}", d["ev_w_out"][i], 1024, 1024)
            kb.conv_add(f"b_ffg{i}", d["ffd_w_gate"][i], 1024, F_DENSE)
            kb.conv_add(f"b_ffu{i}", d["ffd_w_up"][i], 1024, F_DENSE)
            kb.conv_add(f"b_ffd{i}", d["ffd_w_down"][i], F_DENSE, 1024)
        else:
            for k, cols in (("od_fm", OD_FM), ("od_fmp", OD_FMR), ("od_tm", OD_TM)):
                kb.conv_add(f"b_{k}{i}", d[f"{k}{i}"][:, :], 1024, cols)
            kb.conv_add(f"b_phi1_{i}", d["nsa_phi_w1"][i].rearrange("a r c -> (a r) c"), 4096, 256)
            kb.conv_add(f"b_phi2_{i}", d["nsa_phi_w2"][i].rearrange("a r c -> (a r) c"), 512, 64)
            kb.conv_add(f"b_od_wout{i}", d["od_w_out"][i], 1024, 1024)
            kb.conv_add(f"b_mg{i}", d["moe_w_gate"][i].rearrange("e r c -> (e r) c"), 8 * 1024, F_EXPERT)
            kb.conv_add(f"b_mu{i}", d["moe_w_up"][i].rearrange("e r c -> (e r) c"), 8 * 1024, F_EXPERT)
            kb.conv_add(f"b_md{i}", d["moe_w_down"][i].rearrange("e r c -> (e r) c"), 8 * F_EXPERT, 1024)
    if phases is not None:
        kb.conv_budget(1 << 60)
        S.flush()
    MB = 1 << 20
    NEED = {
        "proj_e": lambda i: [f"b_ev_fm{i}", f"b_ev_fmp{i}", f"b_ev_tm{i}"],
        "proj_o": lambda i: [f"b_od_fm{i}", f"b_od_fmp{i}", f"b_od_tm{i}"],
    }
    def run_phase(name, fn, budget_mb, needs=()):
        if not want(name):
            return
        kb.conv_ensure(needs)
        kb.conv_budget(budget_mb * MB)
        fn()

    if phases is None:
        kb.conv_ensure(NEED["proj_e"](0))
    run_phase("prologue", lambda: kb.phase_prologue(d["x"], d["xT"]), 120)
    for L in range(DEPTH):
        i = L // 2
        if L % 2 == 0:
            run_phase(f"proj{L}", lambda: kb.phase_proj(L), 0, NEED["proj_e"](i))
            run_phase(f"diff{L}", lambda: kb.phase_diff(L), 0)
            run_phase(f"dsa{L}", lambda: kb.phase_dsa(L), 450)
            run_phase(f"outproj{L}", lambda: kb.phase_outproj(L), 0, [f"b_ev_wout{i}"])
            run_phase(f"ffn{L}", lambda: kb.phase_ffn_dense(L), 0, [f"b_ffg{i}", f"b_ffu{i}", f"b_ffd{i}"])
        else:
            run_phase(f"proj{L}", lambda: kb.phase_proj(L), 0, NEED["proj_o"](i))
            run_phase(f"cmp{L}", lambda: kb.phase_cmp(L), 0, [f"b_phi1_{i}", f"b_phi2_{i}"])
            run_phase(f"moba{L}", lambda: kb.phase_moba(L), 0)
            run_phase(f"nsa{L}", lambda: kb.phase_nsa(L), 0)
            run_phase(f"outproj{L}", lambda: kb.phase_outproj(L), 0, [f"b_od_wout{i}"])
            moe_fn = (lambda: kb.phase_moe_routed(L)) if MOE_ROUTED else (lambda: kb.phase_moe(L))
            run_phase(f"moe{L}", moe_fn, 0, [f"b_mg{i}", f"b_mu{i}", f"b_md{i}"])
    st.close()
    return nc, kb


_PROGRAM = None


def kernel(**inputs):
    global _PROGRAM
    inp = {k: np.asarray(v) for k, v in inputs.items()}
    B = inp["x"].shape[0]
    assert inp["x"].shape == (8, SEQ, D)
    if _PROGRAM is None:
        _PROGRAM = build_program()[0]
    nc = _PROGRAM
    shared = {}
    shared.update(make_constants())
    for k, shape in PARAM_SPECS.items():
        shared[k] = np.ascontiguousarray(inp[k], dtype=np.float32).reshape(shape)
    shared.update(layout_weights(inp))
    in_maps = []
    for b in range(B):
        m = dict(shared)
        m["x"] = np.ascontiguousarray(inp["x"][b], dtype=np.float32)
        in_maps.append(m)
    res = run_bass_kernel_spmd(nc, in_maps, core_ids=list(range(B)))
    out = np.stack([np.asarray(r["out"], dtype=np.float32) for r in res.results], axis=0)
    return out
```

```python
import math
from contextlib import ExitStack

import numpy as np
import ml_dtypes

import concourse.bass as bass
import concourse.mybir as mybir
from concourse.bass_utils import run_bass_kernel_spmd

F32 = mybir.dt.float32
BF16 = mybir.dt.bfloat16
ALU = mybir.AluOpType
AF = mybir.ActivationFunctionType
AX = mybir.AxisListType

D = 1024
SEQ = 4096
DEPTH = 4
NT = SEQ // 128
HD = 64
LN_EPS = 1e-5
DN_ALPHA = (2 * DEPTH) ** 0.25
F_DENSE = 2816
N_EXPERTS = 8
F_EXPERT = 3584
MOE_CAP = 1536
NEGB = -30000.0
SCALE = HD ** -0.5

EV_SIZES = dict(aq=512, ak=512, av=512, bq=512, bk=128, bv=128, iq=256, ik=64, iw=4)
OD_SIZES = dict(cq=512, ck=512, cv=512, dq=512, dkc=128, dvc=128, dks=128, dvs=128, dkw=128, dvw=128, dg=24)


def _offsets(sizes):
    off, o = {}, 0
    for k, v in sizes.items():
        off[k] = o
        o += v
    return off


EV_OFF = _offsets(EV_SIZES)
OD_OFF = _offsets(OD_SIZES)

ENGS = ("tensor", "vector", "scalar", "gpsimd", "sync")
EPOCH = 60000
NDMASEM = 40
NSWSEM = 12
RELAX_SAME_ENGINE = False
NPROG = 10


class View:
    __slots__ = ("buf", "ap")

    def __init__(self, buf, ap):
        self.buf = buf
        self.ap = ap

    def __getitem__(self, idx):
        return View(self.buf, self.ap[idx])

    def rearrange(self, s, **kw):
        return View(self.buf, self.ap.rearrange(s, **kw))

    def bitcast(self, dt):
        return View(self.buf, self.ap.bitcast(dt))

    def broadcast_to(self, shape):
        return View(self.buf, self.ap.broadcast_to(shape))

    def partition_broadcast(self, n):
        return View(self.buf, self.ap.partition_broadcast(n))


class Buf:
    def __init__(self, t, name=""):
        self.t = t
        self.w = None
        self.r = []
        self.name = name
        self.kids = {}
        self.psum = False

    def __getitem__(self, idx):
        return View(self, self.t[idx])

    def at(self, key):
        k = self.kids.get(key)
        if k is None:
            k = Buf(self.t, f"{self.name}.{key}")
            self.kids[key] = k
        return k


class Op:
    __slots__ = ("eng", "fn", "deps", "signal", "is_dma", "dsem", "dval", "sigval", "sigsem", "gen")

    def __init__(self, eng, fn, is_dma=False):
        self.eng = eng
        self.fn = fn
        self.deps = []
        self.signal = False
        self.is_dma = is_dma
        self.dsem = None
        self.dval = 0
        self.sigval = 0
        self.sigsem = None
        self.gen = 0


class Sched:
    def __init__(self, nc, st, same_engine_sync=True):
        self.nc = nc
        self.same_engine_sync = same_engine_sync
        self.ops = {e: [] for e in ENGS}
        self.dma_rr = 0
        self.dma_last = [None] * NDMASEM
        self.dma_cnt = [0] * NDMASEM
        self.sig_cnt = {e: 0 for e in ENGS}
        self.psems = {e: [st.enter_context(nc.semaphore(f"p_{e}_{k}")) for k in range(NPROG)] for e in ENGS}
        self.dsems = [st.enter_context(nc.semaphore(f"d_{k}")) for k in range(NDMASEM)]
        self.final_ops = []
        self.n_ops = 0
        self.gen = 0

    def _track(self, op, reads, writes):
        deps = []
        for b in reads:
            if b.w is not None:
                deps.append((b.w, True))
            if b.psum:
                deps.extend((r, False) for r in b.r if r.eng != op.eng)
        for b in writes:
            if b.w is not None:
                deps.append((b.w, False))
            deps.extend((r, False) for r in b.r)
        seen = {}
        for d, raw in deps:
            if d is op:
                continue
            if RELAX_SAME_ENGINE and (not raw) and (not d.is_dma) and (not op.is_dma) and d.eng == op.eng:
                continue
            if id(d) in seen:
                continue
            seen[id(d)] = True
            op.deps.append(d)
        for b in reads:
            b.r.append(op)
        for b in writes:
            b.w = op
            b.r = []

    def op(self, eng, fn, reads=(), writes=()):
        o = Op(eng, fn)
        o.gen = self.gen
        self._track(o, reads, writes)
        self.ops[eng].append(o)
        self.n_ops += 1
        return o

    def dma_op(self, eng, fn, reads=(), writes=()):
        o = Op(eng, fn, is_dma=True)
        o.gen = self.gen
        self._track(o, reads, writes)
        if eng == "gpsimd":
            self.dma_rr_sw = (getattr(self, "dma_rr_sw", -1) + 1) % NSWSEM
            k = self.dma_rr_sw
        else:
            self.dma_rr = (self.dma_rr + 1) % (NDMASEM - NSWSEM)
            k = NSWSEM + self.dma_rr
        prev = self.dma_last[k]
        if prev is not None:
            o.deps.append(prev)
        self.dma_cnt[k] += 1
        o.dsem = k
        o.dval = 16 * self.dma_cnt[k]
        self.dma_last[k] = o
        self.ops[eng].append(o)
        self.n_ops += 1
        return o

    @staticmethod
    def _bufs(*views):
        out = []
        for v in views:
            if isinstance(v, View) and v.buf not in out:
                out.append(v.buf)
        return out

    @staticmethod
    def _ap(v):
        return v.ap if isinstance(v, View) else v

    def dma(self, out, in_, eng="sync", final=False, **kw):
        o_ap, i_ap = out.ap, in_.ap
        o = self.dma_op(eng, lambda e: e.dma_start(out=o_ap, in_=i_ap, **kw), reads=[in_.buf], writes=[out.buf])
        if final:
            self.final_ops.append(o)
        return o

    def dma_T(self, out, in_, eng="sync"):
        o_ap, i_ap = out.ap, in_.ap
        return self.dma_op(eng, lambda e: e.dma_start_transpose(out=o_ap, in_=i_ap), reads=[in_.buf], writes=[out.buf])

    def matmul(self, out, lhsT, rhs, start=True, stop=True, skip=False):
        o_ap, l_ap, r_ap = out.ap, lhsT.ap, rhs.ap
        return self.op("tensor",
                       lambda e: e.matmul(o_ap, l_ap, r_ap, start=start, stop=stop, skip_group_check=skip),
                       reads=self._bufs(lhsT, rhs), writes=[out.buf])

    def act(self, out, in_, func, bias=None, scale=1.0, accum_out=None, eng="scalar"):
        o_ap, i_ap = out.ap, in_.ap
        kw = {}
        if bias is not None:
            kw["bias"] = self._ap(bias)
        if accum_out is not None:
            kw["accum_out"] = accum_out.ap
        sc = self._ap(scale)
        writes = self._bufs(out, accum_out)
        return self.op("scalar", lambda e: e.activation(o_ap, i_ap, func, scale=sc, **kw),
                       reads=self._bufs(in_, bias, scale, accum_out), writes=writes)

    def tt(self, out, in0, in1, op, eng="vector"):
        o_ap, a_ap, b_ap = out.ap, in0.ap, in1.ap
        return self.op(eng, lambda e: e.tensor_tensor(o_ap, a_ap, b_ap, op),
                       reads=self._bufs(in0, in1), writes=[out.buf])

    def ts(self, out, in0, s1, s2, op0, op1=None, accum_out=None, eng="vector"):
        o_ap, a_ap = out.ap, in0.ap
        s1a, s2a = self._ap(s1), self._ap(s2)
        kw = {}
        if op1 is not None:
            kw["op1"] = op1
        if accum_out is not None:
            kw["accum_out"] = accum_out.ap
        return self.op(eng, lambda e: e.tensor_scalar(o_ap, a_ap, s1a, s2a, op0, **kw),
                       reads=self._bufs(in0, s1, s2, accum_out), writes=self._bufs(out, accum_out))

    def stt(self, out, in0, scalar, in1, op0, op1, eng="vector"):
        o_ap, a_ap, b_ap = out.ap, in0.ap, in1.ap
        sa = self._ap(scalar)
        return self.op(eng, lambda e: e.scalar_tensor_tensor(o_ap, a_ap, sa, b_ap, op0, op1),
                       reads=self._bufs(in0, scalar, in1), writes=[out.buf])

    def copy(self, out, in_, eng="vector"):
        o_ap, i_ap = out.ap, in_.ap
        if eng == "scalar":
            return self.op("scalar", lambda e: e.copy(o_ap, i_ap), reads=[in_.buf], writes=[out.buf])
        return self.op(eng, lambda e: e.tensor_copy(o_ap, i_ap), reads=[in_.buf], writes=[out.buf])

    def memset(self, out, val, eng="vector"):
        o_ap = out.ap
        return self.op(eng, lambda e: e.memset(o_ap, val), writes=[out.buf])

    def reduce(self, out, in_, op, axis=AX.X, eng="vector"):
        o_ap, i_ap = out.ap, in_.ap
        return self.op(eng, lambda e: e.tensor_reduce(o_ap, i_ap, axis, op), reads=[in_.buf], writes=[out.buf])

    def max8(self, out, in_):
        o_ap, i_ap = out.ap, in_.ap
        return self.op("vector", lambda e: e.max(o_ap, i_ap), reads=[in_.buf], writes=[out.buf])

    def match_replace(self, out, to_replace, in_values, imm):
        o_ap, t_ap, i_ap = out.ap, to_replace.ap, in_values.ap
        return self.op("vector", lambda e: e.match_replace(o_ap, t_ap, i_ap, imm),
                       reads=self._bufs(to_replace, in_values), writes=[out.buf])

    def recip(self, out, in_):
        o_ap, i_ap = out.ap, in_.ap
        return self.op("vector", lambda e: e.reciprocal(o_ap, i_ap), reads=[in_.buf], writes=[out.buf])

    def bn_stats(self, out, in_):
        o_ap, i_ap = out.ap, in_.ap
        return self.op("vector", lambda e: e.bn_stats(o_ap, i_ap), reads=[in_.buf], writes=[out.buf])

    def bn_aggr(self, out, in_):
        o_ap, i_ap = out.ap, in_.ap
        return self.op("vector", lambda e: e.bn_aggr(o_ap, i_ap), reads=[in_.buf], writes=[out.buf])

    def flush(self):
        nc = self.nc
        same = self.same_engine_sync
        gen = self.gen
        for e in ENGS:
            for o in self.ops[e]:
                o.deps = [d for d in o.deps if d.gen == gen]
                for d in o.deps:
                    if d.is_dma:
                        continue
                    if d.eng == e and (e == "tensor" or not same):
                        continue
                    d.signal = True
        for e in ENGS:
            for o in self.ops[e]:
                if o.signal and not o.is_dma and o.sigsem is None:
                    c = self.sig_cnt[e]
                    o.sigsem = (e, c // EPOCH)
                    o.sigval = c % EPOCH + 1
                    self.sig_cnt[e] = c + 1
            assert self.sig_cnt[e] < EPOCH * NPROG, "out of progress semaphores"
        psems, dsems = self.psems, self.dsems
        outstanding = [d for d in self.dma_last if d is not None]
        ops_by_eng = self.ops

        def make(e):
            ops = ops_by_eng[e]

            def body(eng):
                waited = {}
                for o in ops:
                    for d in o.deps:
                        if d.is_dma:
                            key, sem, val = ("d", d.dsem), dsems[d.dsem], d.dval
                        else:
                            if d.eng == e and (e == "tensor" or not same):
                                continue
                            key, sem, val = d.sigsem, psems[d.sigsem[0]][d.sigsem[1]], d.sigval
                        if waited.get(key, 0) >= val:
                            continue
                        waited[key] = val
                        eng.wait_ge(sem, val)
                    ins = o.fn(eng)
                    if o.is_dma:
                        ins.then_inc(dsems[o.dsem], 16)
                    elif o.signal:
                        ins.then_inc(psems[o.sigsem[0]][o.sigsem[1]], 1)
                if e == "sync":
                    for d in outstanding:
                        if waited.get(("d", d.dsem), 0) >= d.dval:
                            continue
                        waited[("d", d.dsem)] = d.dval
                        eng.wait_ge(dsems[d.dsem], d.dval)
            return body

        with nc.Block() as block:
            block.tensor(make("tensor"))
            block.vector(make("vector"))
            block.scalar(make("scalar"))
            block.gpsimd(make("gpsimd"))
            block.sync(make("sync"))
        self.ops = {e: [] for e in ENGS}
        self.gen += 1


def _bf(a):
    return np.ascontiguousarray(np.asarray(a, dtype=np.float32).astype(ml_dtypes.bfloat16))


def make_constants():
    c = {}
    pos = np.arange(SEQ, dtype=np.float32)
    inv = (10000.0 ** (-np.arange(0, HD, 2, dtype=np.float32) / HD)).astype(np.float32)
    ang = (pos[None, :] * inv[:, None]).astype(np.float32)
    cos = np.cos(ang).astype(np.float32)
    sin = np.sin(ang).astype(np.float32)
    p = np.arange(128)
    f = p % 32
    sign = np.where((p % 64) < 32, -1.0, 1.0).astype(np.float32)
    c["c_cos"] = np.ascontiguousarray(cos[f])
    c["c_sin"] = np.ascontiguousarray(sin[f] * sign[:, None])
    eye = np.eye(128, dtype=np.float32)
    c["c_ident"] = _bf(eye)
    c["c_ident4"] = _bf(np.tile(eye, (1, 4)))
    c["c_identf"] = eye.copy()
    q = np.arange(128)[:, None]
    k = np.arange(128)[None, :]
    c["c_causal"] = _bf(np.where(k <= q, 0.0, NEGB))
    c["c_causalf"] = np.where(k <= q, 0.0, -1e30).astype(np.float32)
    c["c_winfar"] = _bf(np.where(k > q, 0.0, NEGB))
    e16 = np.zeros((16, 16, 128), np.float32)
    for n in range(16):
        e16[n, n, :] = 1.0
    c["c_e16"] = _bf(e16)
    e64 = np.zeros((64, 32, 128), np.float32)
    for kt in range(32):
        for kk in range(128):
            e64[2 * kt + kk // 64, kt, kk] = 1.0
    c["c_e64"] = _bf(e64)
    n = np.arange(256)[None, None, :]
    tq = (np.arange(32)[:, None, None] * 128 + np.arange(128)[None, :, None])
    valid = (n < 255) & (16 * n + 31 <= tq)
    c["c_cmpb"] = _bf(np.where(valid, 0.0, NEGB))
    j = np.arange(64)[None, None, :]
    cur = tq // 64
    causal = j <= cur
    forced = (j == 0) | ((j >= cur - 1) & causal)
    c["c_seladd"] = np.where(forced, 1e30, np.where(causal, 0.0, -1e30)).astype(np.float32)
    cs = np.arange(255) * 16
    sbs = np.arange(64) * 64
    shares = (cs[:, None] <= sbs[None, :] + 63) & (cs[:, None] + 31 >= sbs[None, :])
    m = np.zeros((256, 64), np.float32)
    m[:255] = shares
    c["c_c2s"] = _bf(m)
    pp = np.arange(128)
    c["c_tri"] = _bf((pp[:, None] < pp[None, :]).astype(np.float32))
    c["c_ones"] = _bf(np.ones((128, 128), np.float32))
    c["c_ebase"] = np.tile((np.arange(8, dtype=np.float32) * MOE_CAP)[None, None, :], (128, 32, 1))
    c["c_pow2"] = np.tile((2.0 ** -(np.arange(24) + 1.0)).astype(np.float32)[None, :], (128, 1))
    return c


def _perm_cols(w):
    d, n = w.shape
    return np.ascontiguousarray(w.reshape(d, n // 64, 2, 32)[:, :, ::-1, :].reshape(d, n))


def layout_weights(inp):
    out = {}
    for i in range(2):
        w = inp["ev_w_in"][i]
        o = EV_OFF
        fm = np.concatenate([w[:, o[k]:o[k] + EV_SIZES[k]] for k in ("aq", "ak", "bq", "bk", "iq", "ik")], axis=1)
        out[f"ev_fm{i}"] = np.ascontiguousarray(fm)
        out[f"ev_fmp{i}"] = _perm_cols(fm)
        out[f"ev_tm{i}"] = np.ascontiguousarray(
            np.concatenate([w[:, o[k]:o[k] + EV_SIZES[k]] for k in ("av", "bv", "iw")], axis=1))
        w = inp["od_w_in"][i]
        o = OD_OFF
        rope = np.concatenate([w[:, o[k]:o[k] + OD_SIZES[k]] for k in ("cq", "ck", "dq", "dks", "dkw")], axis=1)
        rest = np.concatenate([w[:, o[k]:o[k] + OD_SIZES[k]] for k in ("dkc", "dvc")], axis=1)
        out[f"od_fm{i}"] = np.ascontiguousarray(np.concatenate([rope, rest], axis=1))
        out[f"od_fmp{i}"] = _perm_cols(rope)
        out[f"od_tm{i}"] = np.ascontiguousarray(
            np.concatenate([w[:, o[k]:o[k] + OD_SIZES[k]] for k in ("cv", "dvs", "dvw", "dg")], axis=1))
        out[f"nsa_peT{i}"] = np.ascontiguousarray(np.transpose(inp["nsa_pe"][i], (0, 2, 1)))
    return out


PROJ_SKIP = set()
MOE_ROUTED = True
NFILL = 0
EV_FM = 1984
EV_TM = 644
OD_FM = 2048
OD_FMR = 1792
OD_TM = 792


class KB:
    def __init__(self, nc, st, dbg=()):
        self.nc = nc
        self.st = st
        self.S = Sched(nc, st)
        self.d = {}
        self.dbg = set(dbg)

    def din(self, name, shape, dt):
        ap = self.nc.dram_tensor(name, list(shape), dt, kind="ExternalInput").ap()
        self.d[name] = Buf(ap, name)
        return self.d[name]

    def dscr(self, name, shape, dt, out=False):
        if out or name in self.dbg:
            ap = self.nc.dram_tensor(name, list(shape), dt, kind="ExternalOutput").ap()
        else:
            ap = self.nc.dram_tensor(name, list(shape), dt).ap()
        self.d[name] = Buf(ap, name)
        return self.d[name]

    _uid = 0

    def sb(self, ps, name, shape, dt):
        KB._uid += 1
        nm = f"s{KB._uid}_{name}"
        return Buf(ps.enter_context(self.nc.sbuf_tensor(nm, list(shape), dt)), nm)

    def banks(self, ps, n=8):
        out = []
        for k in range(n):
            KB._uid += 1
            nm = f"p{KB._uid}_bank{k}"
            out.append(Buf(ps.enter_context(self.nc.psum_tensor(nm, [128, 512], F32)), nm))
            out[-1].psum = True
        return out

    def const(self, ps, name, shape, dt, src=None, eng="sync"):
        b = self.sb(ps, name, shape, dt)
        src = self.d[name] if src is None else src
        idx = tuple(slice(None) for _ in shape)
        self.S.dma(b[idx], src[idx], eng=eng)
        return b

    @staticmethod
    def run_units(units, depth=2, fill=None, nfill=0):
        n = len(units)
        for i in range(n + depth):
            if i < n:
                units[i][0]()
                if fill is not None:
                    for _ in range(nfill):
                        fill()
            j = i - depth
            if j >= 0:
                units[j][1]()

    def make_fill(self, ps, bank, ident4=None):
        S = self.S
        if ident4 is None:
            ident4 = self.const(ps, "c_ident4", [128, 512], BF16)
        ones = self.const(ps, "c_ones", [128, 128], BF16)
        fb = Buf(bank.t, "fillbank")
        fb.psum = True

        def fill():
            S.matmul(fb[:, :], lhsT=ones[:, :], rhs=ident4[:, :], start=True, stop=True, skip=True)
        return fill

    def convert(self, dst, src_view, rows):
        S = self.S
        for r0 in range(0, rows, 128):
            r1 = min(rows, r0 + 128)
            S.dma(dst.at(r0)[r0:r1, :], src_view[r0:r1, :], eng="gpsimd")

    def convert_tiled(self, dst, src_view, rows, gcols=256):
        S = self.S
        nc_ = rows // 128
        for c in range(nc_):
            S.dma(View(dst.at(c), dst.t[:, :, c * gcols:(c + 1) * gcols]),
                  src_view[c * 128:(c + 1) * 128, :].rearrange("p (g j) -> g p j", j=gcols), eng="gpsimd")

    def conv_add(self, dst_name, src_view, rows, cols, tiled=False, dst_view=None):
        if not hasattr(self, "cq"):
            self.cq, self.cdone = [], set()
        self.cq.append((dst_name, src_view, rows, rows * cols * 6, tiled, dst_view))

    def conv_budget(self, nbytes):
        while getattr(self, "cq", None) and nbytes > 0:
            name, src, rows, b, tiled, dst_view = self.cq.pop(0)
            if tiled:
                self.convert_tiled(dst_view, src, rows)
            else:
                self.convert(self.d[name], src, rows)
            self.cdone.add(name)
            nbytes -= b

    def conv_ensure(self, names):
        if not hasattr(self, "cq"):
            return
        if any(n not in self.cdone for n in names):
            while any(n not in self.cdone for n in names):
                self.conv_budget(1)
            self.S.flush()

    def emit_xT(self, src, t, identf, tbanks, stage, xT_d, xT32=None):
        S = self.S
        for hb in range(2):
            bank = tbanks[hb]
            for cc in range(4):
                c = hb * 4 + cc
                S.matmul(bank[:, cc * 128:(cc + 1) * 128], lhsT=src[:, c * 128:(c + 1) * 128], rhs=identf[:, :],
                         start=True, stop=True, skip=True)
            S.copy(stage[:, hb * 4:(hb + 1) * 4, (t % 4) * 128:(t % 4 + 1) * 128],
                   bank[:, :].rearrange("p (c t) -> p c t", c=4), eng="scalar" if hb == 0 else "vector")
            if xT32 is not None:
                S.copy(xT32[:, hb * 4:(hb + 1) * 4, :], bank[:, :].rearrange("p (c t) -> p c t", c=4),
                       eng="vector" if hb == 0 else "scalar")
        if t % 4 == 3:
            g = t // 4
            for c in range(8):
                S.dma(xT_d.at((c, g))[c * 128:(c + 1) * 128, g * 512:(g + 1) * 512], stage[:, c, :])

    def phase_prologue(self, x_src, xT_d):
        S = self.S
        with ExitStack() as ps:
            identf = self.const(ps, "c_identf", [128, 128], F32)
            xt = [self.sb(ps, f"xt{k}", [128, 1024], F32) for k in range(2)]
            stage = [self.sb(ps, f"stage{k}", [128, 8, 512], BF16) for k in range(2)]
            bk = self.banks(ps, 4)
            for t in range(NT):
                S.dma(xt[t % 2][:, :], x_src[t * 128:(t + 1) * 128, :])
                self.emit_xT(xt[t % 2][:, :], t, identf, bk[(t % 2) * 2:(t % 2) * 2 + 2], stage[(t // 4) % 2], xT_d)
            S.flush()

    def phase_proj(self, L):
        S, d = self.S, self.d
        even = L % 2 == 0
        i = L // 2
        if even:
            fm, fmp, tm = d[f"b_ev_fm{i}"], d[f"b_ev_fmp{i}"], d[f"b_ev_tm{i}"]
            NFM, NR, NTM = EV_FM, EV_FM, EV_TM
        else:
            fm, fmp, tm = d[f"b_od_fm{i}"], d[f"b_od_fmp{i}"], d[f"b_od_tm{i}"]
            NFM, NR, NTM = OD_FM, OD_FMR, OD_TM
        xT_d, qk_d, vt_d = d["xT"], d["qk"], d["vt"]
        with ExitStack() as ps:
            xT = self.sb(ps, "xT", [128, 8, SEQ], BF16)
            for c in range(8):
                S.dma(xT.at(c)[:, c, :], xT_d[c * 128:(c + 1) * 128, :])
            xTb = [xT.at(c) for c in range(8)]
            cos = self.const(ps, "c_cos", [128, SEQ], F32)
            sin = self.const(ps, "c_sin", [128, SEQ], F32)
            wA = [self.sb(ps, f"wA{k}", [128, 8, 512], BF16) for k in range(2)]
            wB = [self.sb(ps, f"wB{k}", [128, 8, 512], BF16) for k in range(2)]
            osb = [self.sb(ps, f"osb{k}", [128, 2048], BF16) for k in range(2)]
            oraw = [self.sb(ps, f"oraw{k}", [128, 2048], BF16) for k in range(2)]
            t1 = [self.sb(ps, f"t1_{k}", [128, 512], F32) for k in range(2)]
            t2 = [self.sb(ps, f"t2_{k}", [128, 512], F32) for k in range(2)]
            tf = [self.sb(ps, f"tf_{k}", [128, 512], F32) for k in range(2)]
            wtm = self.sb(ps, "wtm", [128, 8, NTM], BF16)
            vsb = [self.sb(ps, f"vsb{k}", [128, NTM], BF16) for k in range(2)]
            sm = [self.sb(ps, f"sm{k}", [128, 24], F32) for k in range(2)]
            bk = self.banks(ps, 8)
            fm_v = fm[:, :].rearrange("(c p) n -> p c n", p=128)
            fmp_v = fmp[:, :].rearrange("(c p) n -> p c n", p=128)
            if not even:
                km = self.sb(ps, "km", [128, 4, 16], F32)
                kmb = self.sb(ps, "kmb", [128, 4, 16], BF16)
                gb = self.sb(ps, "gb", [128, 24], F32)
                S.dma(gb[:, :], d["nsa_gate_b"][i:i + 1, :].broadcast_to([128, 24]))
            ntile = (NFM + 127) // 128
            u = 0
            ostage = 0
            SK = PROJ_SKIP
            for ct in range(ntile if "fm" not in SK else 0):
                rows = min(128, NFM - ct * 128)
                grp, cg = ct // 4, ct % 4
                rope = ct * 128 < NR
                A, B = wA[grp % 2], wB[grp % 2]
                if cg == 0:
                    w = min(512, NFM - grp * 512)
                    S.dma(A[:, :, :w], fm_v[:, :, grp * 512:grp * 512 + w])
                    wb = min(512, NR - grp * 512)
                    if wb > 0:
                        S.dma(B[:, :, :wb], fmp_v[:, :, grp * 512:grp * 512 + wb])
                is_ck = (not even) and 4 <= ct < 8
                is_dq = (not even) and 8 <= ct < 12
                for tc in range(8):
                    tok = slice(tc * 512, (tc + 1) * 512)
                    half, hc = tc // 4, tc % 4
                    if hc == 0:
                        ostage += 1
                    ob = osb[ostage % 2]
                    orw = oraw[ostage % 2]
                    ocol = slice(hc * 512, (hc + 1) * 512)
                    pa = bk[(2 * u) % 8]
                    pb = bk[(2 * u + 1) % 8]
                    for c in range(8):
                        S.matmul(pa[:rows, :], lhsT=A[:, c, cg * 128:cg * 128 + rows],
                                 rhs=View(xTb[c], xT.t[:, c, tok]), start=(c == 0), stop=(c == 7))
                    if rope:
                        for c in range(8):
                            S.matmul(pb[:rows, :], lhsT=B[:, c, cg * 128:cg * 128 + rows],
                                     rhs=View(xTb[c], xT.t[:, c, tok]), start=(c == 0), stop=(c == 7))
                        a1, a2 = t1[u % 2], t2[u % 2]
                        S.tt(a1[:rows, :], pa[:rows, :], cos[:rows, tok], ALU.mult)
                        S.tt(a2[:rows, :], pb[:rows, :], sin[:rows, tok], ALU.mult)
                        if is_ck and "km" not in SK:
                            f = tf[u % 2]
                            S.tt(f[:rows, :], a1[:rows, :], a2[:rows, :], ALU.add, eng="gpsimd")
                            S.copy(ob[:rows, ocol], f[:rows, :], eng="scalar")
                            S.reduce(km[:, ct - 4, tc * 2:(tc + 1) * 2],
                                     f[:, :].rearrange("p (b t) -> p b t", b=2), ALU.add)
                        else:
                            S.tt(ob[:rows, ocol], a1[:rows, :], a2[:rows, :], ALU.add, eng="gpsimd")
                        if is_dq:
                            S.copy(orw[:rows, ocol], pa[:rows, :], eng="scalar")
                    else:
                        S.copy(ob[:rows, ocol], pa[:rows, :], eng="scalar")
                    u += 1
                    if hc == 3:
                        S.dma(qk_d.at((ct, half))[ct * 128:ct * 128 + rows, half * 2048:(half + 1) * 2048], ob[:rows, :])
                        if is_dq:
                            r0 = (ct - 8) * 128
                            S.dma(d["qraw"].at((ct, half))[r0:r0 + 128, half * 2048:(half + 1) * 2048], orw[:, :])
            if not even and "km" not in SK and "fm" not in SK:
                S.ts(kmb[:, :, :], km[:, :, :], 1.0 / 256.0, None, ALU.mult)
                for k4 in range(4):
                    S.dma(d["kmean"].at(k4)[k4 * 128:(k4 + 1) * 128, :], kmb[:, k4, :])
            tm_v = tm[:, :].rearrange("(c p) n -> p c n", p=128)
            S.dma(wtm[:, :, :], tm_v)
            n1 = NTM - 512
            for t in range(NT if "tm" not in SK else 0):
                p0, p1 = bk[(2 * t) % 8], bk[(2 * t + 1) % 8]
                tk = slice(t * 128, (t + 1) * 128)
                for c in range(8):
                    S.matmul(p0[:, :], lhsT=View(xTb[c], xT.t[:, c, tk]), rhs=wtm[:, c, 0:512], start=(c == 0), stop=(c == 7))
                for c in range(8):
                    S.matmul(p1[:, :n1], lhsT=View(xTb[c], xT.t[:, c, tk]), rhs=wtm[:, c, 512:NTM], start=(c == 0), stop=(c == 7))
                vb, s = vsb[t % 2], sm[t % 2]
                S.copy(vb[:, 0:512], p0[:, :], eng="scalar")
                if even:
                    S.copy(vb[:, 512:640], p1[:, 0:128])
                    S.copy(s[:, 0:4], p1[:, 128:132])
                    S.dma(vt_d.at(t)[tk, 0:640], vb[:, 0:640])
                    S.dma(d["iw"].at(t)[tk, :], s[:, 0:4])
                else:
                    S.copy(vb[:, 512:768], p1[:, 0:256])
                    S.tt(s[:, 0:24], p1[:, 256:280], gb[:, :], ALU.add)
                    S.act(s[:, 0:24], s[:, 0:24], AF.Sigmoid)
                    S.dma(vt_d.at(t)[tk, 0:768], vb[:, 0:768])
                    S.dma(d["gates"].at(t)[tk, :], s[:, 0:24])
            S.flush()

    def phase_diff(self, L):
        S, d = self.S, self.d
        i = L // 2
        lam_init = 0.8 - 0.6 * math.exp(-0.3 * L)
        qk_d, vt_d, mix_d = d["qk"], d["vt"], d["mix"]
        with ExitStack() as ps:
            ident = self.const(ps, "c_ident", [128, 128], BF16)
            causal = self.const(ps, "c_causal", [128, 128], BF16)
            lamb = self.sb(ps, "lamb", [128, 256], F32)
            S.dma(lamb[:, :], d["dif_lambda"][i:i + 1].rearrange("o a b -> o (a b)").broadcast_to([128, 256]))
            sub = self.sb(ps, "sub", [128, 128], F32)
            S.dma(sub[:, :], d["dif_subln"][i:i + 1, :].broadcast_to([128, 128]))
            prod = self.sb(ps, "prod", [128, 2, 64], F32)
            S.tt(prod[:, 0, :], lamb[:, 0:64], lamb[:, 64:128], ALU.mult)
            S.tt(prod[:, 1, :], lamb[:, 128:192], lamb[:, 192:256], ALU.mult)
            ssum = self.sb(ps, "ssum", [128, 2], F32)
            S.reduce(ssum[:, :], prod[:, :, :], ALU.add)
            ee = self.sb(ps, "ee", [128, 2], F32)
            S.act(ee[:, :], ssum[:, :], AF.Exp)
            neglam = self.sb(ps, "neglam", [128, 1], F32)
            S.tt(neglam[:, :], ee[:, 1:2], ee[:, 0:1], ALU.subtract)
            S.ts(neglam[:, :], neglam[:, :], -lam_init, None, ALU.add)
            sw = self.sb(ps, "sw", [128, 128], F32)
            S.ts(sw[:, :], sub[:, :], 1.0 - lam_init, None, ALU.mult)
            KT = [[self.sb(ps, f"KT{k}{j}", [64, SEQ], BF16) for j in range(2)] for k in range(2)]
            QT = [[self.sb(ps, f"QT{k}{j}", [64, SEQ], BF16) for j in range(2)] for k in range(2)]
            Vaug = [self.sb(ps, f"Vaug{k}", [128, NT, 129], BF16) for k in range(2)]
            for k in range(2):
                S.memset(Vaug[k][:, :, 128:129], 1.0)
            pT = [self.sb(ps, f"pT{k}", [128, 512], BF16) for k in range(3)]
            rs = [self.sb(ps, f"rs{k}", [128, 2], F32) for k in range(2)]
            rr = [self.sb(ps, f"rr{k}", [128, 2], F32) for k in range(2)]
            tt_ = [self.sb(ps, f"tt{k}", [128, 128], F32) for k in range(2)]
            oo = [self.sb(ps, f"oo{k}", [128, 128], F32) for k in range(2)]
            jk = [self.sb(ps, f"jk{k}", [128, 128], F32) for k in range(2)]
            ss = [self.sb(ps, f"ss{k}", [128, 1], F32) for k in range(2)]
            mo = [self.sb(ps, f"mo{k}", [128, 128], BF16) for k in range(2)]
            bk = self.banks(ps, 8)
            scb = bk[0:3]
            accs = [[bk[3], bk[4]], [bk[5], bk[6]]]
            tmp0 = [self.sb(ps, f"tmp0_{k}", [128, 4, 128], F32) for k in range(2)]
            cnt = dict(u=0, pp=0, a=0)
            fill = self.make_fill(ps, bk[7])

            def make_unit(h, c, j, kt, kt_, qt_, va, acc):
                b0 = max(0, kt - 4 * c)
                col0 = b0 * 128
                diag = kt >= 4 * c
                uu = cnt["u"]
                cnt["u"] += 1
                sc = scb[uu % 3]
                p = pT[uu % 3]

                def score():
                    S.matmul(sc[:, col0:512], lhsT=kt_[j][:, kt * 128:(kt + 1) * 128],
                             rhs=qt_[j][:, c * 512 + col0:(c + 1) * 512], start=True, stop=not diag)
                    if diag:
                        S.matmul(sc[:, col0:col0 + 128], lhsT=causal[:, :], rhs=ident[:, :], start=False, stop=True)
                    S.act(p[:, col0:512], sc[:, col0:512], AF.Exp, scale=SCALE)

                def pv():
                    for b in range(b0, 4):
                        bank = acc[b // 2]
                        o0 = (b % 2) * 129
                        S.matmul(bank[:, o0:o0 + 129], lhsT=p[:, b * 128:(b + 1) * 128], rhs=va[:, kt, :],
                                 start=(kt == 0 and b % 2 == 0), stop=(kt == 4 * c + b), skip=True)
                    if kt == 4 * c + 3:
                        post(h, c, j, acc)
                return (score, pv)

            def post(h, c, j, acc):
                t0 = tmp0[c % 2]
                for b in range(4):
                    qt = 4 * c + b
                    o0 = (b % 2) * 129
                    A = acc[b // 2]
                    k2 = cnt["pp"] % 2
                    cnt["pp"] += 1
                    S.ts(rs[k2][:, 0:1], A[:, o0 + 128:o0 + 129], 1e-30, None, ALU.max)
                    S.recip(rr[k2][:, 0:1], rs[k2][:, 0:1])
                    if j == 0:
                        S.ts(t0[:, b, :], A[:, o0:o0 + 128], rr[k2][:, 0:1], None, ALU.mult)
                        continue
                    S.ts(tt_[k2][:, :], A[:, o0:o0 + 128], rr[k2][:, 0:1], neglam[:, 0:1], ALU.mult, ALU.mult)
                    S.tt(oo[k2][:, :], t0[:, b, :], tt_[k2][:, :], ALU.add, eng="gpsimd")
                    S.memset(ss[k2][:, :], 0.0)
                    S.act(jk[k2][:, :], oo[k2][:, :], AF.Square, accum_out=ss[k2][:, 0:1])
                    S.ts(ss[k2][:, :], ss[k2][:, :], 1.0 / 128.0, LN_EPS, ALU.mult, ALU.add)
                    S.act(ss[k2][:, :], ss[k2][:, :], AF.Ln)
                    S.act(ss[k2][:, :], ss[k2][:, :], AF.Exp, scale=-0.5)
                    S.stt(mo[k2][:, :], oo[k2][:, :], ss[k2][:, 0:1], sw[:, :], ALU.mult, ALU.mult)
                    S.dma(mix_d.at(("a", h, qt))[qt * 128:(qt + 1) * 128, h * 128:(h + 1) * 128], mo[k2][:, :])

            for h in range(4):
                kt_, qt_, va = KT[h % 2], QT[h % 2], Vaug[h % 2]
                for j in range(2):
                    r0 = (h * 2 + j) * 64
                    S.dma(qt_[j][:, :], qk_d[r0:r0 + 64, :])
                    S.dma(kt_[j][:, :], qk_d[512 + r0:512 + r0 + 64, :])
                S.dma(va[:, :, 0:128], vt_d[:, h * 128:(h + 1) * 128].rearrange("(kt p) e -> p kt e", p=128))
                units = []
                for c in range(8):
                    for j in range(2):
                        acc = accs[cnt["a"] % 2]
                        cnt["a"] += 1
                        for kt in range(4 * c + 4):
                            units.append(make_unit(h, c, j, kt, kt_, qt_, va, acc))
                self.run_units(units, fill=fill, nfill=NFILL)
            S.flush()

    def phase_dsa(self, L, nit=22):
        S, d = self.S, self.d
        qk_d, vt_d, mix_d = d["qk"], d["vt"], d["mix"]
        R_BQ, R_BK, R_IQ, R_IK = 1024, 1536, 1664, 1920
        with ExitStack() as ps:
            ident4 = self.const(ps, "c_ident4", [128, 512], BF16)
            causalf = self.const(ps, "c_causalf", [128, 128], F32)
            pow2 = self.const(ps, "c_pow2", [128, 24], F32)
            bkT = self.sb(ps, "bkT", [64, 2, SEQ], BF16)
            S.dma(bkT[:, :, :], qk_d[R_BK:R_BK + 128, :].rearrange("(g d) t -> d g t", d=64))
            ikT = self.sb(ps, "ikT", [64, SEQ], BF16)
            S.dma(ikT[:, :], qk_d[R_IK:R_IK + 64, :])
            Vaug = self.sb(ps, "Vaug", [128, NT, 2, 65], BF16)
            S.memset(Vaug[:, :, :, 64:65], 1.0)
            for g in range(2):
                S.dma(Vaug[:, :, g, 0:64], vt_d[:, 512 + g * 64:512 + (g + 1) * 64].rearrange("(kt p) e -> p kt e", p=128))
            iqT = [self.sb(ps, f"iqT{k}", [64, 4, 128], BF16) for k in range(2)]
            bqT = [self.sb(ps, f"bqT{k}", [64, 8 * 128], BF16) for k in range(2)]
            iwt = [self.sb(ps, f"iwt{k}", [128, 4], F32) for k in range(2)]
            score = [self.sb(ps, f"score{k}", [128, SEQ], F32) for k in range(2)]
            biasq = [self.sb(ps, f"biasq{k}", [128, SEQ], BF16) for k in range(2)]
            junk = self.sb(ps, "junk", [128, SEQ], BF16)
            rl = [self.sb(ps, f"rl{k}", [128, 512], F32) for k in range(2)]
            pT = [self.sb(ps, f"pT{k}", [128, 512], BF16) for k in range(3)]
            mn = self.sb(ps, "mn", [128, 1], F32)
            mx = self.sb(ps, "mx", [128, 1], F32)
            w0 = self.sb(ps, "w0", [128, 1], F32)
            halfs = self.sb(ps, "halfs", [128, 24], F32)
            lo = self.sb(ps, "lo", [128, 1], F32)
            mid = self.sb(ps, "mid", [128, 1], F32)
            cnt = self.sb(ps, "cnt", [128, 24], F32)
            c256 = self.sb(ps, "c256", [128, 1], F32)
            S.memset(c256[:, :], 256.0)
            step = self.sb(ps, "step", [128, 1], F32)
            rs = self.sb(ps, "rs", [128, 4, 1], F32)
            rr = self.sb(ps, "rr", [128, 4, 1], F32)
            mo = [self.sb(ps, f"mo{k}", [128, 512], BF16) for k in range(2)]
            bk = self.banks(ps, 8)
            idxb = bk[0:2]
            scb = bk[2:5]
            accb = bk[5:7]
            cnts = dict(u=0, v=0, a=0)
            fill = self.make_fill(ps, bk[7], ident4=ident4)

            def index_tile(qt):
                k2 = qt % 2
                N = 128 * (qt + 1)
                qs = slice(qt * 128, (qt + 1) * 128)
                S.dma(iqT[k2][:, :, :], qk_d[R_IQ:R_IQ + 256, qs].rearrange("(h d) q -> d h q", d=64))
                S.dma(bqT[k2][:, :].rearrange("d (h q) -> d h q", h=8), qk_d[R_BQ:R_BQ + 512, qs].rearrange("(h d) q -> d h q", d=64))
                S.dma(iwt[k2][:, :], d["iw"][qs, :])
                sc = score[k2]
                nch = (N + 511) // 512
                for ch in range(nch):
                    w = min(512, N - ch * 512)
                    ks = slice(ch * 512, ch * 512 + w)
                    for h in range(4):
                        pl = idxb[cnts["v"] % 2]
                        r = rl[cnts["v"] % 2]
                        cnts["v"] += 1
                        S.matmul(pl[:, :w], lhsT=iqT[k2][:, h, :], rhs=ikT[:, ks], start=True, stop=True)
                        S.act(r[:, :w], pl[:, :w], AF.Relu)
                        if h == 0:
                            S.ts(sc[:, ks], r[:, :w], iwt[k2][:, 0:1], None, ALU.mult)
                        else:
                            S.stt(sc[:, ks], r[:, :w], iwt[k2][:, h:h + 1], sc[:, ks], ALU.mult, ALU.add)
                S.reduce(mn[:, :], sc[:, :N], ALU.min)
                S.reduce(mx[:, :], sc[:, :N], ALU.max)
                S.tt(sc[:, N - 128:N], sc[:, N - 128:N], causalf[:, :], ALU.add)
                S.tt(w0[:, :], mx[:, :], mn[:, :], ALU.subtract)
                S.ts(halfs[:, :], pow2[:, :], w0[:, 0:1], None, ALU.mult)
                S.memset(cnt[:, :], 0.0)
                S.copy(lo[:, :], mn[:, :])
                S.tt(mid[:, :], mn[:, :], halfs[:, 0:1], ALU.add)
                for n in range(nit):
                    S.ts(junk[:, :N], sc[:, :N], mid[:, 0:1], 0.0, ALU.is_ge, ALU.add, accum_out=cnt[:, n:n + 1])
                    S.ts(step[:, :], cnt[:, n:n + 1], c256[:, 0:1], halfs[:, n:n + 1], ALU.is_ge, ALU.mult)
                    S.tt(lo[:, :], lo[:, :], step[:, :], ALU.add)
                    if n + 1 < nit:
                        S.tt(mid[:, :], lo[:, :], halfs[:, n + 1:n + 2], ALU.add)
                S.ts(biasq[k2][:, :N], sc[:, :N], lo[:, 0:1], NEGB, ALU.is_lt, ALU.mult)
                if "dbg_lo" in d:
                    dl = self.sb(ps, f"dl{qt}", [128, 4], F32)
                    S.copy(dl[:, 0:1], lo[:, :])
                    S.copy(dl[:, 1:2], cnt[:, nit - 1:nit])
                    S.copy(dl[:, 2:3], mn[:, :])
                    S.copy(dl[:, 3:4], mx[:, :])
                    S.dma(d["dbg_lo"].at(qt)[qs, :], dl[:, :])

            def attend_tile(qt):
                k2 = qt % 2
                m = mo[qt % 2]
                units = []
                for g in range(2):
                    acc = accb[cnts["a"] % 2]
                    cnts["a"] += 1
                    for kt in range(qt + 1):
                        units.append(make_unit(qt, k2, m, g, kt, acc))
                self.run_units(units, fill=fill, nfill=NFILL)
                S.dma(mix_d.at(("b", qt))[qt * 128:(qt + 1) * 128, 512:1024], m[:, :])

            def make_unit(qt, k2, m, g, kt, acc):
                uu = cnts["u"]
                cnts["u"] += 1
                sc = scb[uu % 3]
                p = pT[uu % 3]
                ks = slice(kt * 128, (kt + 1) * 128)

                def score():
                    S.matmul(sc[:, :], lhsT=bkT[:, g, ks], rhs=bqT[k2][:, g * 512:(g + 1) * 512], start=True, stop=False)
                    S.matmul(sc[:, :], lhsT=biasq[k2][:, ks], rhs=ident4[:, :], start=False, stop=True)
                    S.act(p[:, :], sc[:, :], AF.Exp, scale=SCALE)

                def pv():
                    for r in range(4):
                        S.matmul(acc[:, r * 65:(r + 1) * 65], lhsT=p[:, r * 128:(r + 1) * 128], rhs=Vaug[:, kt, g, :],
                                 start=(kt == 0 and r == 0), stop=(kt == qt), skip=True)
                    if kt == qt:
                        a3 = acc[:, 0:260].rearrange("p (r e) -> p r e", e=65)
                        S.ts(rs[:, :, :], a3[:, :, 64:65], 1e-30, None, ALU.max)
                        S.recip(rr[:, :, :], rs[:, :, :])
                        for r in range(4):
                            hcol = (4 * g + r) * 64
                            S.ts(m[:, hcol:hcol + 64], acc[:, r * 65:r * 65 + 64], rr[:, r, :], None, ALU.mult)
                return (score, pv)

            index_tile(0)
            for qt in range(NT):
                if qt + 1 < NT:
                    index_tile(qt + 1)
                attend_tile(qt)
            S.flush()

    def ln_alloc(self, ps, L, which, nbuf=2):
        S, d = self.S, self.d
        r = {"nbuf": nbuf}
        r["g"] = self.sb(ps, "lng", [128, D], F32)
        r["b"] = self.sb(ps, "lnb", [128, D], F32)
        S.dma(r["g"][:, :], d[f"ln_{which}_g"][L:L + 1, :].broadcast_to([128, D]))
        S.dma(r["b"][:, :], d[f"ln_{which}_b"][L:L + 1, :].broadcast_to([128, D]))
        r["xr"] = [self.sb(ps, f"xr{k}", [128, D], F32) for k in range(nbuf)]
        r["z"] = [self.sb(ps, f"z{k}", [128, D], F32) for k in range(nbuf)]
        r["xn"] = [self.sb(ps, f"xn{k}", [128, D], F32) for k in range(nbuf)]
        r["st"] = [self.sb(ps, f"st{k}", [128, 2, 6], F32) for k in range(nbuf)]
        r["mv"] = [self.sb(ps, f"mv{k}", [128, 2], F32) for k in range(nbuf)]
        r["rstd"] = [self.sb(ps, f"rstd{k}", [128, 1], F32) for k in range(nbuf)]
        r["stage"] = [self.sb(ps, f"stage{k}", [128, 8, 512], BF16) for k in range(nbuf)]
        r["identf"] = self.const(ps, "c_identf", [128, 128], F32)
        return r

    def ln_tile(self, r, t, y_views, res_src, dst, tbanks, want_xT=True, xT32=None, defer=False):
        S, d = self.S, self.d
        nb = r["nbuf"]
        k = t % nb
        tk = slice(t * 128, (t + 1) * 128)
        xr, z, xn = r["xr"][k], r["z"][k], r["xn"][k]
        S.dma(xr[:, :], View(res_src.at(t), res_src.t[tk, :]))
        for hh in range(2):
            cs = slice(hh * 512, (hh + 1) * 512)
            S.stt(z[:, cs], xr[:, cs], float(DN_ALPHA), y_views[hh], ALU.mult, ALU.add)
            S.bn_stats(r["st"][k][:, hh, :], z[:, cs])
        S.bn_aggr(r["mv"][k][:, :], r["st"][k][:, :, :])
        rstd = r["rstd"][k]
        S.ts(rstd[:, :], r["mv"][k][:, 1:2], LN_EPS, None, ALU.add)
        S.act(rstd[:, :], rstd[:, :], AF.Ln)
        S.act(rstd[:, :], rstd[:, :], AF.Exp, scale=-0.5)
        S.ts(xn[:, :], z[:, :], r["mv"][k][:, 0:1], rstd[:, 0:1], ALU.subtract, ALU.mult)
        S.tt(xn[:, :], xn[:, :], r["g"][:, :], ALU.mult, eng="gpsimd")
        S.tt(xn[:, :], xn[:, :], r["b"][:, :], ALU.add, eng="gpsimd")
        final = dst.name == "out"
        S.dma(View(dst.at(t), dst.t[tk, :]), xn[:, :], final=final)
        if want_xT:
            def later():
                self.emit_xT(xn[:, :], t, r["identf"], tbanks, r["stage"][(t // 4) % nb], d["xT"], xT32=xT32)
            if defer:
                return later
            later()
        return None

    def phase_outproj(self, L):
        S, d = self.S, self.d
        even = L % 2 == 0
        i = L // 2
        wout_d = d[f"b_ev_wout{i}"] if even else d[f"b_od_wout{i}"]
        res_src = d["x"] if (L == 0 or "force_x" in self.dbg) else d["xres"]
        with ExitStack() as ps:
            r = self.ln_alloc(ps, L, "mix")
            ident = self.const(ps, "c_ident", [128, 128], BF16)
            wout = self.sb(ps, "wout", [128, 8, D], BF16)
            S.dma(wout[:, :, :], wout_d[:, :].rearrange("(c p) n -> p c n", p=128))
            mixt = [self.sb(ps, f"mixt{k}", [128, D], BF16) for k in range(2)]
            mixT = [self.sb(ps, f"mixT{k}", [128, 8, 128], BF16) for k in range(2)]
            bk = self.banks(ps, 8)
            if not even:
                wr = self.sb(ps, "wr", [128, 8, 8], F32)
                S.dma(wr[:, :, :], d["moe_w_router"][i].rearrange("(c p) e -> p c e", p=128))
                br = self.sb(ps, "br", [128, 8], F32)
                S.dma(br[:, :], d["moe_b_router"][i:i + 1, :].broadcast_to([128, 8]))
                xT32 = [self.sb(ps, f"xT32_{k}", [128, 8, 128], F32) for k in range(2)]
                lg = [self.sb(ps, f"lg{k}", [128, 8], F32) for k in range(2)]
                m8 = [self.sb(ps, f"m8{k}", [128, 8], F32) for k in range(2)]
                dd = [self.sb(ps, f"dd{k}", [128, 1], F32) for k in range(2)]
                g12 = [self.sb(ps, f"g12{k}", [128, 2], F32) for k in range(2)]
                G = [self.sb(ps, f"G{k}", [128, 8], F32) for k in range(2)]
                G2 = [self.sb(ps, f"G2{k}", [128, 8], F32) for k in range(2)]
            pends = []
            for t in range(NT):
                k = t % 2
                tk = slice(t * 128, (t + 1) * 128)
                S.dma(mixt[k][:, :], View(d["mix"].at(("r", t)), d["mix"].t[tk, :]))
                for hb in range(2):
                    tb = bk[hb]
                    for cc in range(4):
                        c = hb * 4 + cc
                        S.matmul(tb[:, cc * 128:(cc + 1) * 128], lhsT=mixt[k][:, c * 128:(c + 1) * 128], rhs=ident[:, :],
                                 start=True, stop=True, skip=True)
                    S.copy(mixT[k][:, hb * 4:(hb + 1) * 4, :], tb[:, :].rearrange("p (c t) -> p c t", c=4),
                           eng="scalar" if hb == 0 else "vector")
                yb = [bk[2 + 2 * k], bk[3 + 2 * k]]
                for hh in range(2):
                    for c in range(8):
                        S.matmul(yb[hh][:, :], lhsT=mixT[k][:, c, :], rhs=wout[:, c, hh * 512:(hh + 1) * 512],
                                 start=(c == 0), stop=(c == 7))
                pend = self.ln_tile(r, t, [yb[0][:, :], yb[1][:, :]], res_src, d["xres"], bk[6:8],
                                    xT32=None if even else xT32[k], defer=True)
                pends.append((t, k, pend))
                if len(pends) > 1:
                    self._outproj_tail(*pends.pop(0), even, locals())
            while pends:
                self._outproj_tail(*pends.pop(0), even, locals())
            if False:
                if not even:
                    lp = bk[0]
                    for c in range(8):
                        S.matmul(lp[:, 0:8], lhsT=xT32[k][:, c, :], rhs=wr[:, c, :], start=(c == 0), stop=(c == 7))
                    S.tt(lg[k][:, :], lp[:, 0:8], br[:, :], ALU.add)
                    S.max8(m8[k][:, :], lg[k][:, :])
                    S.tt(dd[k][:, :], m8[k][:, 0:1], m8[k][:, 1:2], ALU.subtract)
                    S.act(g12[k][:, 0:1], dd[k][:, :], AF.Sigmoid)
                    S.act(g12[k][:, 1:2], dd[k][:, :], AF.Sigmoid, scale=-1.0)
                    S.ts(G[k][:, :], lg[k][:, :], m8[k][:, 0:1], g12[k][:, 0:1], ALU.is_equal, ALU.mult)
                    S.ts(G2[k][:, :], lg[k][:, :], m8[k][:, 1:2], g12[k][:, 1:2], ALU.is_equal, ALU.mult)
                    S.tt(G[k][:, :], G[k][:, :], G2[k][:, :], ALU.add)
                    S.dma(d["moeg"].at(t)[tk, :], G[k][:, :])
                    S.ts(G2[k][:, :], lg[k][:, :], m8[k][:, 0:1], None, ALU.is_equal)
                    S.dma(d["moem1"].at(t)[tk, :], G2[k][:, :])
                    S.ts(G[k][:, :], lg[k][:, :], m8[k][:, 1:2], None, ALU.is_equal)
                    S.dma(d["moem2"].at(t)[tk, :], G[k][:, :])
                    S.dma(d["moegv"].at(t)[tk, :], g12[k][:, :])
            S.flush()

    def _outproj_tail(self, t, k, pend, even, L_):
        S, d = self.S, self.d
        if pend is not None:
            pend()
        if even:
            return
        bk, xT32, wr, br = L_["bk"], L_["xT32"], L_["wr"], L_["br"]
        lg, m8, dd, g12, G, G2 = L_["lg"], L_["m8"], L_["dd"], L_["g12"], L_["G"], L_["G2"]
        tk = slice(t * 128, (t + 1) * 128)
        lp = bk[0]
        for c in range(8):
            S.matmul(lp[:, 0:8], lhsT=xT32[k][:, c, :], rhs=wr[:, c, :], start=(c == 0), stop=(c == 7))
        S.tt(lg[k][:, :], lp[:, 0:8], br[:, :], ALU.add)
        S.max8(m8[k][:, :], lg[k][:, :])
        S.tt(dd[k][:, :], m8[k][:, 0:1], m8[k][:, 1:2], ALU.subtract)
        S.act(g12[k][:, 0:1], dd[k][:, :], AF.Sigmoid)
        S.act(g12[k][:, 1:2], dd[k][:, :], AF.Sigmoid, scale=-1.0)
        S.ts(G[k][:, :], lg[k][:, :], m8[k][:, 0:1], g12[k][:, 0:1], ALU.is_equal, ALU.mult)
        S.ts(G2[k][:, :], lg[k][:, :], m8[k][:, 1:2], g12[k][:, 1:2], ALU.is_equal, ALU.mult)
        S.tt(G[k][:, :], G[k][:, :], G2[k][:, :], ALU.add)
        S.dma(d["moeg"].at(t)[tk, :], G[k][:, :])
        S.ts(G2[k][:, :], lg[k][:, :], m8[k][:, 0:1], None, ALU.is_equal)
        S.dma(d["moem1"].at(t)[tk, :], G2[k][:, :])
        S.ts(G[k][:, :], lg[k][:, :], m8[k][:, 1:2], None, ALU.is_equal)
        S.dma(d["moem2"].at(t)[tk, :], G[k][:, :])
        S.dma(d["moegv"].at(t)[tk, :], g12[k][:, :])

    def phase_ffn_dense(self, L):
        S, d = self.S, self.d
        i = L // 2
        NF = F_DENSE // 128
        final = L == DEPTH - 1
        dst = d["out"] if final else d["xres"]
        with ExitStack() as ps:
            r = self.ln_alloc(ps, L, "ffn")
            Wd = self.sb(ps, "Wd", [128, NF, D], BF16)
            wd_v = d[f"b_ffd{i}"][:, :].rearrange("(f p) n -> p f n", p=128)
            for f0 in range(0, NF, 4):
                f1 = min(NF, f0 + 4)
                S.dma(Wd.at(f0)[:, f0:f1, :], wd_v[:, f0:f1, :])
            xTc = [self.sb(ps, f"xTc{k}", [128, 8, 512], BF16) for k in range(2)]
            Wg = [self.sb(ps, f"Wg{k}", [128, 8, 256], BF16) for k in range(2)]
            Wu = [self.sb(ps, f"Wu{k}", [128, 8, 256], BF16) for k in range(2)]
            hT = self.sb(ps, "hT", [128, NF, 512], BF16)
            sg = [self.sb(ps, f"sg{k}", [128, 512], F32) for k in range(2)]
            bk = self.banks(ps, 8)
            g_t = d[f"b_ffg{i}"]
            u_t = d[f"b_ffu{i}"]
            xT_v = d["xT"][:, :].rearrange("(c p) t -> p c t", p=128)
            u = 0
            w = 0
            pend = None
            for tc in range(8):
                xc = xTc[tc % 2]
                S.dma(xc[:, :, :], View(d["xT"].at(("c", tc)), xT_v.ap[:, :, tc * 512:(tc + 1) * 512]))
                for fg in range(NF // 2):
                    wg, wu = Wg[w % 2], Wu[w % 2]
                    w += 1
                    S.dma(wg[:, :, :], g_t[fg].rearrange("p (c j) -> p c j", c=8))
                    S.dma(wu[:, :, :], u_t[fg].rearrange("p (c j) -> p c j", c=8))
                    for f2 in range(2):
                        ft = fg * 2 + f2
                        pg, pu = bk[(2 * u) % 4], bk[(2 * u + 1) % 4]
                        for c in range(8):
                            S.matmul(pg[:, :], lhsT=wg[:, c, f2 * 128:(f2 + 1) * 128], rhs=xc[:, c, :], start=(c == 0), stop=(c == 7))
                        for c in range(8):
                            S.matmul(pu[:, :], lhsT=wu[:, c, f2 * 128:(f2 + 1) * 128], rhs=xc[:, c, :], start=(c == 0), stop=(c == 7))
                        S.act(sg[u % 2][:, :], pg[:, :], AF.Silu)
                        S.tt(View(hT.at(ft), hT.t[:, ft, :]), sg[u % 2][:, :], pu[:, :], ALU.mult)
                        u += 1
                for t4 in range(4):
                    t = tc * 4 + t4
                    yb = [bk[4], bk[5]]
                    for hh in range(2):
                        for ft in range(NF):
                            S.matmul(yb[hh][:, :], lhsT=View(hT.at(ft), hT.t[:, ft, t4 * 128:(t4 + 1) * 128]),
                                     rhs=View(Wd.at((ft // 4) * 4), Wd.t[:, ft, hh * 512:(hh + 1) * 512]),
                                     start=(ft == 0), stop=(ft == NF - 1))
                    if pend is not None:
                        pend()
                    pend = self.ln_tile(r, t, [yb[0][:, :], yb[1][:, :]], d["xres"], dst, bk[6:8], want_xT=not final,
                                        defer=True)
            if pend is not None:
                pend()
            S.flush()

    def phase_cmp(self, L):
        S, d = self.S, self.d
        i = L // 2
        qk_d = d["qk"]
        with ExitStack() as ps:
            bk = self.banks(ps, 8)
            w1 = [self.sb(ps, f"w1_{a}", [64, 32, 256], BF16) for a in range(2)]
            w2 = [self.sb(ps, f"w2_{a}", [128, 2, 64], BF16) for a in range(2)]
            peT = [self.sb(ps, f"peT{a}", [64, 32], F32) for a in range(2)]
            for a in range(2):
                S.dma(w1[a][:, :, :], d[f"b_phi1_{i}"][a * 2048:(a + 1) * 2048, :].rearrange("(l d) j -> d l j", d=64))
                S.dma(w2[a][:, :, :], d[f"b_phi2_{i}"][a * 256:(a + 1) * 256, :].rearrange("(jh p) e -> p jh e", p=128))
                S.dma(peT[a][:, :], d[f"nsa_peT{i}"][a])
            src = [self.sb(ps, f"src{k}", [64, SEQ], BF16) for k in range(2)]
            hl = [self.sb(ps, f"hl{k}", [64, 32, 256], BF16) for k in range(2)]
            zs = [self.sb(ps, f"zs{k}", [128, 256], F32) for k in range(2)]
            z2 = [self.sb(ps, f"z2{k}", [128, 256], F32) for k in range(2)]
            sgm = [self.sb(ps, f"sgm{k}", [128, 256], F32) for k in range(2)]
            gz = [self.sb(ps, f"gz{k}", [128, 2, 256], BF16) for k in range(2)]
            ko = [self.sb(ps, f"ko{k}", [64, 256], BF16) for k in range(2)]
            vo = [self.sb(ps, f"vo{k}", [128, 2, 64], BF16) for k in range(2)]
            for k in range(2):
                S.memset(gz[k][:, :, :], 0.0)
                S.memset(ko[k][:, :], 0.0)
            n = 0
            for a in range(2):
                for g in range(2):
                    k = n % 2
                    n += 1
                    r0 = (1792 if a == 0 else 1920) + g * 64
                    S.dma(src[k][:, :], qk_d[r0:r0 + 64, :])
                    for l in range(32):
                        S.ts(hl[k][:, l, 0:255], src[k][:, l:l + 16 * 254 + 1:16], peT[a][:, l:l + 1], None, ALU.add,
                             eng="vector" if l % 2 == 0 else "gpsimd")
                    for jh in range(2):
                        zb = bk[jh]
                        for l in range(32):
                            S.matmul(zb[:, 0:255], lhsT=w1[a][:, l, jh * 128:(jh + 1) * 128], rhs=hl[k][:, l, 0:255],
                                     start=(l == 0), stop=(l == 31))
                        S.copy(zs[jh][:, 0:255], zb[:, 0:255], eng="scalar")
                        S.tt(z2[jh][:, 0:255], zs[jh][:, 0:255], zs[jh][:, 0:255], ALU.mult)
                        S.ts(z2[jh][:, 0:255], z2[jh][:, 0:255], 0.044715, 1.0, ALU.mult, ALU.add)
                        S.tt(z2[jh][:, 0:255], z2[jh][:, 0:255], zs[jh][:, 0:255], ALU.mult)
                        S.act(sgm[jh][:, 0:255], z2[jh][:, 0:255], AF.Sigmoid, scale=1.5957691216057308)
                        S.tt(gz[k][:, jh, 0:255], zs[jh][:, 0:255], sgm[jh][:, 0:255], ALU.mult)
                    if a == 0:
                        ob = bk[2]
                        for jh in range(2):
                            S.matmul(ob[0:64, 0:255], lhsT=w2[a][:, jh, :], rhs=gz[k][:, jh, 0:255], start=(jh == 0), stop=(jh == 1))
                        S.copy(ko[g][:, 0:255], ob[0:64, 0:255])
                        S.dma(d["kcmpT"].at(g)[g], ko[g][:, :])
                    else:
                        for nt in range(2):
                            ob = bk[3 + nt]
                            for jh in range(2):
                                S.matmul(ob[:, 0:64], lhsT=gz[k][:, jh, nt * 128:(nt + 1) * 128], rhs=w2[a][:, jh, :],
                                         start=(jh == 0), stop=(jh == 1))
                            S.copy(vo[g][:, nt, :], ob[:, 0:64])
                        S.dma(d["vcmp"].at(g)[g].rearrange("(nt p) e -> p nt e", p=128), vo[g][:, :, :])
            S.flush()

    def phase_moba(self, L):
        S, d = self.S, self.d
        qk_d, vt_d, mix_d = d["qk"], d["vt"], d["mix"]
        with ExitStack() as ps:
            ident = self.const(ps, "c_ident", [128, 128], BF16)
            causal = self.const(ps, "c_causal", [128, 128], BF16)
            e16 = self.const(ps, "c_e16", [16, 16, 128], BF16)
            QT = [self.sb(ps, f"QT{k}", [64, SEQ], BF16) for k in range(2)]
            KT = [self.sb(ps, f"KT{k}", [64, SEQ], BF16) for k in range(2)]
            Vaug = [self.sb(ps, f"Vaug{k}", [128, NT, 65], BF16) for k in range(2)]
            kmT = [self.sb(ps, f"kmT{k}", [64, 16], BF16) for k in range(2)]
            for k in range(2):
                S.memset(Vaug[k][:, :, 64:65], 1.0)
            gate = [self.sb(ps, f"gate{k}", [128, 2, 16], F32) for k in range(2)]
            m8 = [self.sb(ps, f"m8{k}", [128, 2, 8], F32) for k in range(2)]
            bias = [self.sb(ps, f"bias{k}", [128, 2, 16], BF16) for k in range(2)]
            biasT = [self.sb(ps, f"biasT{k}", [16, 16, 256], BF16) for k in range(2)]
            pT = [self.sb(ps, f"pT{k}", [128, 256], BF16) for k in range(3)]
            rs = [self.sb(ps, f"rs{k}", [128, 2, 1], F32) for k in range(2)]
            rr = [self.sb(ps, f"rr{k}", [128, 2, 1], F32) for k in range(2)]
            mo = [self.sb(ps, f"mo{k}", [128, NT, 64], BF16) for k in range(2)]
            bk = self.banks(ps, 8)
            scb = bk[0:3]
            accb = bk[3:5]
            gpb = [bk[5], bk[5]]
            tpb = bk[7]
            cnt = dict(u=0)
            fill = self.make_fill(ps, bk[6])

            def make_unit(h, hk, B, kt, qt_, kt_, va, acc):
                uu = cnt["u"]
                cnt["u"] += 1
                sc = scb[uu % 3]
                p = pT[uu % 3]
                ks = slice(kt * 128, (kt + 1) * 128)
                col0 = 128 if kt == 2 * B + 1 else 0
                past = kt < 2 * B
                sel = B > 3
                k2 = B % 2

                def score():
                    last_first = not ((past and sel) or (not past))
                    S.matmul(sc[:, col0:256], lhsT=kt_[:, ks], rhs=qt_[:, B * 256 + col0:(B + 1) * 256],
                             start=True, stop=last_first)
                    if past and sel:
                        S.matmul(sc[:, 0:256], lhsT=e16[:, kt // 2, :], rhs=biasT[hk][:, B, :], start=False, stop=True)
                    if not past:
                        c0 = (kt - 2 * B) * 128
                        S.matmul(sc[:, c0:c0 + 128], lhsT=causal[:, :], rhs=ident[:, :], start=False, stop=True)
                    S.act(p[:, col0:256], sc[:, col0:256], AF.Exp, scale=SCALE)

                def pv():
                    for t in range(col0 // 128, 2):
                        S.matmul(acc[:, t * 65:(t + 1) * 65], lhsT=p[:, t * 128:(t + 1) * 128], rhs=va[:, kt, :],
                                 start=(kt == 0 and t == 0), stop=(kt == 2 * B + t), skip=True)
                    if kt == 2 * B + 1:
                        a3 = acc[:, 0:130].rearrange("p (t e) -> p t e", e=65)
                        S.ts(rs[k2][:, :, :], a3[:, :, 64:65], 1e-30, None, ALU.max)
                        S.recip(rr[k2][:, :, :], rs[k2][:, :, :])
                        for t in range(2):
                            S.ts(mo[hk][:, 2 * B + t, :], acc[:, t * 65:t * 65 + 64], rr[k2][:, t, :], None, ALU.mult)
                return (score, pv)

            for h in range(8):
                hk = h % 2
                qt_, kt_, va = QT[hk], KT[hk], Vaug[hk]
                S.dma(qt_[:, :], qk_d[h * 64:(h + 1) * 64, :])
                S.dma(kt_[:, :], qk_d[512 + h * 64:512 + (h + 1) * 64, :])
                S.dma(va[:, :, 0:64], vt_d[:, h * 64:(h + 1) * 64].rearrange("(kt p) e -> p kt e", p=128))
                S.dma(kmT[hk][:, :], d["kmean"][h * 64:(h + 1) * 64, :])
                for B in range(4, 16):
                    k2 = B % 2
                    gp = gpb[k2]
                    for t in range(2):
                        S.matmul(gp[:, t * 16:(t + 1) * 16], lhsT=qt_[:, (2 * B + t) * 128:(2 * B + t + 1) * 128],
                                 rhs=kmT[hk][:, :], start=True, stop=True, skip=True)
                    S.memset(gate[k2][:, :, :], -1e30)
                    S.copy(gate[k2][:, :, 0:B], gp[:, 0:32].rearrange("p (t n) -> p t n", t=2)[:, :, 0:B])
                    for t in range(2):
                        S.max8(m8[k2][:, t, :], gate[k2][:, t, :])
                        S.ts(bias[k2][:, t, :], gate[k2][:, t, :], m8[k2][:, t, 2:3], NEGB, ALU.is_lt, ALU.mult)
                        S.matmul(tpb[0:16, t * 128:(t + 1) * 128], lhsT=bias[k2][:, t, :], rhs=ident[:, :],
                                 start=True, stop=True, skip=True)
                    S.copy(biasT[hk][:, B, :], tpb[0:16, 0:256], eng="scalar")
                units = []
                for B in range(16):
                    acc = accb[B % 2]
                    for kt in range(2 * B + 2):
                        units.append(make_unit(h, hk, B, kt, qt_, kt_, va, acc))
                self.run_units(units, fill=fill, nfill=NFILL)
                mv = mix_d[:, h * 64:(h + 1) * 64].rearrange("(qt p) e -> p qt e", p=128)
                for q4 in range(4):
                    S.dma(View(mix_d.at(("c", h, q4)), mv.ap[:, q4 * 8:(q4 + 1) * 8, :]), mo[hk][:, q4 * 8:(q4 + 1) * 8, :])
            S.flush()

    def phase_nsa(self, L):
        S, d = self.S, self.d
        qk_d, vt_d, mix_d = d["qk"], d["vt"], d["mix"]
        with ExitStack() as ps:
            ident = self.const(ps, "c_ident", [128, 128], BF16)
            ident4 = self.const(ps, "c_ident4", [128, 512], BF16)
            causal = self.const(ps, "c_causal", [128, 128], BF16)
            winfar = self.const(ps, "c_winfar", [128, 128], BF16)
            e64 = self.const(ps, "c_e64", [64, 32, 128], BF16)
            KsT = self.sb(ps, "KsT", [64, SEQ], BF16)
            KwT = self.sb(ps, "KwT", [64, SEQ], BF16)
            Vs = self.sb(ps, "Vs", [128, NT, 65], BF16)
            Vw = self.sb(ps, "Vw", [128, NT, 65], BF16)
            kcT = self.sb(ps, "kcT", [64, 256], BF16)
            Vc = self.sb(ps, "Vc", [128, 2, 129], BF16)
            qraw = [self.sb(ps, f"qraw{k}", [64, 512], BF16) for k in range(2)]
            qrot = [self.sb(ps, f"qrot{k}", [64, 512], BF16) for k in range(2)]
            cmpb = [self.sb(ps, f"cmpb{k}", [128, 256], BF16) for k in range(2)]
            sadd = [self.sb(ps, f"sadd{k}", [128, 64], F32) for k in range(2)]
            gts = [self.sb(ps, f"gts{k}", [128, 4, 3], F32) for k in range(2)]
            pT = [self.sb(ps, f"pT{k}", [128, 512], BF16) for k in range(3)]
            rcp = [self.sb(ps, f"rcp{k}", [128, 4, 3], F32) for k in range(2)]
            sums = [self.sb(ps, f"sums{k}", [128, 4, 3], F32) for k in range(2)]
            coef = [self.sb(ps, f"coef{k}", [128, 4, 3], F32) for k in range(2)]
            imp = [self.sb(ps, f"imp{k}", [128, 64], F32) for k in range(2)]
            imp3 = [self.sb(ps, f"imp3{k}", [128, 64], F32) for k in range(2)]
            m8a = [self.sb(ps, f"m8a{k}", [128, 8], F32) for k in range(2)]
            m8b = [self.sb(ps, f"m8b{k}", [128, 8], F32) for k in range(2)]
            sbias = [self.sb(ps, f"sbias{k}", [128, 64], BF16) for k in range(2)]
            biasT4 = [self.sb(ps, f"biasT4{k}", [64, 512], BF16) for k in range(2)]
            oo = [self.sb(ps, f"oo{k}", [128, 64], F32) for k in range(2)]
            mo = [self.sb(ps, f"mo{k}", [128, 256], BF16) for k in range(2)]
            bk = self.banks(ps, 8)
            scb = bk[0:3]
            accC = bk[3:5]
            accS, accW = bk[5], bk[6]
            cnt = dict(u=0)
            fill = self.make_fill(ps, bk[7], ident4=ident4)
            for g in range(2):
                S.dma(KsT[:, :], qk_d[1536 + g * 64:1536 + (g + 1) * 64, :])
                S.dma(KwT[:, :], qk_d[1664 + g * 64:1664 + (g + 1) * 64, :])
                S.memset(Vs[:, :, 64:65], 1.0)
                S.memset(Vw[:, :, 64:65], 1.0)
                S.memset(Vc[:, :, 64:65], 1.0)
                S.dma(Vs[:, :, 0:64], vt_d[:, 512 + g * 64:512 + (g + 1) * 64].rearrange("(kt p) e -> p kt e", p=128))
                S.dma(Vw[:, :, 0:64], vt_d[:, 640 + g * 64:640 + (g + 1) * 64].rearrange("(kt p) e -> p kt e", p=128))
                S.dma(kcT[:, :], d["kcmpT"][g])
                S.dma(Vc[:, :, 0:64], d["vcmp"][g].rearrange("(nt p) e -> p nt e", p=128))
                S.dma(Vc[:, :, 65:129], d["c_c2s"][:, :].rearrange("(nt p) e -> p nt e", p=128))
                for qt in range(NT):
                    k2 = qt % 2
                    qs = slice(qt * 128, (qt + 1) * 128)
                    S.dma(qraw[k2][:, :].rearrange("d (r q) -> d r q", r=4),
                          d["qraw"][g * 256:(g + 1) * 256, qs].rearrange("(r d) q -> d r q", d=64))
                    S.dma(qrot[k2][:, :].rearrange("d (r q) -> d r q", r=4),
                          qk_d[1024 + g * 256:1024 + (g + 1) * 256, qs].rearrange("(r d) q -> d r q", d=64))
                    S.dma(cmpb[k2][:, :], d["c_cmpb"][qt])
                    S.dma(sadd[k2][:, :], d["c_seladd"][qt])
                    S.dma(gts[k2][:, :, :], d["gates"][qs, g * 12:(g + 1) * 12].rearrange("p (r b) -> p r b", b=3))
                    def mk(kind, kt, k2=k2, qt=qt):
                        uu = cnt["u"]
                        cnt["u"] += 1
                        sc = scb[uu % 3]
                        p = pT[uu % 3]
                        ks = slice(kt * 128, (kt + 1) * 128)

                        def score():
                            if kind == "c":
                                S.matmul(sc[:, :], lhsT=kcT[:, ks], rhs=qraw[k2][:, :], start=True, stop=False)
                                S.matmul(sc[:, :], lhsT=cmpb[k2][:, ks], rhs=ident4[:, :], start=False, stop=True)
                            elif kind == "s":
                                S.matmul(sc[:, :], lhsT=KsT[:, ks], rhs=qrot[k2][:, :], start=True, stop=False)
                                S.matmul(sc[:, :], lhsT=e64[:, kt, :], rhs=biasT4[k2][:, :], start=False, stop=(kt != qt))
                                if kt == qt:
                                    S.matmul(sc[:, :], lhsT=causal[:, :], rhs=ident4[:, :], start=False, stop=True)
                            else:
                                edge = (kt == qt) or (kt == qt - 4)
                                S.matmul(sc[:, :], lhsT=KwT[:, ks], rhs=qrot[k2][:, :], start=True, stop=not edge)
                                if kt == qt:
                                    S.matmul(sc[:, :], lhsT=causal[:, :], rhs=ident4[:, :], start=False, stop=True)
                                elif kt == qt - 4:
                                    S.matmul(sc[:, :], lhsT=winfar[:, :], rhs=ident4[:, :], start=False, stop=True)
                            S.act(p[:, :], sc[:, :], AF.Exp, scale=SCALE)

                        def pv():
                            if kind == "c":
                                for r in range(4):
                                    o0 = (r % 2) * 129
                                    S.matmul(accC[r // 2][:, o0:o0 + 129], lhsT=p[:, r * 128:(r + 1) * 128], rhs=Vc[:, kt, :],
                                             start=(kt == 0 and r % 2 == 0), stop=(kt == 1), skip=True)
                            elif kind == "s":
                                for r in range(4):
                                    S.matmul(accS[:, r * 65:(r + 1) * 65], lhsT=p[:, r * 128:(r + 1) * 128], rhs=Vs[:, kt, :],
                                             start=(kt == 0 and r == 0), stop=(kt == qt), skip=True)
                            else:
                                k0 = max(0, qt - 4)
                                for r in range(4):
                                    S.matmul(accW[:, r * 65:(r + 1) * 65], lhsT=p[:, r * 128:(r + 1) * 128], rhs=Vw[:, kt, :],
                                             start=(kt == k0 and r == 0), stop=(kt == qt), skip=True)
                        return (score, pv)

                    units = [mk("c", 0), mk("c", 1)] + [mk("w", kt) for kt in range(max(0, qt - 4), qt + 1)]
                    self.run_units(units, fill=fill, nfill=NFILL)
                    for r in range(4):
                        o0 = (r % 2) * 129
                        S.ts(sums[k2][:, r, 0:1], accC[r // 2][:, o0 + 64:o0 + 65], 1e-30, None, ALU.max)
                    S.recip(rcp[k2][:, :, 0:1], sums[k2][:, :, 0:1])
                    for r in range(4):
                        o0 = (r % 2) * 129
                        iu = accC[r // 2][:, o0 + 65:o0 + 129]
                        if r == 0:
                            S.ts(imp[k2][:, :], iu, rcp[k2][:, 0, 0:1], None, ALU.mult)
                        else:
                            S.stt(imp[k2][:, :], iu, rcp[k2][:, r, 0:1], imp[k2][:, :], ALU.mult, ALU.add)
                    S.tt(imp[k2][:, :], imp[k2][:, :], sadd[k2][:, :], ALU.add)
                    S.max8(m8a[k2][:, :], imp[k2][:, :])
                    S.match_replace(imp3[k2][:, :], m8a[k2][:, :], imp[k2][:, :], -3.0e38)
                    S.max8(m8b[k2][:, :], imp3[k2][:, :])
                    S.ts(sbias[k2][:, :], imp[k2][:, :], m8b[k2][:, 7:8], NEGB, ALU.is_lt, ALU.mult)
                    tp = scb[cnt["u"] % 3]
                    cnt["u"] += 1
                    S.matmul(tp[0:64, 0:128], lhsT=sbias[k2][:, :], rhs=ident[:, :], start=True, stop=True, skip=True)
                    for r in range(4):
                        S.copy(biasT4[k2][:, r * 128:(r + 1) * 128], tp[0:64, 0:128], eng="scalar" if r % 2 == 0 else "vector")
                    self.run_units([mk("s", kt) for kt in range(qt + 1)], fill=fill, nfill=NFILL)
                    s3 = accS[:, 0:260].rearrange("p (r e) -> p r e", e=65)
                    w3 = accW[:, 0:260].rearrange("p (r e) -> p r e", e=65)
                    S.ts(sums[k2][:, :, 1:2], s3[:, :, 64:65], 1e-30, None, ALU.max)
                    S.ts(sums[k2][:, :, 2:3], w3[:, :, 64:65], 1e-30, None, ALU.max)
                    S.recip(rcp[k2][:, :, 1:3], sums[k2][:, :, 1:3])
                    S.tt(coef[k2][:, :, :], gts[k2][:, :, :], rcp[k2][:, :, :], ALU.mult)
                    for r in range(4):
                        o0 = (r % 2) * 129
                        o = oo[r % 2]
                        S.ts(o[:, :], accC[r // 2][:, o0:o0 + 64], coef[k2][:, r, 0:1], None, ALU.mult)
                        S.stt(o[:, :], accS[:, r * 65:r * 65 + 64], coef[k2][:, r, 1:2], o[:, :], ALU.mult, ALU.add)
                        S.stt(mo[k2][:, r * 64:(r + 1) * 64], accW[:, r * 65:r * 65 + 64], coef[k2][:, r, 2:3], o[:, :],
                              ALU.mult, ALU.add)
                    S.dma(mix_d.at(("d", g, qt))[qs, 512 + g * 256:512 + (g + 1) * 256], mo[k2][:, :])
            S.flush()

    def phase_moe(self, L):
        S, d = self.S, self.d
        i = L // 2
        NF = F_EXPERT // 128
        final = L == DEPTH - 1
        dst = d["out"] if final else d["xres"]
        with ExitStack() as ps:
            r = self.ln_alloc(ps, L, "ffn", nbuf=1)
            xTc = [self.sb(ps, f"xTc{k}", [128, 8, 512], BF16) for k in range(2)]
            Gc = [self.sb(ps, f"Gc{k}", [128, 4, 8], F32) for k in range(2)]
            Wg = [self.sb(ps, f"Wg{k}", [128, 8, 256], BF16) for k in range(2)]
            Wu = [self.sb(ps, f"Wu{k}", [128, 8, 256], BF16) for k in range(2)]
            Wdh = [self.sb(ps, f"Wdh{k}", [128, NF, 512], BF16) for k in range(2)]
            hT = self.sb(ps, "hT", [128, NF, 512], BF16)
            sg = [self.sb(ps, f"sg{k}", [128, 512], F32) for k in range(2)]
            acc = self.sb(ps, "acc", [128, 4, D], F32)
            bk = self.banks(ps, 8)
            g_all = d[f"b_mg{i}"]
            u_all = d[f"b_mu{i}"]
            d_all = d[f"b_md{i}"]
            xT_v = d["xT"][:, :].rearrange("(c p) t -> p c t", p=128)
            u = 0
            w = 0
            wd = 0
            for tc in range(8):
                xc = xTc[tc % 2]
                gc = Gc[tc % 2]
                S.dma(xc[:, :, :], View(d["xT"].at(("c", tc)), xT_v.ap[:, :, tc * 512:(tc + 1) * 512]))
                S.dma(gc[:, :, :], d["moeg"][tc * 512:(tc + 1) * 512, :].rearrange("(t p) e -> p t e", p=128))
                for e in range(N_EXPERTS):
                    g_v = None
                    u_v = None
                    d_v = d_all[e * F_EXPERT:(e + 1) * F_EXPERT, :].rearrange("(f p) n -> p f n", p=128)
                    for fg in range(NF // 2):
                        wg, wu = Wg[w % 2], Wu[w % 2]
                        w += 1
                        S.dma(wg[:, :, :], g_all[e * (NF // 2) + fg].rearrange("p (c j) -> p c j", c=8))
                        S.dma(wu[:, :, :], u_all[e * (NF // 2) + fg].rearrange("p (c j) -> p c j", c=8))
                        for f2 in range(2):
                            ft = fg * 2 + f2
                            pg, pu = bk[(2 * u) % 4], bk[(2 * u + 1) % 4]
                            for c in range(8):
                                S.matmul(pg[:, :], lhsT=wg[:, c, f2 * 128:(f2 + 1) * 128], rhs=xc[:, c, :], start=(c == 0), stop=(c == 7))
                            for c in range(8):
                                S.matmul(pu[:, :], lhsT=wu[:, c, f2 * 128:(f2 + 1) * 128], rhs=xc[:, c, :], start=(c == 0), stop=(c == 7))
                            S.act(sg[u % 2][:, :], pg[:, :], AF.Silu)
                            S.tt(View(hT.at(ft), hT.t[:, ft, :]), sg[u % 2][:, :], pu[:, :], ALU.mult)
                            u += 1
                    for hh in range(2):
                        wdh = Wdh[wd % 2]
                        wd += 1
                        for f0 in range(0, NF, 7):
                            S.dma(View(wdh.at(f0), wdh.t[:, f0:f0 + 7, :]), d_v[:, f0:f0 + 7, hh * 512:(hh + 1) * 512])
                        for t4 in range(4):
                            py = bk[4 + (t4 % 2)]
                            for ft in range(NF):
                                S.matmul(py[:, :], lhsT=View(hT.at(ft), hT.t[:, ft, t4 * 128:(t4 + 1) * 128]),
                                         rhs=View(wdh.at((ft // 7) * 7), wdh.t[:, ft, :]), start=(ft == 0), stop=(ft == NF - 1))
                            av = View(acc.at((t4, hh)), acc.t[:, t4, hh * 512:(hh + 1) * 512])
                            if e == 0:
                                S.ts(av, py[:, :], gc[:, t4, e:e + 1], None, ALU.mult)
                            else:
                                S.stt(av, py[:, :], gc[:, t4, e:e + 1], av, ALU.mult, ALU.add)
                for t4 in range(4):
                    t = tc * 4 + t4
                    yv = [View(acc.at((t4, hh)), acc.t[:, t4, hh * 512:(hh + 1) * 512]) for hh in range(2)]
                    self.ln_tile(r, t, yv, d["xres"], dst, bk[6:8], want_xT=not final)
            S.flush()

    def phase_moe_routed(self, L):
        S, d = self.S, self.d
        i = L // 2
        NF = F_EXPERT // 128
        CAP = MOE_CAP
        NCH = CAP // 512
        ROWS = 8 * CAP
        U32 = mybir.dt.uint32
        final = L == DEPTH - 1
        dst = d["out"] if final else d["xres"]
        xs_d, ys_d = d["xs"], d["ys"]
        with ExitStack() as ps:
            r = self.ln_alloc(ps, L, "ffn", nbuf=1)
            ident = self.const(ps, "c_ident", [128, 128], BF16)
            tri = self.const(ps, "c_tri", [128, 128], BF16)
            ones = self.const(ps, "c_ones", [128, 128], BF16)
            ebase = self.const(ps, "c_ebase", [128, 32, 8], F32)
            bk = self.banks(ps, 8)
            m1 = self.sb(ps, "m1", [128, 32, 8], F32)
            m2 = self.sb(ps, "m2", [128, 32, 8], F32)
            gv = self.sb(ps, "gv", [128, 32, 2], F32)
            S.dma(m1[:, :, :], d["moem1"][:, :].rearrange("(t p) e -> p t e", p=128))
            S.dma(m2[:, :, :], d["moem2"][:, :].rearrange("(t p) e -> p t e", p=128))
            S.dma(gv[:, :, :], d["moegv"][:, :].rearrange("(t p) e -> p t e", p=128))
            selb = self.sb(ps, "selb", [128, 256], BF16)
            S.tt(selb[:, :].rearrange("p (t e) -> p t e", e=8), m1[:, :, :], m2[:, :, :], ALU.add)
            S.matmul(bk[0][:, 0:256], lhsT=tri[:, :], rhs=selb[:, :], start=True, stop=True)
            S.matmul(bk[1][:, 0:256], lhsT=ones[:, :], rhs=selb[:, :], start=True, stop=True)
            tot = self.sb(ps, "tot", [128, 32, 8], F32)
            S.copy(tot[:, :, :], bk[1][:, 0:256].rearrange("p (t e) -> p t e", e=8))
            off = self.sb(ps, "off", [128, 32, 8], F32)
            S.memset(off[:, 0, :], 0.0)
            for t in range(1, 32):
                S.tt(off[:, t, :], off[:, t - 1, :], tot[:, t - 1, :], ALU.add)
            pos = self.sb(ps, "pos", [128, 32, 8], F32)
            S.tt(pos[:, :, :], off[:, :, :], bk[0][:, 0:256].rearrange("p (t e) -> p t e", e=8), ALU.add)
            slot = self.sb(ps, "slot", [128, 32, 8], F32)
            S.tt(slot[:, :, :], pos[:, :, :], ebase[:, :, :], ALU.add)
            S.ts(pos[:, :, :], pos[:, :, :], float(CAP), 1.0e6, ALU.is_ge, ALU.mult)
            S.tt(slot[:, :, :], slot[:, :, :], pos[:, :, :], ALU.add)
            sl_f = self.sb(ps, "sl_f", [128, 2, 32], F32)
            tmp = self.sb(ps, "rtmp", [128, 32, 8], F32)
            S.tt(tmp[:, :, :], slot[:, :, :], m1[:, :, :], ALU.mult)
            S.reduce(sl_f[:, 0, :], tmp[:, :, :], ALU.add)
            S.tt(tmp[:, :, :], slot[:, :, :], m2[:, :, :], ALU.mult)
            S.reduce(sl_f[:, 1, :], tmp[:, :, :], ALU.add)
            sl_i = self.sb(ps, "sl_i", [128, 2, 32], U32)
            S.copy(sl_i[:, :, :], sl_f[:, :, :])
            okf = self.sb(ps, "okf", [128, 2, 32], F32)
            S.ts(okf[:, :, :], sl_f[:, :, :], 1.0e5, None, ALU.is_lt)
            geff = self.sb(ps, "geff", [128, 2, 32], F32)
            S.tt(geff[:, 0, :], gv[:, :, 0], okf[:, 0, :], ALU.mult)
            S.tt(geff[:, 1, :], gv[:, :, 1], okf[:, 1, :], ALU.mult)
            zt = self.sb(ps, "zt", [128, D], BF16)
            S.memset(zt[:, :], 0.0)
            for r0 in range(0, ROWS, 128):
                S.dma(xs_d.at(("z", r0))[r0:r0 + 128, :], zt[:, :])
            S.flush()
            KB._uid += 1
            breg = ps.enter_context(self.nc.gpsimd.register(f"moe_bound{KB._uid}"))
            S.op("gpsimd", lambda e: e.reg_mov(breg, ROWS - 1))
            xf = [self.sb(ps, f"xf{k}", [128, D], F32) for k in range(2)]
            xb = [self.sb(ps, f"xb{k}", [128, D], BF16) for k in range(2)]
            for t in range(NT):
                k = t % 2
                tk = slice(t * 128, (t + 1) * 128)
                S.dma(xf[k][:, :], View(d["xres"].at(t), d["xres"].t[tk, :]))
                S.copy(xb[k][:, :], xf[k][:, :], eng="scalar")
                for j in range(2):
                    idx_ap = sl_i.t[:, j, t:t + 1]
                    o_ap, i_ap = xs_d.t[:, :], xb[k].t[:, :]
                    S.dma_op("gpsimd",
                             lambda e, o_ap=o_ap, i_ap=i_ap, idx_ap=idx_ap: e.indirect_dma_start(
                                 out=o_ap, out_offset=bass.IndirectOffsetOnAxis(ap=idx_ap, axis=0), in_=i_ap, in_offset=None,
                                 bounds_check=breg, oob_is_err=False),
                             reads=[xb[k], sl_i], writes=[xs_d.at(("sc", t, j))])
            S.flush()
            xtm = self.sb(ps, "xtm", [128, 4, D], BF16)
            xTc = self.sb(ps, "xTc", [128, 8, 512], BF16)
            Wg = [self.sb(ps, f"Wg{k}", [128, 8, 256], BF16) for k in range(2)]
            Wu = [self.sb(ps, f"Wu{k}", [128, 8, 256], BF16) for k in range(2)]
            Wdh = [self.sb(ps, f"Wdh{k}", [128, NF, 512], BF16) for k in range(2)]
            hT = self.sb(ps, "hT", [128, NF, 512], BF16)
            sg = [self.sb(ps, f"sg{k}", [128, 512], F32) for k in range(2)]
            yo = [self.sb(ps, f"yo{k}", [128, 512], F32) for k in range(2)]
            g_all, u_all, d_all = d[f"b_mg{i}"], d[f"b_mu{i}"], d[f"b_md{i}"]
            u = 0
            w = 0
            wd = 0
            yy = 0
            for e in range(N_EXPERTS):
                g_v = None
                u_v = None
                d_v = d_all[e * F_EXPERT:(e + 1) * F_EXPERT, :].rearrange("(f p) n -> p f n", p=128)
                for c in range(NCH):
                    r0 = e * CAP + c * 512
                    S.dma(xtm[:, :, :], View(xs_d.at(("ld", r0)), xs_d.t[r0:r0 + 512, :].rearrange("(s p) n -> p s n", p=128)))
                    for s4 in range(4):
                        for hb in range(2):
                            tb = bk[6 + hb]
                            for cc in range(4):
                                dc = hb * 4 + cc
                                S.matmul(tb[:, cc * 128:(cc + 1) * 128], lhsT=xtm[:, s4, dc * 128:(dc + 1) * 128], rhs=ident[:, :],
                                         start=True, stop=True, skip=True)
                            S.copy(xTc[:, hb * 4:(hb + 1) * 4, s4 * 128:(s4 + 1) * 128],
                                   tb[:, :].rearrange("p (c t) -> p c t", c=4), eng="scalar" if hb == 0 else "vector")
                    for fg in range(NF // 2):
                        wg, wu = Wg[w % 2], Wu[w % 2]
                        w += 1
                        S.dma(wg[:, :, :], g_all[e * (NF // 2) + fg].rearrange("p (c j) -> p c j", c=8))
                        S.dma(wu[:, :, :], u_all[e * (NF // 2) + fg].rearrange("p (c j) -> p c j", c=8))
                        for f2 in range(2):
                            ft = fg * 2 + f2
                            pg, pu = bk[(2 * u) % 4], bk[(2 * u + 1) % 4]
                            for cI in range(8):
                                S.matmul(pg[:, :], lhsT=wg[:, cI, f2 * 128:(f2 + 1) * 128], rhs=xTc[:, cI, :], start=(cI == 0), stop=(cI == 7))
                            for cI in range(8):
                                S.matmul(pu[:, :], lhsT=wu[:, cI, f2 * 128:(f2 + 1) * 128], rhs=xTc[:, cI, :], start=(cI == 0), stop=(cI == 7))
                            S.act(sg[u % 2][:, :], pg[:, :], AF.Silu)
                            S.tt(View(hT.at(ft), hT.t[:, ft, :]), sg[u % 2][:, :], pu[:, :], ALU.mult)
                            u += 1
                    for hh in range(2):
                        wdh = Wdh[wd % 2]
                        wd += 1
                        for f0 in range(0, NF, 7):
                            S.dma(View(wdh.at(f0), wdh.t[:, f0:f0 + 7, :]), d_v[:, f0:f0 + 7, hh * 512:(hh + 1) * 512])
                        for t4 in range(4):
                            py = bk[4 + (t4 % 2)]
                            for ft in range(NF):
                                S.matmul(py[:, :], lhsT=View(hT.at(ft), hT.t[:, ft, t4 * 128:(t4 + 1) * 128]),
                                         rhs=View(wdh.at((ft // 7) * 7), wdh.t[:, ft, :]), start=(ft == 0), stop=(ft == NF - 1))
                            y = yo[yy % 2]
                            yy += 1
                            S.copy(y[:, :], py[:, :], eng="scalar" if t4 % 2 == 0 else "vector")
                            rr0 = r0 + t4 * 128
                            S.dma(View(ys_d.at((rr0, hh)), ys_d.t[rr0:rr0 + 128, hh * 512:(hh + 1) * 512]), y[:, :])
            S.flush()
            y1 = self.sb(ps, "y1", [128, D], F32)
            y2 = self.sb(ps, "y2", [128, D], F32)
            acc = self.sb(ps, "acc", [128, D], F32)
            S.memset(y1[:, :], 0.0)
            S.memset(y2[:, :], 0.0)
            S.op("gpsimd", lambda e: e.reg_mov(breg, ROWS - 1))
            for t in range(NT):
                for j, yb in enumerate((y1, y2)):
                    idx_ap = sl_i.t[:, j, t:t + 1]
                    o_ap, i_ap = yb.t[:, :], ys_d.t[:, :]
                    S.dma_op("gpsimd",
                             lambda e, o_ap=o_ap, i_ap=i_ap, idx_ap=idx_ap: e.indirect_dma_start(
                                 out=o_ap, out_offset=None, in_=i_ap, in_offset=bass.IndirectOffsetOnAxis(ap=idx_ap, axis=0),
                                 bounds_check=breg, oob_is_err=False),
                             reads=[sl_i], writes=[yb])
                S.ts(acc[:, :], y1[:, :], geff[:, 0, t:t + 1], None, ALU.mult)
                S.stt(acc[:, :], y2[:, :], geff[:, 1, t:t + 1], acc[:, :], ALU.mult, ALU.add)
                self.ln_tile(r, t, [acc[:, 0:512], acc[:, 512:1024]], d["xres"], dst, bk[6:8], want_xT=not final)
            S.flush()


CONST_SPECS = dict(
    c_cos=([128, SEQ], F32), c_sin=([128, SEQ], F32), c_ident=([128, 128], BF16), c_ident4=([128, 512], BF16),
    c_identf=([128, 128], F32), c_causal=([128, 128], BF16), c_causalf=([128, 128], F32), c_winfar=([128, 128], BF16),
    c_e16=([16, 16, 128], BF16), c_e64=([64, 32, 128], BF16), c_cmpb=([32, 128, 256], BF16),
    c_seladd=([32, 128, 64], F32), c_c2s=([256, 64], BF16), c_pow2=([128, 24], F32),
    c_tri=([128, 128], BF16), c_ones=([128, 128], BF16), c_ebase=([128, 32, 8], F32),
)

PARAM_SPECS = dict(
    ev_w_out=[2, 1024, 1024], dif_lambda=[2, 4, 64], dif_subln=[2, 128],
    ffd_w_gate=[2, 1024, F_DENSE], ffd_w_up=[2, 1024, F_DENSE], ffd_w_down=[2, F_DENSE, 1024],
    od_w_out=[2, 1024, 1024], nsa_gate_b=[2, 24], nsa_phi_w1=[2, 2, 2048, 256], nsa_phi_w2=[2, 2, 256, 64],
    moe_w_router=[2, 1024, 8], moe_b_router=[2, 8],
    moe_w_gate=[2, 8, 1024, F_EXPERT], moe_w_up=[2, 8, 1024, F_EXPERT], moe_w_down=[2, 8, F_EXPERT, 1024],
    ln_mix_g=[4, 1024], ln_mix_b=[4, 1024], ln_ffn_g=[4, 1024], ln_ffn_b=[4, 1024],
)
LAYOUT_SPECS = {}
for _i in range(2):
    LAYOUT_SPECS[f"ev_fm{_i}"] = [1024, EV_FM]
    LAYOUT_SPECS[f"ev_fmp{_i}"] = [1024, EV_FM]
    LAYOUT_SPECS[f"ev_tm{_i}"] = [1024, EV_TM]
    LAYOUT_SPECS[f"od_fm{_i}"] = [1024, OD_FM]
    LAYOUT_SPECS[f"od_fmp{_i}"] = [1024, OD_FMR]
    LAYOUT_SPECS[f"od_tm{_i}"] = [1024, OD_TM]
    LAYOUT_SPECS[f"nsa_peT{_i}"] = [2, 64, 32]


def build_program(dbg=(), phases=None):
    nc = bass.Bass("TRN2", target_bir_lowering=False)
    st = ExitStack()
    kb = KB(nc, st, dbg)
    d = kb.d
    kb.din("x", [SEQ, D], F32)
    for k, (shape, dt) in CONST_SPECS.items():
        kb.din(k, shape, dt)
    for k, shape in PARAM_SPECS.items():
        kb.din(k, shape, F32)
    for k, shape in LAYOUT_SPECS.items():
        kb.din(k, shape, F32)
    kb.dscr("out", [SEQ, D], F32, out=True)
    kb.dscr("xres", [SEQ, D], F32)
    kb.dscr("xT", [D, SEQ], BF16)
    kb.dscr("qk", [2048, SEQ], BF16)
    kb.dscr("qraw", [512, SEQ], BF16)
    kb.dscr("vt", [SEQ, 768], BF16)
    kb.dscr("iw", [SEQ, 4], F32)
    kb.dscr("gates", [SEQ, 24], F32)
    kb.dscr("kmean", [512, 16], BF16)
    kb.dscr("mix", [SEQ, D], BF16)
    kb.dscr("moeg", [SEQ, 8], F32)
    kb.dscr("moem1", [SEQ, 8], F32)
    kb.dscr("moem2", [SEQ, 8], F32)
    kb.dscr("moegv", [SEQ, 2], F32)
    kb.dscr("xs", [8 * MOE_CAP, D], BF16)
    kb.dscr("ys", [8 * MOE_CAP, D], F32)
    if "dbg_lo" in kb.dbg:
        kb.dscr("dbg_lo", [SEQ, 4], F32)
    kb.dscr("kcmpT", [2, 64, 256], BF16)
    kb.dscr("vcmp", [2, 256, 64], BF16)
    for i in range(2):
        for k in ("ev_fm", "ev_fmp", "ev_tm", "od_fm", "od_fmp", "od_tm"):
            kb.dscr(f"b_{k}{i}", LAYOUT_SPECS[f"{k}{i}"], BF16)
        kb.dscr(f"b_ev_wout{i}", [1024, 1024], BF16)
        kb.dscr(f"b_od_wout{i}", [1024, 1024], BF16)
        kb.dscr(f"b_ffg{i}", [F_DENSE // 256, 128, 8 * 256], BF16)
        kb.dscr(f"b_ffu{i}", [F_DENSE // 256, 128, 8 * 256], BF16)
        kb.dscr(f"b_ffd{i}", [F_DENSE, 1024], BF16)
        kb.dscr(f"b_phi1_{i}", [2 * 2048, 256], BF16)
        kb.dscr(f"b_phi2_{i}", [2 * 256, 64], BF16)
        kb.dscr(f"b_mg{i}", [8 * (F_EXPERT // 256), 128, 8 * 256], BF16)
        kb.dscr(f"b_mu{i}", [8 * (F_EXPERT // 256), 128, 8 * 256], BF16)
        kb.dscr(f"b_md{i}", [8 * F_EXPERT, 1024], BF16)

    def want(name):
        return phases is None or name in phases

    S = kb.S
    for L in range(DEPTH):
        i = L // 2
        if L % 2 == 0:
            for k, cols in (("ev_fm", EV_FM), ("ev_fmp", EV_FM), ("ev_tm", EV_TM)):
                kb.conv_add(f"b_{k}{i}", d[f"{k}{i}"][:, :], 1024, cols)
            kb.conv_add(f"b_ev_wout{i}", d["ev_w_out"][i], 1024, 1024)
            kb.conv_add(f"b_ffg{i}", d["ffd_w_gate"][i], 1024, F_DENSE, tiled=True, dst_view=d[f"b_ffg{i}"])
            kb.conv_add(f"b_ffu{i}", d["ffd_w_up"][i], 1024, F_DENSE, tiled=True, dst_view=d[f"b_ffu{i}"])
            kb.conv_add(f"b_ffd{i}", d["ffd_w_down"][i], F_DENSE, 1024)
        else:
            for k, cols in (("od_fm", OD_FM), ("od_fmp", OD_FMR), ("od_tm", OD_TM)):
                kb.conv_add(f"b_{k}{i}", d[f"{k}{i}"][:, :], 1024, cols)
            kb.conv_add(f"b_phi1_{i}", d["nsa_phi_w1"][i].rearrange("a r c -> (a r) c"), 4096, 256)
            kb.conv_add(f"b_phi2_{i}", d["nsa_phi_w2"][i].rearrange("a r c -> (a r) c"), 512, 64)
            kb.conv_add(f"b_od_wout{i}", d["od_w_out"][i], 1024, 1024)
            NG_E = F_EXPERT // 256
            for e in range(8):
                for nm, src in (("b_mg", "moe_w_gate"), ("b_mu", "moe_w_up")):
                    sub = Buf(d[f"{nm}{i}"].t[e * NG_E:(e + 1) * NG_E], f"{nm}{i}_e{e}")
                    kb.conv_add(f"{nm}{i}" if e == 7 else f"{nm}{i}_part{e}", d[src][i][e], 1024, F_EXPERT,
                                tiled=True, dst_view=sub)
            kb.conv_add(f"b_md{i}", d["moe_w_down"][i].rearrange("e r c -> (e r) c"), 8 * F_EXPERT, 1024)
    if phases is not None:
        kb.conv_budget(1 << 60)
        S.flush()
    MB = 1 << 20
    NEED = {
        "proj_e": lambda i: [f"b_ev_fm{i}", f"b_ev_fmp{i}", f"b_ev_tm{i}"],
        "proj_o": lambda i: [f"b_od_fm{i}", f"b_od_fmp{i}", f"b_od_tm{i}"],
    }
    def run_phase(name, fn, budget_mb, needs=()):
        if not want(name):
            return
        kb.conv_ensure(needs)
        kb.conv_budget(budget_mb * MB)
        fn()

    if phases is None:
        kb.conv_ensure(NEED["proj_e"](0))
    run_phase("prologue", lambda: kb.phase_prologue(d["x"], d["xT"]), 120)
    for L in range(DEPTH):
        i = L // 2
        if L % 2 == 0:
            run_phase(f"proj{L}", lambda: kb.phase_proj(L), 0, NEED["proj_e"](i))
            run_phase(f"diff{L}", lambda: kb.phase_diff(L), 0)
            run_phase(f"dsa{L}", lambda: kb.phase_dsa(L), 450)
            run_phase(f"outproj{L}", lambda: kb.phase_outproj(L), 0, [f"b_ev_wout{i}"])
            run_phase(f"ffn{L}", lambda: kb.phase_ffn_dense(L), 0, [f"b_ffg{i}", f"b_ffu{i}", f"b_ffd{i}"])
        else:
            run_phase(f"proj{L}", lambda: kb.phase_proj(L), 0, NEED["proj_o"](i))
            run_phase(f"cmp{L}", lambda: kb.phase_cmp(L), 0, [f"b_phi1_{i}", f"b_phi2_{i}"])
            run_phase(f"moba{L}", lambda: kb.phase_moba(L), 0)
            run_phase(f"nsa{L}", lambda: kb.phase_nsa(L), 0)
            run_phase(f"outproj{L}", lambda: kb.phase_outproj(L), 0, [f"b_od_wout{i}"])
            moe_fn = (lambda: kb.phase_moe_routed(L)) if MOE_ROUTED else (lambda: kb.phase_moe(L))
            run_phase(f"moe{L}", moe_fn, 0, [f"b_mg{i}", f"b_mu{i}", f"b_md{i}"])
    st.close()
    return nc, kb


_PROGRAM = None


def kernel(**inputs):
    global _PROGRAM
    inp = {k: np.asarray(v) for k, v in inputs.items()}
    B = inp["x"].shape[0]
    assert inp["x"].shape == (8, SEQ, D)
    if _PROGRAM is None:
        _PROGRAM = build_program()[0]
    nc = _PROGRAM
    shared = {}
    shared.update(make_constants())
    for k, shape in PARAM_SPECS.items():
        shared[k] = np.ascontiguousarray(inp[k], dtype=np.float32).reshape(shape)
    shared.update(layout_weights(inp))
    in_maps = []
    for b in range(B):
        m = dict(shared)
        m["x"] = np.ascontiguousarray(inp["x"][b], dtype=np.float32)
        in_maps.append(m)
    res = run_bass_kernel_spmd(nc, in_maps, core_ids=list(range(B)))
    out = np.stack([np.asarray(r["out"], dtype=np.float32) for r in res.results], axis=0)
    return out
```

```python
import math
from contextlib import ExitStack

import numpy as np
import ml_dtypes

import concourse.bass as bass
import concourse.mybir as mybir
from concourse.bass_utils import run_bass_kernel_spmd

F32 = mybir.dt.float32
BF16 = mybir.dt.bfloat16
ALU = mybir.AluOpType
AF = mybir.ActivationFunctionType
AX = mybir.AxisListType

D = 1024
SEQ = 4096
DEPTH = 4
NT = SEQ // 128
HD = 64
LN_EPS = 1e-5
DN_ALPHA = (2 * DEPTH) ** 0.25
F_DENSE = 2816
N_EXPERTS = 8
F_EXPERT = 3584
MOE_CAP = 1536
NEGB = -30000.0
SCALE = HD ** -0.5

EV_SIZES = dict(aq=512, ak=512, av=512, bq=512, bk=128, bv=128, iq=256, ik=64, iw=4)
OD_SIZES = dict(cq=512, ck=512, cv=512, dq=512, dkc=128, dvc=128, dks=128, dvs=128, dkw=128, dvw=128, dg=24)


def _offsets(sizes):
    off, o = {}, 0
    for k, v in sizes.items():
        off[k] = o
        o += v
    return off


EV_OFF = _offsets(EV_SIZES)
OD_OFF = _offsets(OD_SIZES)

ENGS = ("tensor", "vector", "scalar", "gpsimd", "sync")
EPOCH = 60000
NDMASEM = 40
NSWSEM = 12
RELAX_SAME_ENGINE = False
NPROG = 10


class View:
    __slots__ = ("buf", "ap")

    def __init__(self, buf, ap):
        self.buf = buf
        self.ap = ap

    def __getitem__(self, idx):
        return View(self.buf, self.ap[idx])

    def rearrange(self, s, **kw):
        return View(self.buf, self.ap.rearrange(s, **kw))

    def bitcast(self, dt):
        return View(self.buf, self.ap.bitcast(dt))

    def broadcast_to(self, shape):
        return View(self.buf, self.ap.broadcast_to(shape))

    def partition_broadcast(self, n):
        return View(self.buf, self.ap.partition_broadcast(n))


class Buf:
    def __init__(self, t, name=""):
        self.t = t
        self.w = None
        self.r = []
        self.name = name
        self.kids = {}
        self.psum = False

    def __getitem__(self, idx):
        return View(self, self.t[idx])

    def at(self, key):
        k = self.kids.get(key)
        if k is None:
            k = Buf(self.t, f"{self.name}.{key}")
            self.kids[key] = k
        return k


class Op:
    __slots__ = ("eng", "fn", "deps", "signal", "is_dma", "dsem", "dval", "sigval", "sigsem", "gen")

    def __init__(self, eng, fn, is_dma=False):
        self.eng = eng
        self.fn = fn
        self.deps = []
        self.signal = False
        self.is_dma = is_dma
        self.dsem = None
        self.dval = 0
        self.sigval = 0
        self.sigsem = None
        self.gen = 0


class Sched:
    def __init__(self, nc, st, same_engine_sync=True):
        self.nc = nc
        self.same_engine_sync = same_engine_sync
        self.ops = {e: [] for e in ENGS}
        self.dma_rr = 0
        self.dma_last = [None] * NDMASEM
        self.dma_cnt = [0] * NDMASEM
        self.sig_cnt = {e: 0 for e in ENGS}
        self.psems = {e: [st.enter_context(nc.semaphore(f"p_{e}_{k}")) for k in range(NPROG)] for e in ENGS}
        self.dsems = [st.enter_context(nc.semaphore(f"d_{k}")) for k in range(NDMASEM)]
        self.final_ops = []
        self.n_ops = 0
        self.gen = 0

    def _track(self, op, reads, writes):
        deps = []
        for b in reads:
            if b.w is not None:
                deps.append((b.w, True))
            if b.psum:
                deps.extend((r, False) for r in b.r if r.eng != op.eng)
        for b in writes:
            if b.w is not None:
                deps.append((b.w, False))
            deps.extend((r, False) for r in b.r)
        seen = {}
        for d, raw in deps:
            if d is op:
                continue
            if RELAX_SAME_ENGINE and (not raw) and (not d.is_dma) and (not op.is_dma) and d.eng == op.eng:
                continue
            if id(d) in seen:
                continue
            seen[id(d)] = True
            op.deps.append(d)
        for b in reads:
            b.r.append(op)
        for b in writes:
            b.w = op
            b.r = []

    def op(self, eng, fn, reads=(), writes=()):
        o = Op(eng, fn)
        o.gen = self.gen
        self._track(o, reads, writes)
        self.ops[eng].append(o)
        self.n_ops += 1
        return o

    def dma_op(self, eng, fn, reads=(), writes=()):
        o = Op(eng, fn, is_dma=True)
        o.gen = self.gen
        self._track(o, reads, writes)
        if eng == "gpsimd":
            self.dma_rr_sw = (getattr(self, "dma_rr_sw", -1) + 1) % NSWSEM
            k = self.dma_rr_sw
        else:
            self.dma_rr = (self.dma_rr + 1) % (NDMASEM - NSWSEM)
            k = NSWSEM + self.dma_rr
        prev = self.dma_last[k]
        if prev is not None:
            o.deps.append(prev)
        self.dma_cnt[k] += 1
        o.dsem = k
        o.dval = 16 * self.dma_cnt[k]
        self.dma_last[k] = o
        self.ops[eng].append(o)
        self.n_ops += 1
        return o

    @staticmethod
    def _bufs(*views):
        out = []
        for v in views:
            if isinstance(v, View) and v.buf not in out:
                out.append(v.buf)
        return out

    @staticmethod
    def _ap(v):
        return v.ap if isinstance(v, View) else v

    def dma(self, out, in_, eng="sync", final=False, **kw):
        o_ap, i_ap = out.ap, in_.ap
        o = self.dma_op(eng, lambda e: e.dma_start(out=o_ap, in_=i_ap, **kw), reads=[in_.buf], writes=[out.buf])
        if final:
            self.final_ops.append(o)
        return o

    def dma_T(self, out, in_, eng="sync"):
        o_ap, i_ap = out.ap, in_.ap
        return self.dma_op(eng, lambda e: e.dma_start_transpose(out=o_ap, in_=i_ap), reads=[in_.buf], writes=[out.buf])

    def matmul(self, out, lhsT, rhs, start=True, stop=True, skip=False):
        o_ap, l_ap, r_ap = out.ap, lhsT.ap, rhs.ap
        return self.op("tensor",
                       lambda e: e.matmul(o_ap, l_ap, r_ap, start=start, stop=stop, skip_group_check=skip),
                       reads=self._bufs(lhsT, rhs), writes=[out.buf])

    def act(self, out, in_, func, bias=None, scale=1.0, accum_out=None, eng="scalar"):
        o_ap, i_ap = out.ap, in_.ap
        kw = {}
        if bias is not None:
            kw["bias"] = self._ap(bias)
        if accum_out is not None:
            kw["accum_out"] = accum_out.ap
        sc = self._ap(scale)
        writes = self._bufs(out, accum_out)
        return self.op("scalar", lambda e: e.activation(o_ap, i_ap, func, scale=sc, **kw),
                       reads=self._bufs(in_, bias, scale, accum_out), writes=writes)

    def tt(self, out, in0, in1, op, eng="vector"):
        o_ap, a_ap, b_ap = out.ap, in0.ap, in1.ap
        return self.op(eng, lambda e: e.tensor_tensor(o_ap, a_ap, b_ap, op),
                       reads=self._bufs(in0, in1), writes=[out.buf])

    def ts(self, out, in0, s1, s2, op0, op1=None, accum_out=None, eng="vector"):
        o_ap, a_ap = out.ap, in0.ap
        s1a, s2a = self._ap(s1), self._ap(s2)
        kw = {}
        if op1 is not None:
            kw["op1"] = op1
        if accum_out is not None:
            kw["accum_out"] = accum_out.ap
        return self.op(eng, lambda e: e.tensor_scalar(o_ap, a_ap, s1a, s2a, op0, **kw),
                       reads=self._bufs(in0, s1, s2, accum_out), writes=self._bufs(out, accum_out))

    def stt(self, out, in0, scalar, in1, op0, op1, eng="vector"):
        o_ap, a_ap, b_ap = out.ap, in0.ap, in1.ap
        sa = self._ap(scalar)
        return self.op(eng, lambda e: e.scalar_tensor_tensor(o_ap, a_ap, sa, b_ap, op0, op1),
                       reads=self._bufs(in0, scalar, in1), writes=[out.buf])

    def copy(self, out, in_, eng="vector"):
        o_ap, i_ap = out.ap, in_.ap
        if eng == "scalar":
            return self.op("scalar", lambda e: e.copy(o_ap, i_ap), reads=[in_.buf], writes=[out.buf])
        return self.op(eng, lambda e: e.tensor_copy(o_ap, i_ap), reads=[in_.buf], writes=[out.buf])

    def memset(self, out, val, eng="vector"):
        o_ap = out.ap
        return self.op(eng, lambda e: e.memset(o_ap, val), writes=[out.buf])

    def reduce(self, out, in_, op, axis=AX.X, eng="vector"):
        o_ap, i_ap = out.ap, in_.ap
        return self.op(eng, lambda e: e.tensor_reduce(o_ap, i_ap, axis, op), reads=[in_.buf], writes=[out.buf])

    def max8(self, out, in_):
        o_ap, i_ap = out.ap, in_.ap
        return self.op("vector", lambda e: e.max(o_ap, i_ap), reads=[in_.buf], writes=[out.buf])

    def match_replace(self, out, to_replace, in_values, imm):
        o_ap, t_ap, i_ap = out.ap, to_replace.ap, in_values.ap
        return self.op("vector", lambda e: e.match_replace(o_ap, t_ap, i_ap, imm),
                       reads=self._bufs(to_replace, in_values), writes=[out.buf])

    def recip(self, out, in_):
        o_ap, i_ap = out.ap, in_.ap
        return self.op("vector", lambda e: e.reciprocal(o_ap, i_ap), reads=[in_.buf], writes=[out.buf])

    def bn_stats(self, out, in_):
        o_ap, i_ap = out.ap, in_.ap
        return self.op("vector", lambda e: e.bn_stats(o_ap, i_ap), reads=[in_.buf], writes=[out.buf])

    def bn_aggr(self, out, in_):
        o_ap, i_ap = out.ap, in_.ap
        return self.op("vector", lambda e: e.bn_aggr(o_ap, i_ap), reads=[in_.buf], writes=[out.buf])

    def flush(self):
        nc = self.nc
        same = self.same_engine_sync
        gen = self.gen
        for e in ENGS:
            for o in self.ops[e]:
                o.deps = [d for d in o.deps if d.gen == gen]
                for d in o.deps:
                    if d.is_dma:
                        continue
                    if d.eng == e and (e == "tensor" or not same):
                        continue
                    d.signal = True
        for e in ENGS:
            for o in self.ops[e]:
                if o.signal and not o.is_dma and o.sigsem is None:
                    c = self.sig_cnt[e]
                    o.sigsem = (e, c // EPOCH)
                    o.sigval = c % EPOCH + 1
                    self.sig_cnt[e] = c + 1
            assert self.sig_cnt[e] < EPOCH * NPROG, "out of progress semaphores"
        psems, dsems = self.psems, self.dsems
        outstanding = [d for d in self.dma_last if d is not None]
        ops_by_eng = self.ops

        def make(e):
            ops = ops_by_eng[e]

            def body(eng):
                waited = {}
                for o in ops:
                    for d in o.deps:
                        if d.is_dma:
                            key, sem, val = ("d", d.dsem), dsems[d.dsem], d.dval
                        else:
                            if d.eng == e and (e == "tensor" or not same):
                                continue
                            key, sem, val = d.sigsem, psems[d.sigsem[0]][d.sigsem[1]], d.sigval
                        if waited.get(key, 0) >= val:
                            continue
                        waited[key] = val
                        eng.wait_ge(sem, val)
                    ins = o.fn(eng)
                    if o.is_dma:
                        ins.then_inc(dsems[o.dsem], 16)
                    elif o.signal:
                        ins.then_inc(psems[o.sigsem[0]][o.sigsem[1]], 1)
                if e == "sync":
                    for d in outstanding:
                        if waited.get(("d", d.dsem), 0) >= d.dval:
                            continue
                        waited[("d", d.dsem)] = d.dval
                        eng.wait_ge(dsems[d.dsem], d.dval)
            return body

        with nc.Block() as block:
            block.tensor(make("tensor"))
            block.vector(make("vector"))
            block.scalar(make("scalar"))
            block.gpsimd(make("gpsimd"))
            block.sync(make("sync"))
        self.ops = {e: [] for e in ENGS}
        self.gen += 1


def _bf(a):
    return np.ascontiguousarray(np.asarray(a, dtype=np.float32).astype(ml_dtypes.bfloat16))


def make_constants():
    c = {}
    pos = np.arange(SEQ, dtype=np.float32)
    inv = (10000.0 ** (-np.arange(0, HD, 2, dtype=np.float32) / HD)).astype(np.float32)
    ang = (pos[None, :] * inv[:, None]).astype(np.float32)
    cos = np.cos(ang).astype(np.float32)
    sin = np.sin(ang).astype(np.float32)
    p = np.arange(128)
    f = p % 32
    sign = np.where((p % 64) < 32, -1.0, 1.0).astype(np.float32)
    c["c_cos"] = np.ascontiguousarray(cos[f])
    c["c_sin"] = np.ascontiguousarray(sin[f] * sign[:, None])
    eye = np.eye(128, dtype=np.float32)
    c["c_ident"] = _bf(eye)
    c["c_ident4"] = _bf(np.tile(eye, (1, 4)))
    c["c_identf"] = eye.copy()
    q = np.arange(128)[:, None]
    k = np.arange(128)[None, :]
    c["c_causal"] = _bf(np.where(k <= q, 0.0, NEGB))
    c["c_causalf"] = np.where(k <= q, 0.0, -1e30).astype(np.float32)
    c["c_winfar"] = _bf(np.where(k > q, 0.0, NEGB))
    e16 = np.zeros((16, 16, 128), np.float32)
    for n in range(16):
        e16[n, n, :] = 1.0
    c["c_e16"] = _bf(e16)
    e64 = np.zeros((64, 32, 128), np.float32)
    for kt in range(32):
        for kk in range(128):
            e64[2 * kt + kk // 64, kt, kk] = 1.0
    c["c_e64"] = _bf(e64)
    n = np.arange(256)[None, None, :]
    tq = (np.arange(32)[:, None, None] * 128 + np.arange(128)[None, :, None])
    valid = (n < 255) & (16 * n + 31 <= tq)
    c["c_cmpb"] = _bf(np.where(valid, 0.0, NEGB))
    j = np.arange(64)[None, None, :]
    cur = tq // 64
    causal = j <= cur
    forced = (j == 0) | ((j >= cur - 1) & causal)
    c["c_seladd"] = np.where(forced, 1e30, np.where(causal, 0.0, -1e30)).astype(np.float32)
    cs = np.arange(255) * 16
    sbs = np.arange(64) * 64
    shares = (cs[:, None] <= sbs[None, :] + 63) & (cs[:, None] + 31 >= sbs[None, :])
    m = np.zeros((256, 64), np.float32)
    m[:255] = shares
    c["c_c2s"] = _bf(m)
    pp = np.arange(128)
    c["c_tri"] = _bf((pp[:, None] < pp[None, :]).astype(np.float32))
    c["c_ones"] = _bf(np.ones((128, 128), np.float32))
    c["c_ebase"] = np.tile((np.arange(8, dtype=np.float32) * MOE_CAP)[None, None, :], (128, 32, 1))
    c["c_pow2"] = np.tile((2.0 ** -(np.arange(24) + 1.0)).astype(np.float32)[None, :], (128, 1))
    return c


def _perm_cols(w):
    d, n = w.shape
    return np.ascontiguousarray(w.reshape(d, n // 64, 2, 32)[:, :, ::-1, :].reshape(d, n))


def layout_weights(inp):
    out = {}
    for i in range(2):
        w = inp["ev_w_in"][i]
        o = EV_OFF
        fm = np.concatenate([w[:, o[k]:o[k] + EV_SIZES[k]] for k in ("aq", "ak", "bq", "bk", "iq", "ik")], axis=1)
        out[f"ev_fm{i}"] = np.ascontiguousarray(fm)
        out[f"ev_fmp{i}"] = _perm_cols(fm)
        out[f"ev_tm{i}"] = np.ascontiguousarray(
            np.concatenate([w[:, o[k]:o[k] + EV_SIZES[k]] for k in ("av", "bv", "iw")], axis=1))
        w = inp["od_w_in"][i]
        o = OD_OFF
        rope = np.concatenate([w[:, o[k]:o[k] + OD_SIZES[k]] for k in ("cq", "ck", "dq", "dks", "dkw")], axis=1)
        rest = np.concatenate([w[:, o[k]:o[k] + OD_SIZES[k]] for k in ("dkc", "dvc")], axis=1)
        out[f"od_fm{i}"] = np.ascontiguousarray(np.concatenate([rope, rest], axis=1))
        out[f"od_fmp{i}"] = _perm_cols(rope)
        out[f"od_tm{i}"] = np.ascontiguousarray(
            np.concatenate([w[:, o[k]:o[k] + OD_SIZES[k]] for k in ("cv", "dvs", "dvw", "dg")], axis=1))
        out[f"nsa_peT{i}"] = np.ascontiguousarray(np.transpose(inp["nsa_pe"][i], (0, 2, 1)))
    return out


PROJ_SKIP = set()
MOE_ROUTED = True
NFILL = 0
EV_FM = 1984
EV_TM = 644
OD_FM = 2048
OD_FMR = 1792
OD_TM = 792


class KB:
    def __init__(self, nc, st, dbg=()):
        self.nc = nc
        self.st = st
        self.S = Sched(nc, st)
        self.d = {}
        self.dbg = set(dbg)

    def din(self, name, shape, dt):
        ap = self.nc.dram_tensor(name, list(shape), dt, kind="ExternalInput").ap()
        self.d[name] = Buf(ap, name)
        return self.d[name]

    def dscr(self, name, shape, dt, out=False):
        if out or name in self.dbg:
            ap = self.nc.dram_tensor(name, list(shape), dt, kind="ExternalOutput").ap()
        else:
            ap = self.nc.dram_tensor(name, list(shape), dt).ap()
        self.d[name] = Buf(ap, name)
        return self.d[name]

    _uid = 0

    def sb(self, ps, name, shape, dt):
        KB._uid += 1
        nm = f"s{KB._uid}_{name}"
        return Buf(ps.enter_context(self.nc.sbuf_tensor(nm, list(shape), dt)), nm)

    def banks(self, ps, n=8):
        out = []
        for k in range(n):
            KB._uid += 1
            nm = f"p{KB._uid}_bank{k}"
            out.append(Buf(ps.enter_context(self.nc.psum_tensor(nm, [128, 512], F32)), nm))
            out[-1].psum = True
        return out

    def const(self, ps, name, shape, dt, src=None, eng="sync"):
        b = self.sb(ps, name, shape, dt)
        src = self.d[name] if src is None else src
        idx = tuple(slice(None) for _ in shape)
        self.S.dma(b[idx], src[idx], eng=eng)
        return b

    @staticmethod
    def run_units(units, depth=3, fill=None, nfill=0):
        n = len(units)
        for i in range(n + depth):
            if i < n:
                units[i][0]()
                if fill is not None:
                    for _ in range(nfill):
                        fill()
            j = i - depth
            if j >= 0:
                units[j][1]()

    def make_fill(self, ps, bank, ident4=None):
        S = self.S
        if ident4 is None:
            ident4 = self.const(ps, "c_ident4", [128, 512], BF16)
        ones = self.const(ps, "c_ones", [128, 128], BF16)
        fb = Buf(bank.t, "fillbank")
        fb.psum = True

        def fill():
            S.matmul(fb[:, :], lhsT=ones[:, :], rhs=ident4[:, :], start=True, stop=True, skip=True)
        return fill

    def convert(self, dst, src_view, rows):
        S = self.S
        for r0 in range(0, rows, 128):
            r1 = min(rows, r0 + 128)
            S.dma(dst.at(r0)[r0:r1, :], src_view[r0:r1, :], eng="gpsimd")

    def convert_tiled(self, dst, src_view, rows, gcols=256):
        S = self.S
        nc_ = rows // 128
        for c in range(nc_):
            S.dma(View(dst.at(c), dst.t[:, :, c * gcols:(c + 1) * gcols]),
                  src_view[c * 128:(c + 1) * 128, :].rearrange("p (g j) -> g p j", j=gcols), eng="gpsimd")

    def conv_add(self, dst_name, src_view, rows, cols, tiled=False, dst_view=None):
        if not hasattr(self, "cq"):
            self.cq, self.cdone = [], set()
        self.cq.append((dst_name, src_view, rows, rows * cols * 6, tiled, dst_view))

    def conv_budget(self, nbytes):
        while getattr(self, "cq", None) and nbytes > 0:
            name, src, rows, b, tiled, dst_view = self.cq.pop(0)
            if tiled:
                self.convert_tiled(dst_view, src, rows)
            else:
                self.convert(self.d[name], src, rows)
            self.cdone.add(name)
            nbytes -= b

    def conv_ensure(self, names):
        if not hasattr(self, "cq"):
            return
        if any(n not in self.cdone for n in names):
            while any(n not in self.cdone for n in names):
                self.conv_budget(1)
            self.S.flush()

    def emit_xT(self, src, t, identf, tbanks, stage, xT_d, xT32=None):
        S = self.S
        for hb in range(2):
            bank = tbanks[hb]
            for cc in range(4):
                c = hb * 4 + cc
                S.matmul(bank[:, cc * 128:(cc + 1) * 128], lhsT=src[:, c * 128:(c + 1) * 128], rhs=identf[:, :],
                         start=True, stop=True, skip=True)
            S.copy(stage[:, hb * 4:(hb + 1) * 4, (t % 4) * 128:(t % 4 + 1) * 128],
                   bank[:, :].rearrange("p (c t) -> p c t", c=4), eng="scalar" if hb == 0 else "vector")
            if xT32 is not None:
                S.copy(xT32[:, hb * 4:(hb + 1) * 4, :], bank[:, :].rearrange("p (c t) -> p c t", c=4),
                       eng="vector" if hb == 0 else "scalar")
        if t % 4 == 3:
            g = t // 4
            for c in range(8):
                S.dma(xT_d.at((c, g))[c * 128:(c + 1) * 128, g * 512:(g + 1) * 512], stage[:, c, :])

    def phase_prologue(self, x_src, xT_d):
        S = self.S
        with ExitStack() as ps:
            identf = self.const(ps, "c_identf", [128, 128], F32)
            xt = [self.sb(ps, f"xt{k}", [128, 1024], F32) for k in range(2)]
            stage = [self.sb(ps, f"stage{k}", [128, 8, 512], BF16) for k in range(2)]
            bk = self.banks(ps, 4)
            for t in range(NT):
                S.dma(xt[t % 2][:, :], x_src[t * 128:(t + 1) * 128, :])
                self.emit_xT(xt[t % 2][:, :], t, identf, bk[(t % 2) * 2:(t % 2) * 2 + 2], stage[(t // 4) % 2], xT_d)
            S.flush()

    def phase_proj(self, L):
        S, d = self.S, self.d
        even = L % 2 == 0
        i = L // 2
        if even:
            fm, fmp, tm = d[f"b_ev_fm{i}"], d[f"b_ev_fmp{i}"], d[f"b_ev_tm{i}"]
            NFM, NR, NTM = EV_FM, EV_FM, EV_TM
        else:
            fm, fmp, tm = d[f"b_od_fm{i}"], d[f"b_od_fmp{i}"], d[f"b_od_tm{i}"]
            NFM, NR, NTM = OD_FM, OD_FMR, OD_TM
        xT_d, qk_d, vt_d = d["xT"], d["qk"], d["vt"]
        with ExitStack() as ps:
            xT = self.sb(ps, "xT", [128, 8, SEQ], BF16)
            for c in range(8):
                S.dma(xT.at(c)[:, c, :], xT_d[c * 128:(c + 1) * 128, :])
            xTb = [xT.at(c) for c in range(8)]
            cos = self.const(ps, "c_cos", [128, SEQ], F32)
            sin = self.const(ps, "c_sin", [128, SEQ], F32)
            wA = [self.sb(ps, f"wA{k}", [128, 8, 512], BF16) for k in range(2)]
            wB = [self.sb(ps, f"wB{k}", [128, 8, 512], BF16) for k in range(2)]
            osb = [self.sb(ps, f"osb{k}", [128, 2048], BF16) for k in range(2)]
            oraw = [self.sb(ps, f"oraw{k}", [128, 2048], BF16) for k in range(2)]
            t1 = [self.sb(ps, f"t1_{k}", [128, 512], F32) for k in range(2)]
            t2 = [self.sb(ps, f"t2_{k}", [128, 512], F32) for k in range(2)]
            tf = [self.sb(ps, f"tf_{k}", [128, 512], F32) for k in range(2)]
            wtm = self.sb(ps, "wtm", [128, 8, NTM], BF16)
            vsb = [self.sb(ps, f"vsb{k}", [128, NTM], BF16) for k in range(2)]
            sm = [self.sb(ps, f"sm{k}", [128, 24], F32) for k in range(2)]
            bk = self.banks(ps, 8)
            fm_v = fm[:, :].rearrange("(c p) n -> p c n", p=128)
            fmp_v = fmp[:, :].rearrange("(c p) n -> p c n", p=128)
            if not even:
                km = self.sb(ps, "km", [128, 4, 16], F32)
                kmb = self.sb(ps, "kmb", [128, 4, 16], BF16)
                gb = self.sb(ps, "gb", [128, 24], F32)
                S.dma(gb[:, :], d["nsa_gate_b"][i:i + 1, :].broadcast_to([128, 24]))
            ntile = (NFM + 127) // 128
            u = 0
            ostage = 0
            SK = PROJ_SKIP
            for ct in range(ntile if "fm" not in SK else 0):
                rows = min(128, NFM - ct * 128)
                grp, cg = ct // 4, ct % 4
                rope = ct * 128 < NR
                A, B = wA[grp % 2], wB[grp % 2]
                if cg == 0:
                    w = min(512, NFM - grp * 512)
                    S.dma(A[:, :, :w], fm_v[:, :, grp * 512:grp * 512 + w])
                    wb = min(512, NR - grp * 512)
                    if wb > 0:
                        S.dma(B[:, :, :wb], fmp_v[:, :, grp * 512:grp * 512 + wb])
                is_ck = (not even) and 4 <= ct < 8
                is_dq = (not even) and 8 <= ct < 12
                for tc in range(8):
                    tok = slice(tc * 512, (tc + 1) * 512)
                    half, hc = tc // 4, tc % 4
                    if hc == 0:
                        ostage += 1
                    ob = osb[ostage % 2]
                    orw = oraw[ostage % 2]
                    ocol = slice(hc * 512, (hc + 1) * 512)
                    pa = bk[(2 * u) % 8]
                    pb = bk[(2 * u + 1) % 8]
                    for c in range(8):
                        S.matmul(pa[:rows, :], lhsT=A[:, c, cg * 128:cg * 128 + rows],
                                 rhs=View(xTb[c], xT.t[:, c, tok]), start=(c == 0), stop=(c == 7))
                    if rope:
                        for c in range(8):
                            S.matmul(pb[:rows, :], lhsT=B[:, c, cg * 128:cg * 128 + rows],
                                     rhs=View(xTb[c], xT.t[:, c, tok]), start=(c == 0), stop=(c == 7))
                        a1, a2 = t1[u % 2], t2[u % 2]
                        S.tt(a1[:rows, :], pa[:rows, :], cos[:rows, tok], ALU.mult)
                        S.tt(a2[:rows, :], pb[:rows, :], sin[:rows, tok], ALU.mult)
                        if is_ck and "km" not in SK:
                            f = tf[u % 2]
                            S.tt(f[:rows, :], a1[:rows, :], a2[:rows, :], ALU.add, eng="gpsimd")
                            S.copy(ob[:rows, ocol], f[:rows, :], eng="scalar")
                            S.reduce(km[:, ct - 4, tc * 2:(tc + 1) * 2],
                                     f[:, :].rearrange("p (b t) -> p b t", b=2), ALU.add)
                        else:
                            S.tt(ob[:rows, ocol], a1[:rows, :], a2[:rows, :], ALU.add, eng="gpsimd")
                        if is_dq:
                            S.copy(orw[:rows, ocol], pa[:rows, :], eng="scalar")
                    else:
                        S.copy(ob[:rows, ocol], pa[:rows, :], eng="scalar")
                    u += 1
                    if hc == 3:
                        S.dma(qk_d.at((ct, half))[ct * 128:ct * 128 + rows, half * 2048:(half + 1) * 2048], ob[:rows, :])
                        if is_dq:
                            r0 = (ct - 8) * 128
                            S.dma(d["qraw"].at((ct, half))[r0:r0 + 128, half * 2048:(half + 1) * 2048], orw[:, :])
            if not even and "km" not in SK and "fm" not in SK:
                S.ts(kmb[:, :, :], km[:, :, :], 1.0 / 256.0, None, ALU.mult)
                for k4 in range(4):
                    S.dma(d["kmean"].at(k4)[k4 * 128:(k4 + 1) * 128, :], kmb[:, k4, :])
            tm_v = tm[:, :].rearrange("(c p) n -> p c n", p=128)
            S.dma(wtm[:, :, :], tm_v)
            n1 = NTM - 512
            for t in range(NT if "tm" not in SK else 0):
                p0, p1 = bk[(2 * t) % 8], bk[(2 * t + 1) % 8]
                tk = slice(t * 128, (t + 1) * 128)
                for c in range(8):
                    S.matmul(p0[:, :], lhsT=View(xTb[c], xT.t[:, c, tk]), rhs=wtm[:, c, 0:512], start=(c == 0), stop=(c == 7))
                for c in range(8):
                    S.matmul(p1[:, :n1], lhsT=View(xTb[c], xT.t[:, c, tk]), rhs=wtm[:, c, 512:NTM], start=(c == 0), stop=(c == 7))
                vb, s = vsb[t % 2], sm[t % 2]
                S.copy(vb[:, 0:512], p0[:, :], eng="scalar")
                if even:
                    S.copy(vb[:, 512:640], p1[:, 0:128])
                    S.copy(s[:, 0:4], p1[:, 128:132])
                    S.dma(vt_d.at(t)[tk, 0:640], vb[:, 0:640])
                    S.dma(d["iw"].at(t)[tk, :], s[:, 0:4])
                else:
                    S.copy(vb[:, 512:768], p1[:, 0:256])
                    S.tt(s[:, 0:24], p1[:, 256:280], gb[:, :], ALU.add)
                    S.act(s[:, 0:24], s[:, 0:24], AF.Sigmoid)
                    S.dma(vt_d.at(t)[tk, 0:768], vb[:, 0:768])
                    S.dma(d["gates"].at(t)[tk, :], s[:, 0:24])
            S.flush()

    def phase_diff(self, L):
        S, d = self.S, self.d
        i = L // 2
        lam_init = 0.8 - 0.6 * math.exp(-0.3 * L)
        qk_d, vt_d, mix_d = d["qk"], d["vt"], d["mix"]
        with ExitStack() as ps:
            ident = self.const(ps, "c_ident", [128, 128], BF16)
            causal = self.const(ps, "c_causal", [128, 128], BF16)
            lamb = self.sb(ps, "lamb", [128, 256], F32)
            S.dma(lamb[:, :], d["dif_lambda"][i:i + 1].rearrange("o a b -> o (a b)").broadcast_to([128, 256]))
            sub = self.sb(ps, "sub", [128, 128], F32)
            S.dma(sub[:, :], d["dif_subln"][i:i + 1, :].broadcast_to([128, 128]))
            prod = self.sb(ps, "prod", [128, 2, 64], F32)
            S.tt(prod[:, 0, :], lamb[:, 0:64], lamb[:, 64:128], ALU.mult)
            S.tt(prod[:, 1, :], lamb[:, 128:192], lamb[:, 192:256], ALU.mult)
            ssum = self.sb(ps, "ssum", [128, 2], F32)
            S.reduce(ssum[:, :], prod[:, :, :], ALU.add)
            ee = self.sb(ps, "ee", [128, 2], F32)
            S.act(ee[:, :], ssum[:, :], AF.Exp)
            neglam = self.sb(ps, "neglam", [128, 1], F32)
            S.tt(neglam[:, :], ee[:, 1:2], ee[:, 0:1], ALU.subtract)
            S.ts(neglam[:, :], neglam[:, :], -lam_init, None, ALU.add)
            sw = self.sb(ps, "sw", [128, 128], F32)
            S.ts(sw[:, :], sub[:, :], 1.0 - lam_init, None, ALU.mult)
            KT = [[self.sb(ps, f"KT{k}{j}", [64, SEQ], BF16) for j in range(2)] for k in range(2)]
            QT = [[self.sb(ps, f"QT{k}{j}", [64, SEQ], BF16) for j in range(2)] for k in range(2)]
            Vaug = [self.sb(ps, f"Vaug{k}", [128, NT, 129], BF16) for k in range(2)]
            for k in range(2):
                S.memset(Vaug[k][:, :, 128:129], 1.0)
            pT = [self.sb(ps, f"pT{k}", [128, 512], BF16) for k in range(4)]
            rs = [self.sb(ps, f"rs{k}", [128, 2], F32) for k in range(2)]
            rr = [self.sb(ps, f"rr{k}", [128, 2], F32) for k in range(2)]
            tt_ = [self.sb(ps, f"tt{k}", [128, 128], F32) for k in range(2)]
            oo = [self.sb(ps, f"oo{k}", [128, 128], F32) for k in range(2)]
            jk = [self.sb(ps, f"jk{k}", [128, 128], F32) for k in range(2)]
            ss = [self.sb(ps, f"ss{k}", [128, 1], F32) for k in range(2)]
            mo = [self.sb(ps, f"mo{k}", [128, 128], BF16) for k in range(2)]
            bk = self.banks(ps, 8)
            scb = [bk[0], bk[1], bk[2], bk[7]]
            accs = [[bk[3], bk[4]], [bk[5], bk[6]]]
            tmp0 = [self.sb(ps, f"tmp0_{k}", [128, 4, 128], F32) for k in range(2)]
            cnt = dict(u=0, pp=0, a=0)
            fill = None

            def make_unit(h, c, j, kt, kt_, qt_, va, acc):
                b0 = max(0, kt - 4 * c)
                col0 = b0 * 128
                diag = kt >= 4 * c
                uu = cnt["u"]
                cnt["u"] += 1
                sc = scb[uu % 4]
                p = pT[uu % 4]

                def score():
                    S.matmul(sc[:, col0:512], lhsT=kt_[j][:, kt * 128:(kt + 1) * 128],
                             rhs=qt_[j][:, c * 512 + col0:(c + 1) * 512], start=True, stop=not diag)
                    if diag:
                        S.matmul(sc[:, col0:col0 + 128], lhsT=causal[:, :], rhs=ident[:, :], start=False, stop=True)
                    S.act(p[:, col0:512], sc[:, col0:512], AF.Exp, scale=SCALE)

                def pv():
                    for b in range(b0, 4):
                        bank = acc[b // 2]
                        o0 = (b % 2) * 129
                        S.matmul(bank[:, o0:o0 + 129], lhsT=p[:, b * 128:(b + 1) * 128], rhs=va[:, kt, :],
                                 start=(kt == 0 and b % 2 == 0), stop=(kt == 4 * c + b), skip=True)
                    if kt == 4 * c + 3:
                        post(h, c, j, acc)
                return (score, pv)

            def post(h, c, j, acc):
                t0 = tmp0[c % 2]
                for b in range(4):
                    qt = 4 * c + b
                    o0 = (b % 2) * 129
                    A = acc[b // 2]
                    k2 = cnt["pp"] % 2
                    cnt["pp"] += 1
                    S.ts(rs[k2][:, 0:1], A[:, o0 + 128:o0 + 129], 1e-30, None, ALU.max)
                    S.recip(rr[k2][:, 0:1], rs[k2][:, 0:1])
                    if j == 0:
                        S.ts(t0[:, b, :], A[:, o0:o0 + 128], rr[k2][:, 0:1], None, ALU.mult)
                        continue
                    S.ts(tt_[k2][:, :], A[:, o0:o0 + 128], rr[k2][:, 0:1], neglam[:, 0:1], ALU.mult, ALU.mult)
                    S.tt(oo[k2][:, :], t0[:, b, :], tt_[k2][:, :], ALU.add, eng="gpsimd")
                    S.memset(ss[k2][:, :], 0.0)
                    S.act(jk[k2][:, :], oo[k2][:, :], AF.Square, accum_out=ss[k2][:, 0:1])
                    S.ts(ss[k2][:, :], ss[k2][:, :], 1.0 / 128.0, LN_EPS, ALU.mult, ALU.add)
                    S.act(ss[k2][:, :], ss[k2][:, :], AF.Ln)
                    S.act(ss[k2][:, :], ss[k2][:, :], AF.Exp, scale=-0.5)
                    S.stt(mo[k2][:, :], oo[k2][:, :], ss[k2][:, 0:1], sw[:, :], ALU.mult, ALU.mult)
                    S.dma(mix_d.at(("a", h, qt))[qt * 128:(qt + 1) * 128, h * 128:(h + 1) * 128], mo[k2][:, :])

            for h in range(4):
                kt_, qt_, va = KT[h % 2], QT[h % 2], Vaug[h % 2]
                for j in range(2):
                    r0 = (h * 2 + j) * 64
                    S.dma(qt_[j][:, :], qk_d[r0:r0 + 64, :])
                    S.dma(kt_[j][:, :], qk_d[512 + r0:512 + r0 + 64, :])
                S.dma(va[:, :, 0:128], vt_d[:, h * 128:(h + 1) * 128].rearrange("(kt p) e -> p kt e", p=128))
                units = []
                for c in range(8):
                    for j in range(2):
                        acc = accs[cnt["a"] % 2]
                        cnt["a"] += 1
                        for kt in range(4 * c + 4):
                            units.append(make_unit(h, c, j, kt, kt_, qt_, va, acc))
                self.run_units(units, fill=fill, nfill=NFILL)
            S.flush()

    def phase_dsa(self, L, nit=22):
        S, d = self.S, self.d
        qk_d, vt_d, mix_d = d["qk"], d["vt"], d["mix"]
        R_BQ, R_BK, R_IQ, R_IK = 1024, 1536, 1664, 1920
        with ExitStack() as ps:
            ident4 = self.const(ps, "c_ident4", [128, 512], BF16)
            causalf = self.const(ps, "c_causalf", [128, 128], F32)
            pow2 = self.const(ps, "c_pow2", [128, 24], F32)
            bkT = self.sb(ps, "bkT", [64, 2, SEQ], BF16)
            S.dma(bkT[:, :, :], qk_d[R_BK:R_BK + 128, :].rearrange("(g d) t -> d g t", d=64))
            ikT = self.sb(ps, "ikT", [64, SEQ], BF16)
            S.dma(ikT[:, :], qk_d[R_IK:R_IK + 64, :])
            Vaug = self.sb(ps, "Vaug", [128, NT, 2, 65], BF16)
            S.memset(Vaug[:, :, :, 64:65], 1.0)
            for g in range(2):
                S.dma(Vaug[:, :, g, 0:64], vt_d[:, 512 + g * 64:512 + (g + 1) * 64].rearrange("(kt p) e -> p kt e", p=128))
            iqT = [self.sb(ps, f"iqT{k}", [64, 4, 128], BF16) for k in range(2)]
            bqT = [self.sb(ps, f"bqT{k}", [64, 8 * 128], BF16) for k in range(2)]
            iwt = [self.sb(ps, f"iwt{k}", [128, 4], F32) for k in range(2)]
            score = [self.sb(ps, f"score{k}", [128, SEQ], F32) for k in range(2)]
            biasq = [self.sb(ps, f"biasq{k}", [128, SEQ], BF16) for k in range(2)]
            junk = self.sb(ps, "junk", [128, SEQ], BF16)
            rl = [self.sb(ps, f"rl{k}", [128, 512], F32) for k in range(2)]
            pT = [self.sb(ps, f"pT{k}", [128, 512], BF16) for k in range(4)]
            mn = self.sb(ps, "mn", [128, 1], F32)
            mx = self.sb(ps, "mx", [128, 1], F32)
            w0 = self.sb(ps, "w0", [128, 1], F32)
            halfs = self.sb(ps, "halfs", [128, 24], F32)
            lo = self.sb(ps, "lo", [128, 1], F32)
            mid = self.sb(ps, "mid", [128, 1], F32)
            cnt = self.sb(ps, "cnt", [128, 24], F32)
            c256 = self.sb(ps, "c256", [128, 1], F32)
            S.memset(c256[:, :], 256.0)
            step = self.sb(ps, "step", [128, 1], F32)
            rs = self.sb(ps, "rs", [128, 4, 1], F32)
            rr = self.sb(ps, "rr", [128, 4, 1], F32)
            mo = [self.sb(ps, f"mo{k}", [128, 512], BF16) for k in range(2)]
            bk = self.banks(ps, 8)
            idxb = bk[0:2]
            scb = [bk[2], bk[3], bk[4], bk[7]]
            accb = bk[5:7]
            cnts = dict(u=0, v=0, a=0)
            fill = None

            def index_tile(qt):
                k2 = qt % 2
                N = 128 * (qt + 1)
                qs = slice(qt * 128, (qt + 1) * 128)
                S.dma(iqT[k2][:, :, :], qk_d[R_IQ:R_IQ + 256, qs].rearrange("(h d) q -> d h q", d=64))
                S.dma(bqT[k2][:, :].rearrange("d (h q) -> d h q", h=8), qk_d[R_BQ:R_BQ + 512, qs].rearrange("(h d) q -> d h q", d=64))
                S.dma(iwt[k2][:, :], d["iw"][qs, :])
                sc = score[k2]
                nch = (N + 511) // 512
                for ch in range(nch):
                    w = min(512, N - ch * 512)
                    ks = slice(ch * 512, ch * 512 + w)
                    for h in range(4):
                        pl = idxb[cnts["v"] % 2]
                        r = rl[cnts["v"] % 2]
                        cnts["v"] += 1
                        S.matmul(pl[:, :w], lhsT=iqT[k2][:, h, :], rhs=ikT[:, ks], start=True, stop=True)
                        S.act(r[:, :w], pl[:, :w], AF.Relu)
                        if h == 0:
                            S.ts(sc[:, ks], r[:, :w], iwt[k2][:, 0:1], None, ALU.mult)
                        else:
                            S.stt(sc[:, ks], r[:, :w], iwt[k2][:, h:h + 1], sc[:, ks], ALU.mult, ALU.add)
                S.reduce(mn[:, :], sc[:, :N], ALU.min)
                S.reduce(mx[:, :], sc[:, :N], ALU.max)
                S.tt(sc[:, N - 128:N], sc[:, N - 128:N], causalf[:, :], ALU.add)
                S.tt(w0[:, :], mx[:, :], mn[:, :], ALU.subtract)
                S.ts(halfs[:, :], pow2[:, :], w0[:, 0:1], None, ALU.mult)
                S.memset(cnt[:, :], 0.0)
                S.copy(lo[:, :], mn[:, :])
                S.tt(mid[:, :], mn[:, :], halfs[:, 0:1], ALU.add)
                for n in range(nit):
                    S.ts(junk[:, :N], sc[:, :N], mid[:, 0:1], 0.0, ALU.is_ge, ALU.add, accum_out=cnt[:, n:n + 1])
                    S.ts(step[:, :], cnt[:, n:n + 1], c256[:, 0:1], halfs[:, n:n + 1], ALU.is_ge, ALU.mult)
                    S.tt(lo[:, :], lo[:, :], step[:, :], ALU.add)
                    if n + 1 < nit:
                        S.tt(mid[:, :], lo[:, :], halfs[:, n + 1:n + 2], ALU.add)
                S.ts(biasq[k2][:, :N], sc[:, :N], lo[:, 0:1], NEGB, ALU.is_lt, ALU.mult)
                if "dbg_lo" in d:
                    dl = self.sb(ps, f"dl{qt}", [128, 4], F32)
                    S.copy(dl[:, 0:1], lo[:, :])
                    S.copy(dl[:, 1:2], cnt[:, nit - 1:nit])
                    S.copy(dl[:, 2:3], mn[:, :])
                    S.copy(dl[:, 3:4], mx[:, :])
                    S.dma(d["dbg_lo"].at(qt)[qs, :], dl[:, :])

            def attend_tile(qt):
                k2 = qt % 2
                m = mo[qt % 2]
                units = []
                for g in range(2):
                    acc = accb[cnts["a"] % 2]
                    cnts["a"] += 1
                    for kt in range(qt + 1):
                        units.append(make_unit(qt, k2, m, g, kt, acc))
                self.run_units(units, fill=fill, nfill=NFILL)
                S.dma(mix_d.at(("b", qt))[qt * 128:(qt + 1) * 128, 512:1024], m[:, :])

            def make_unit(qt, k2, m, g, kt, acc):
                uu = cnts["u"]
                cnts["u"] += 1
                sc = scb[uu % 4]
                p = pT[uu % 4]
                ks = slice(kt * 128, (kt + 1) * 128)

                def score():
                    S.matmul(sc[:, :], lhsT=bkT[:, g, ks], rhs=bqT[k2][:, g * 512:(g + 1) * 512], start=True, stop=False)
                    S.matmul(sc[:, :], lhsT=biasq[k2][:, ks], rhs=ident4[:, :], start=False, stop=True)
                    S.act(p[:, :], sc[:, :], AF.Exp, scale=SCALE)

                def pv():
                    for r in range(4):
                        S.matmul(acc[:, r * 65:(r + 1) * 65], lhsT=p[:, r * 128:(r + 1) * 128], rhs=Vaug[:, kt, g, :],
                                 start=(kt == 0 and r == 0), stop=(kt == qt), skip=True)
                    if kt == qt:
                        a3 = acc[:, 0:260].rearrange("p (r e) -> p r e", e=65)
                        S.ts(rs[:, :, :], a3[:, :, 64:65], 1e-30, None, ALU.max)
                        S.recip(rr[:, :, :], rs[:, :, :])
                        for r in range(4):
                            hcol = (4 * g + r) * 64
                            S.ts(m[:, hcol:hcol + 64], acc[:, r * 65:r * 65 + 64], rr[:, r, :], None, ALU.mult)
                return (score, pv)

            index_tile(0)
            for qt in range(NT):
                if qt + 1 < NT:
                    index_tile(qt + 1)
                attend_tile(qt)
            S.flush()

    def ln_alloc(self, ps, L, which, nbuf=2):
        S, d = self.S, self.d
        r = {"nbuf": nbuf}
        r["g"] = self.sb(ps, "lng", [128, D], F32)
        r["b"] = self.sb(ps, "lnb", [128, D], F32)
        S.dma(r["g"][:, :], d[f"ln_{which}_g"][L:L + 1, :].broadcast_to([128, D]))
        S.dma(r["b"][:, :], d[f"ln_{which}_b"][L:L + 1, :].broadcast_to([128, D]))
        r["xr"] = [self.sb(ps, f"xr{k}", [128, D], F32) for k in range(nbuf)]
        r["z"] = [self.sb(ps, f"z{k}", [128, D], F32) for k in range(nbuf)]
        r["xn"] = [self.sb(ps, f"xn{k}", [128, D], F32) for k in range(nbuf)]
        r["st"] = [self.sb(ps, f"st{k}", [128, 2, 6], F32) for k in range(nbuf)]
        r["mv"] = [self.sb(ps, f"mv{k}", [128, 2], F32) for k in range(nbuf)]
        r["rstd"] = [self.sb(ps, f"rstd{k}", [128, 1], F32) for k in range(nbuf)]
        r["stage"] = [self.sb(ps, f"stage{k}", [128, 8, 512], BF16) for k in range(nbuf)]
        r["identf"] = self.const(ps, "c_identf", [128, 128], F32)
        return r

    def ln_tile(self, r, t, y_views, res_src, dst, tbanks, want_xT=True, xT32=None, defer=False):
        S, d = self.S, self.d
        nb = r["nbuf"]
        k = t % nb
        tk = slice(t * 128, (t + 1) * 128)
        xr, z, xn = r["xr"][k], r["z"][k], r["xn"][k]
        S.dma(xr[:, :], View(res_src.at(t), res_src.t[tk, :]))
        for hh in range(2):
            cs = slice(hh * 512, (hh + 1) * 512)
            S.stt(z[:, cs], xr[:, cs], float(DN_ALPHA), y_views[hh], ALU.mult, ALU.add)
            S.bn_stats(r["st"][k][:, hh, :], z[:, cs])
        S.bn_aggr(r["mv"][k][:, :], r["st"][k][:, :, :])
        rstd = r["rstd"][k]
        S.ts(rstd[:, :], r["mv"][k][:, 1:2], LN_EPS, None, ALU.add)
        S.act(rstd[:, :], rstd[:, :], AF.Ln)
        S.act(rstd[:, :], rstd[:, :], AF.Exp, scale=-0.5)
        S.ts(xn[:, :], z[:, :], r["mv"][k][:, 0:1], rstd[:, 0:1], ALU.subtract, ALU.mult)
        S.tt(xn[:, :], xn[:, :], r["g"][:, :], ALU.mult, eng="gpsimd")
        S.tt(xn[:, :], xn[:, :], r["b"][:, :], ALU.add, eng="gpsimd")
        final = dst.name == "out"
        S.dma(View(dst.at(t), dst.t[tk, :]), xn[:, :], final=final)
        if want_xT:
            def later():
                self.emit_xT(xn[:, :], t, r["identf"], tbanks, r["stage"][(t // 4) % nb], d["xT"], xT32=xT32)
            if defer:
                return later
            later()
        return None

    def phase_outproj(self, L):
        S, d = self.S, self.d
        even = L % 2 == 0
        i = L // 2
        wout_d = d[f"b_ev_wout{i}"] if even else d[f"b_od_wout{i}"]
        res_src = d["x"] if (L == 0 or "force_x" in self.dbg) else d["xres"]
        with ExitStack() as ps:
            r = self.ln_alloc(ps, L, "mix")
            ident = self.const(ps, "c_ident", [128, 128], BF16)
            wout = self.sb(ps, "wout", [128, 8, D], BF16)
            S.dma(wout[:, :, :], wout_d[:, :].rearrange("(c p) n -> p c n", p=128))
            mixt = [self.sb(ps, f"mixt{k}", [128, D], BF16) for k in range(2)]
            mixT = [self.sb(ps, f"mixT{k}", [128, 8, 128], BF16) for k in range(2)]
            bk = self.banks(ps, 8)
            if not even:
                wr = self.sb(ps, "wr", [128, 8, 8], F32)
                S.dma(wr[:, :, :], d["moe_w_router"][i].rearrange("(c p) e -> p c e", p=128))
                br = self.sb(ps, "br", [128, 8], F32)
                S.dma(br[:, :], d["moe_b_router"][i:i + 1, :].broadcast_to([128, 8]))
                xT32 = [self.sb(ps, f"xT32_{k}", [128, 8, 128], F32) for k in range(2)]
                lg = [self.sb(ps, f"lg{k}", [128, 8], F32) for k in range(2)]
                m8 = [self.sb(ps, f"m8{k}", [128, 8], F32) for k in range(2)]
                dd = [self.sb(ps, f"dd{k}", [128, 1], F32) for k in range(2)]
                g12 = [self.sb(ps, f"g12{k}", [128, 2], F32) for k in range(2)]
                G = [self.sb(ps, f"G{k}", [128, 8], F32) for k in range(2)]
                G2 = [self.sb(ps, f"G2{k}", [128, 8], F32) for k in range(2)]
            pends = []
            for t in range(NT):
                k = t % 2
                tk = slice(t * 128, (t + 1) * 128)
                S.dma(mixt[k][:, :], View(d["mix"].at(("r", t)), d["mix"].t[tk, :]))
                for hb in range(2):
                    tb = bk[hb]
                    for cc in range(4):
                        c = hb * 4 + cc
                        S.matmul(tb[:, cc * 128:(cc + 1) * 128], lhsT=mixt[k][:, c * 128:(c + 1) * 128], rhs=ident[:, :],
                                 start=True, stop=True, skip=True)
                    S.copy(mixT[k][:, hb * 4:(hb + 1) * 4, :], tb[:, :].rearrange("p (c t) -> p c t", c=4),
                           eng="scalar" if hb == 0 else "vector")
                yb = [bk[2 + 2 * k], bk[3 + 2 * k]]
                for hh in range(2):
                    for c in range(8):
                        S.matmul(yb[hh][:, :], lhsT=mixT[k][:, c, :], rhs=wout[:, c, hh * 512:(hh + 1) * 512],
                                 start=(c == 0), stop=(c == 7))
                pend = self.ln_tile(r, t, [yb[0][:, :], yb[1][:, :]], res_src, d["xres"], bk[6:8],
                                    xT32=None if even else xT32[k], defer=True)
                pends.append((t, k, pend))
                if len(pends) > 1:
                    self._outproj_tail(*pends.pop(0), even, locals())
            while pends:
                self._outproj_tail(*pends.pop(0), even, locals())
            if False:
                if not even:
                    lp = bk[0]
                    for c in range(8):
                        S.matmul(lp[:, 0:8], lhsT=xT32[k][:, c, :], rhs=wr[:, c, :], start=(c == 0), stop=(c == 7))
                    S.tt(lg[k][:, :], lp[:, 0:8], br[:, :], ALU.add)
                    S.max8(m8[k][:, :], lg[k][:, :])
                    S.tt(dd[k][:, :], m8[k][:, 0:1], m8[k][:, 1:2], ALU.subtract)
                    S.act(g12[k][:, 0:1], dd[k][:, :], AF.Sigmoid)
                    S.act(g12[k][:, 1:2], dd[k][:, :], AF.Sigmoid, scale=-1.0)
                    S.ts(G[k][:, :], lg[k][:, :], m8[k][:, 0:1], g12[k][:, 0:1], ALU.is_equal, ALU.mult)
                    S.ts(G2[k][:, :], lg[k][:, :], m8[k][:, 1:2], g12[k][:, 1:2], ALU.is_equal, ALU.mult)
                    S.tt(G[k][:, :], G[k][:, :], G2[k][:, :], ALU.add)
                    S.dma(d["moeg"].at(t)[tk, :], G[k][:, :])
                    S.ts(G2[k][:, :], lg[k][:, :], m8[k][:, 0:1], None, ALU.is_equal)
                    S.dma(d["moem1"].at(t)[tk, :], G2[k][:, :])
                    S.ts(G[k][:, :], lg[k][:, :], m8[k][:, 1:2], None, ALU.is_equal)
                    S.dma(d["moem2"].at(t)[tk, :], G[k][:, :])
                    S.dma(d["moegv"].at(t)[tk, :], g12[k][:, :])
            S.flush()

    def _outproj_tail(self, t, k, pend, even, L_):
        S, d = self.S, self.d
        if pend is not None:
            pend()
        if even:
            return
        bk, xT32, wr, br = L_["bk"], L_["xT32"], L_["wr"], L_["br"]
        lg, m8, dd, g12, G, G2 = L_["lg"], L_["m8"], L_["dd"], L_["g12"], L_["G"], L_["G2"]
        tk = slice(t * 128, (t + 1) * 128)
        lp = bk[0]
        for c in range(8):
            S.matmul(lp[:, 0:8], lhsT=xT32[k][:, c, :], rhs=wr[:, c, :], start=(c == 0), stop=(c == 7))
        S.tt(lg[k][:, :], lp[:, 0:8], br[:, :], ALU.add)
        S.max8(m8[k][:, :], lg[k][:, :])
        S.tt(dd[k][:, :], m8[k][:, 0:1], m8[k][:, 1:2], ALU.subtract)
        S.act(g12[k][:, 0:1], dd[k][:, :], AF.Sigmoid)
        S.act(g12[k][:, 1:2], dd[k][:, :], AF.Sigmoid, scale=-1.0)
        S.ts(G[k][:, :], lg[k][:, :], m8[k][:, 0:1], g12[k][:, 0:1], ALU.is_equal, ALU.mult)
        S.ts(G2[k][:, :], lg[k][:, :], m8[k][:, 1:2], g12[k][:, 1:2], ALU.is_equal, ALU.mult)
        S.tt(G[k][:, :], G[k][:, :], G2[k][:, :], ALU.add)
        S.dma(d["moeg"].at(t)[tk, :], G[k][:, :])
        S.ts(G2[k][:, :], lg[k][:, :], m8[k][:, 0:1], None, ALU.is_equal)
        S.dma(d["moem1"].at(t)[tk, :], G2[k][:, :])
        S.ts(G[k][:, :], lg[k][:, :], m8[k][:, 1:2], None, ALU.is_equal)
        S.dma(d["moem2"].at(t)[tk, :], G[k][:, :])
        S.dma(d["moegv"].at(t)[tk, :], g12[k][:, :])

    def phase_ffn_dense(self, L):
        S, d = self.S, self.d
        i = L // 2
        NF = F_DENSE // 128
        final = L == DEPTH - 1
        dst = d["out"] if final else d["xres"]
        with ExitStack() as ps:
            r = self.ln_alloc(ps, L, "ffn")
            Wd = self.sb(ps, "Wd", [128, NF, D], BF16)
            wd_v = d[f"b_ffd{i}"][:, :].rearrange("(f p) n -> p f n", p=128)
            for f0 in range(0, NF, 4):
                f1 = min(NF, f0 + 4)
                S.dma(Wd.at(f0)[:, f0:f1, :], wd_v[:, f0:f1, :])
            xTc = [self.sb(ps, f"xTc{k}", [128, 8, 512], BF16) for k in range(2)]
            Wg = [self.sb(ps, f"Wg{k}", [128, 8, 256], BF16) for k in range(2)]
            Wu = [self.sb(ps, f"Wu{k}", [128, 8, 256], BF16) for k in range(2)]
            hT = self.sb(ps, "hT", [128, NF, 512], BF16)
            sg = [self.sb(ps, f"sg{k}", [128, 512], F32) for k in range(2)]
            bk = self.banks(ps, 8)
            g_t = d[f"b_ffg{i}"]
            u_t = d[f"b_ffu{i}"]
            xT_v = d["xT"][:, :].rearrange("(c p) t -> p c t", p=128)
            u = 0
            w = 0
            pend = None
            for tc in range(8):
                xc = xTc[tc % 2]
                S.dma(xc[:, :, :], View(d["xT"].at(("c", tc)), xT_v.ap[:, :, tc * 512:(tc + 1) * 512]))
                for fg in range(NF // 2):
                    wg, wu = Wg[w % 2], Wu[w % 2]
                    w += 1
                    S.dma(wg[:, :, :], g_t[fg].rearrange("p (c j) -> p c j", c=8))
                    S.dma(wu[:, :, :], u_t[fg].rearrange("p (c j) -> p c j", c=8))
                    for f2 in range(2):
                        ft = fg * 2 + f2
                        pg, pu = bk[(2 * u) % 4], bk[(2 * u + 1) % 4]
                        for c in range(8):
                            S.matmul(pg[:, :], lhsT=wg[:, c, f2 * 128:(f2 + 1) * 128], rhs=xc[:, c, :], start=(c == 0), stop=(c == 7))
                        for c in range(8):
                            S.matmul(pu[:, :], lhsT=wu[:, c, f2 * 128:(f2 + 1) * 128], rhs=xc[:, c, :], start=(c == 0), stop=(c == 7))
                        S.act(sg[u % 2][:, :], pg[:, :], AF.Silu)
                        S.tt(View(hT.at(ft), hT.t[:, ft, :]), sg[u % 2][:, :], pu[:, :], ALU.mult)
                        u += 1
                for t4 in range(4):
                    t = tc * 4 + t4
                    yb = [bk[4], bk[5]]
                    for hh in range(2):
                        for ft in range(NF):
                            S.matmul(yb[hh][:, :], lhsT=View(hT.at(ft), hT.t[:, ft, t4 * 128:(t4 + 1) * 128]),
                                     rhs=View(Wd.at((ft // 4) * 4), Wd.t[:, ft, hh * 512:(hh + 1) * 512]),
                                     start=(ft == 0), stop=(ft == NF - 1))
                    if pend is not None:
                        pend()
                    pend = self.ln_tile(r, t, [yb[0][:, :], yb[1][:, :]], d["xres"], dst, bk[6:8], want_xT=not final,
                                        defer=True)
            if pend is not None:
                pend()
            S.flush()

    def phase_cmp(self, L):
        S, d = self.S, self.d
        i = L // 2
        qk_d = d["qk"]
        with ExitStack() as ps:
            bk = self.banks(ps, 8)
            w1 = [self.sb(ps, f"w1_{a}", [64, 32, 256], BF16) for a in range(2)]
            w2 = [self.sb(ps, f"w2_{a}", [128, 2, 64], BF16) for a in range(2)]
            peT = [self.sb(ps, f"peT{a}", [64, 32], F32) for a in range(2)]
            for a in range(2):
                S.dma(w1[a][:, :, :], d[f"b_phi1_{i}"][a * 2048:(a + 1) * 2048, :].rearrange("(l d) j -> d l j", d=64))
                S.dma(w2[a][:, :, :], d[f"b_phi2_{i}"][a * 256:(a + 1) * 256, :].rearrange("(jh p) e -> p jh e", p=128))
                S.dma(peT[a][:, :], d[f"nsa_peT{i}"][a])
            src = [self.sb(ps, f"src{k}", [64, SEQ], BF16) for k in range(2)]
            hl = [self.sb(ps, f"hl{k}", [64, 32, 256], BF16) for k in range(2)]
            zs = [self.sb(ps, f"zs{k}", [128, 256], F32) for k in range(2)]
            z2 = [self.sb(ps, f"z2{k}", [128, 256], F32) for k in range(2)]
            sgm = [self.sb(ps, f"sgm{k}", [128, 256], F32) for k in range(2)]
            gz = [self.sb(ps, f"gz{k}", [128, 2, 256], BF16) for k in range(2)]
            ko = [self.sb(ps, f"ko{k}", [64, 256], BF16) for k in range(2)]
            vo = [self.sb(ps, f"vo{k}", [128, 2, 64], BF16) for k in range(2)]
            for k in range(2):
                S.memset(gz[k][:, :, :], 0.0)
                S.memset(ko[k][:, :], 0.0)
            n = 0
            for a in range(2):
                for g in range(2):
                    k = n % 2
                    n += 1
                    r0 = (1792 if a == 0 else 1920) + g * 64
                    S.dma(src[k][:, :], qk_d[r0:r0 + 64, :])
                    for l in range(32):
                        S.ts(hl[k][:, l, 0:255], src[k][:, l:l + 16 * 254 + 1:16], peT[a][:, l:l + 1], None, ALU.add,
                             eng="vector" if l % 2 == 0 else "gpsimd")
                    for jh in range(2):
                        zb = bk[jh]
                        for l in range(32):
                            S.matmul(zb[:, 0:255], lhsT=w1[a][:, l, jh * 128:(jh + 1) * 128], rhs=hl[k][:, l, 0:255],
                                     start=(l == 0), stop=(l == 31))
                        S.copy(zs[jh][:, 0:255], zb[:, 0:255], eng="scalar")
                        S.tt(z2[jh][:, 0:255], zs[jh][:, 0:255], zs[jh][:, 0:255], ALU.mult)
                        S.ts(z2[jh][:, 0:255], z2[jh][:, 0:255], 0.044715, 1.0, ALU.mult, ALU.add)
                        S.tt(z2[jh][:, 0:255], z2[jh][:, 0:255], zs[jh][:, 0:255], ALU.mult)
                        S.act(sgm[jh][:, 0:255], z2[jh][:, 0:255], AF.Sigmoid, scale=1.5957691216057308)
                        S.tt(gz[k][:, jh, 0:255], zs[jh][:, 0:255], sgm[jh][:, 0:255], ALU.mult)
                    if a == 0:
                        ob = bk[2]
                        for jh in range(2):
                            S.matmul(ob[0:64, 0:255], lhsT=w2[a][:, jh, :], rhs=gz[k][:, jh, 0:255], start=(jh == 0), stop=(jh == 1))
                        S.copy(ko[g][:, 0:255], ob[0:64, 0:255])
                        S.dma(d["kcmpT"].at(g)[g], ko[g][:, :])
                    else:
                        for nt in range(2):
                            ob = bk[3 + nt]
                            for jh in range(2):
                                S.matmul(ob[:, 0:64], lhsT=gz[k][:, jh, nt * 128:(nt + 1) * 128], rhs=w2[a][:, jh, :],
                                         start=(jh == 0), stop=(jh == 1))
                            S.copy(vo[g][:, nt, :], ob[:, 0:64])
                        S.dma(d["vcmp"].at(g)[g].rearrange("(nt p) e -> p nt e", p=128), vo[g][:, :, :])
            S.flush()

    def phase_moba(self, L):
        S, d = self.S, self.d
        qk_d, vt_d, mix_d = d["qk"], d["vt"], d["mix"]
        with ExitStack() as ps:
            ident = self.const(ps, "c_ident", [128, 128], BF16)
            causal = self.const(ps, "c_causal", [128, 128], BF16)
            e16 = self.const(ps, "c_e16", [16, 16, 128], BF16)
            QT = [self.sb(ps, f"QT{k}", [64, SEQ], BF16) for k in range(2)]
            KT = [self.sb(ps, f"KT{k}", [64, SEQ], BF16) for k in range(2)]
            Vaug = [self.sb(ps, f"Vaug{k}", [128, NT, 65], BF16) for k in range(2)]
            kmT = [self.sb(ps, f"kmT{k}", [64, 16], BF16) for k in range(2)]
            for k in range(2):
                S.memset(Vaug[k][:, :, 64:65], 1.0)
            gate = [self.sb(ps, f"gate{k}", [128, 2, 16], F32) for k in range(2)]
            m8 = [self.sb(ps, f"m8{k}", [128, 2, 8], F32) for k in range(2)]
            bias = [self.sb(ps, f"bias{k}", [128, 2, 16], BF16) for k in range(2)]
            biasT = [self.sb(ps, f"biasT{k}", [16, 16, 256], BF16) for k in range(2)]
            pT = [self.sb(ps, f"pT{k}", [128, 256], BF16) for k in range(4)]
            rs = [self.sb(ps, f"rs{k}", [128, 2, 1], F32) for k in range(2)]
            rr = [self.sb(ps, f"rr{k}", [128, 2, 1], F32) for k in range(2)]
            mo = [self.sb(ps, f"mo{k}", [128, NT, 64], BF16) for k in range(2)]
            bk = self.banks(ps, 8)
            scb = [bk[0], bk[1], bk[2], bk[6]]
            accb = bk[3:5]
            gpb = [bk[5], bk[5]]
            tpb = bk[7]
            cnt = dict(u=0)
            fill = None

            def make_unit(h, hk, B, kt, qt_, kt_, va, acc):
                uu = cnt["u"]
                cnt["u"] += 1
                sc = scb[uu % 4]
                p = pT[uu % 4]
                ks = slice(kt * 128, (kt + 1) * 128)
                col0 = 128 if kt == 2 * B + 1 else 0
                past = kt < 2 * B
                sel = B > 3
                k2 = B % 2

                def score():
                    last_first = not ((past and sel) or (not past))
                    S.matmul(sc[:, col0:256], lhsT=kt_[:, ks], rhs=qt_[:, B * 256 + col0:(B + 1) * 256],
                             start=True, stop=last_first)
                    if past and sel:
                        S.matmul(sc[:, 0:256], lhsT=e16[:, kt // 2, :], rhs=biasT[hk][:, B, :], start=False, stop=True)
                    if not past:
                        c0 = (kt - 2 * B) * 128
                        S.matmul(sc[:, c0:c0 + 128], lhsT=causal[:, :], rhs=ident[:, :], start=False, stop=True)
                    S.act(p[:, col0:256], sc[:, col0:256], AF.Exp, scale=SCALE)

                def pv():
                    for t in range(col0 // 128, 2):
                        S.matmul(acc[:, t * 65:(t + 1) * 65], lhsT=p[:, t * 128:(t + 1) * 128], rhs=va[:, kt, :],
                                 start=(kt == 0 and t == 0), stop=(kt == 2 * B + t), skip=True)
                    if kt == 2 * B + 1:
                        a3 = acc[:, 0:130].rearrange("p (t e) -> p t e", e=65)
                        S.ts(rs[k2][:, :, :], a3[:, :, 64:65], 1e-30, None, ALU.max)
                        S.recip(rr[k2][:, :, :], rs[k2][:, :, :])
                        for t in range(2):
                            S.ts(mo[hk][:, 2 * B + t, :], acc[:, t * 65:t * 65 + 64], rr[k2][:, t, :], None, ALU.mult)
                return (score, pv)

            for h in range(8):
                hk = h % 2
                qt_, kt_, va = QT[hk], KT[hk], Vaug[hk]
                S.dma(qt_[:, :], qk_d[h * 64:(h + 1) * 64, :])
                S.dma(kt_[:, :], qk_d[512 + h * 64:512 + (h + 1) * 64, :])
                S.dma(va[:, :, 0:64], vt_d[:, h * 64:(h + 1) * 64].rearrange("(kt p) e -> p kt e", p=128))
                S.dma(kmT[hk][:, :], d["kmean"][h * 64:(h + 1) * 64, :])
                for B in range(4, 16):
                    k2 = B % 2
                    gp = gpb[k2]
                    for t in range(2):
                        S.matmul(gp[:, t * 16:(t + 1) * 16], lhsT=qt_[:, (2 * B + t) * 128:(2 * B + t + 1) * 128],
                                 rhs=kmT[hk][:, :], start=True, stop=True, skip=True)
                    S.memset(gate[k2][:, :, :], -1e30)
                    S.copy(gate[k2][:, :, 0:B], gp[:, 0:32].rearrange("p (t n) -> p t n", t=2)[:, :, 0:B])
                    for t in range(2):
                        S.max8(m8[k2][:, t, :], gate[k2][:, t, :])
                        S.ts(bias[k2][:, t, :], gate[k2][:, t, :], m8[k2][:, t, 2:3], NEGB, ALU.is_lt, ALU.mult)
                        S.matmul(tpb[0:16, t * 128:(t + 1) * 128], lhsT=bias[k2][:, t, :], rhs=ident[:, :],
                                 start=True, stop=True, skip=True)
                    S.copy(biasT[hk][:, B, :], tpb[0:16, 0:256], eng="scalar")
                units = []
                for B in range(16):
                    acc = accb[B % 2]
                    for kt in range(2 * B + 2):
                        units.append(make_unit(h, hk, B, kt, qt_, kt_, va, acc))
                self.run_units(units, fill=fill, nfill=NFILL)
                mv = mix_d[:, h * 64:(h + 1) * 64].rearrange("(qt p) e -> p qt e", p=128)
                for q4 in range(4):
                    S.dma(View(mix_d.at(("c", h, q4)), mv.ap[:, q4 * 8:(q4 + 1) * 8, :]), mo[hk][:, q4 * 8:(q4 + 1) * 8, :])
            S.flush()

    def phase_nsa(self, L):
        S, d = self.S, self.d
        qk_d, vt_d, mix_d = d["qk"], d["vt"], d["mix"]
        with ExitStack() as ps:
            ident = self.const(ps, "c_ident", [128, 128], BF16)
            ident4 = self.const(ps, "c_ident4", [128, 512], BF16)
            causal = self.const(ps, "c_causal", [128, 128], BF16)
            winfar = self.const(ps, "c_winfar", [128, 128], BF16)
            e64 = self.const(ps, "c_e64", [64, 32, 128], BF16)
            KsT = self.sb(ps, "KsT", [64, SEQ], BF16)
            KwT = self.sb(ps, "KwT", [64, SEQ], BF16)
            Vs = self.sb(ps, "Vs", [128, NT, 65], BF16)
            Vw = self.sb(ps, "Vw", [128, NT, 65], BF16)
            kcT = self.sb(ps, "kcT", [64, 256], BF16)
            Vc = self.sb(ps, "Vc", [128, 2, 129], BF16)
            qraw = [self.sb(ps, f"qraw{k}", [64, 512], BF16) for k in range(2)]
            qrot = [self.sb(ps, f"qrot{k}", [64, 512], BF16) for k in range(2)]
            cmpb = [self.sb(ps, f"cmpb{k}", [128, 256], BF16) for k in range(2)]
            sadd = [self.sb(ps, f"sadd{k}", [128, 64], F32) for k in range(2)]
            gts = [self.sb(ps, f"gts{k}", [128, 4, 3], F32) for k in range(2)]
            pT = [self.sb(ps, f"pT{k}", [128, 512], BF16) for k in range(4)]
            rcp = [self.sb(ps, f"rcp{k}", [128, 4, 3], F32) for k in range(2)]
            sums = [self.sb(ps, f"sums{k}", [128, 4, 3], F32) for k in range(2)]
            coef = [self.sb(ps, f"coef{k}", [128, 4, 3], F32) for k in range(2)]
            imp = [self.sb(ps, f"imp{k}", [128, 64], F32) for k in range(2)]
            imp3 = [self.sb(ps, f"imp3{k}", [128, 64], F32) for k in range(2)]
            m8a = [self.sb(ps, f"m8a{k}", [128, 8], F32) for k in range(2)]
            m8b = [self.sb(ps, f"m8b{k}", [128, 8], F32) for k in range(2)]
            sbias = [self.sb(ps, f"sbias{k}", [128, 64], BF16) for k in range(2)]
            biasT4 = [self.sb(ps, f"biasT4{k}", [64, 512], BF16) for k in range(2)]
            oo = [self.sb(ps, f"oo{k}", [128, 64], F32) for k in range(2)]
            mo = [self.sb(ps, f"mo{k}", [128, 256], BF16) for k in range(2)]
            bk = self.banks(ps, 8)
            scb = [bk[0], bk[1], bk[2], bk[7]]
            accC = bk[3:5]
            accS, accW = bk[5], bk[6]
            cnt = dict(u=0)
            fill = None
            for g in range(2):
                S.dma(KsT[:, :], qk_d[1536 + g * 64:1536 + (g + 1) * 64, :])
                S.dma(KwT[:, :], qk_d[1664 + g * 64:1664 + (g + 1) * 64, :])
                S.memset(Vs[:, :, 64:65], 1.0)
                S.memset(Vw[:, :, 64:65], 1.0)
                S.memset(Vc[:, :, 64:65], 1.0)
                S.dma(Vs[:, :, 0:64], vt_d[:, 512 + g * 64:512 + (g + 1) * 64].rearrange("(kt p) e -> p kt e", p=128))
                S.dma(Vw[:, :, 0:64], vt_d[:, 640 + g * 64:640 + (g + 1) * 64].rearrange("(kt p) e -> p kt e", p=128))
                S.dma(kcT[:, :], d["kcmpT"][g])
                S.dma(Vc[:, :, 0:64], d["vcmp"][g].rearrange("(nt p) e -> p nt e", p=128))
                S.dma(Vc[:, :, 65:129], d["c_c2s"][:, :].rearrange("(nt p) e -> p nt e", p=128))
                for qt in range(NT):
                    k2 = qt % 2
                    qs = slice(qt * 128, (qt + 1) * 128)
                    S.dma(qraw[k2][:, :].rearrange("d (r q) -> d r q", r=4),
                          d["qraw"][g * 256:(g + 1) * 256, qs].rearrange("(r d) q -> d r q", d=64))
                    S.dma(qrot[k2][:, :].rearrange("d (r q) -> d r q", r=4),
                          qk_d[1024 + g * 256:1024 + (g + 1) * 256, qs].rearrange("(r d) q -> d r q", d=64))
                    S.dma(cmpb[k2][:, :], d["c_cmpb"][qt])
                    S.dma(sadd[k2][:, :], d["c_seladd"][qt])
                    S.dma(gts[k2][:, :, :], d["gates"][qs, g * 12:(g + 1) * 12].rearrange("p (r b) -> p r b", b=3))
                    def mk(kind, kt, k2=k2, qt=qt):
                        uu = cnt["u"]
                        cnt["u"] += 1
                        sc = scb[uu % 4]
                        p = pT[uu % 4]
                        ks = slice(kt * 128, (kt + 1) * 128)

                        def score():
                            if kind == "c":
                                S.matmul(sc[:, :], lhsT=kcT[:, ks], rhs=qraw[k2][:, :], start=True, stop=False)
                                S.matmul(sc[:, :], lhsT=cmpb[k2][:, ks], rhs=ident4[:, :], start=False, stop=True)
                            elif kind == "s":
                                S.matmul(sc[:, :], lhsT=KsT[:, ks], rhs=qrot[k2][:, :], start=True, stop=False)
                                S.matmul(sc[:, :], lhsT=e64[:, kt, :], rhs=biasT4[k2][:, :], start=False, stop=(kt != qt))
                                if kt == qt:
                                    S.matmul(sc[:, :], lhsT=causal[:, :], rhs=ident4[:, :], start=False, stop=True)
                            else:
                                edge = (kt == qt) or (kt == qt - 4)
                                S.matmul(sc[:, :], lhsT=KwT[:, ks], rhs=qrot[k2][:, :], start=True, stop=not edge)
                                if kt == qt:
                                    S.matmul(sc[:, :], lhsT=causal[:, :], rhs=ident4[:, :], start=False, stop=True)
                                elif kt == qt - 4:
                                    S.matmul(sc[:, :], lhsT=winfar[:, :], rhs=ident4[:, :], start=False, stop=True)
                            S.act(p[:, :], sc[:, :], AF.Exp, scale=SCALE)

                        def pv():
                            if kind == "c":
                                for r in range(4):
                                    o0 = (r % 2) * 129
                                    S.matmul(accC[r // 2][:, o0:o0 + 129], lhsT=p[:, r * 128:(r + 1) * 128], rhs=Vc[:, kt, :],
                                             start=(kt == 0 and r % 2 == 0), stop=(kt == 1), skip=True)
                            elif kind == "s":
                                for r in range(4):
                                    S.matmul(accS[:, r * 65:(r + 1) * 65], lhsT=p[:, r * 128:(r + 1) * 128], rhs=Vs[:, kt, :],
                                             start=(kt == 0 and r == 0), stop=(kt == qt), skip=True)
                            else:
                                k0 = max(0, qt - 4)
                                for r in range(4):
                                    S.matmul(accW[:, r * 65:(r + 1) * 65], lhsT=p[:, r * 128:(r + 1) * 128], rhs=Vw[:, kt, :],
                                             start=(kt == k0 and r == 0), stop=(kt == qt), skip=True)
                        return (score, pv)

                    units = [mk("c", 0), mk("c", 1)] + [mk("w", kt) for kt in range(max(0, qt - 4), qt + 1)]
                    self.run_units(units, fill=fill, nfill=NFILL)
                    for r in range(4):
                        o0 = (r % 2) * 129
                        S.ts(sums[k2][:, r, 0:1], accC[r // 2][:, o0 + 64:o0 + 65], 1e-30, None, ALU.max)
                    S.recip(rcp[k2][:, :, 0:1], sums[k2][:, :, 0:1])
                    for r in range(4):
                        o0 = (r % 2) * 129
                        iu = accC[r // 2][:, o0 + 65:o0 + 129]
                        if r == 0:
                            S.ts(imp[k2][:, :], iu, rcp[k2][:, 0, 0:1], None, ALU.mult)
                        else:
                            S.stt(imp[k2][:, :], iu, rcp[k2][:, r, 0:1], imp[k2][:, :], ALU.mult, ALU.add)
                    S.tt(imp[k2][:, :], imp[k2][:, :], sadd[k2][:, :], ALU.add)
                    S.max8(m8a[k2][:, :], imp[k2][:, :])
                    S.match_replace(imp3[k2][:, :], m8a[k2][:, :], imp[k2][:, :], -3.0e38)
                    S.max8(m8b[k2][:, :], imp3[k2][:, :])
                    S.ts(sbias[k2][:, :], imp[k2][:, :], m8b[k2][:, 7:8], NEGB, ALU.is_lt, ALU.mult)
                    tp = scb[cnt["u"] % 4]
                    cnt["u"] += 1
                    S.matmul(tp[0:64, 0:128], lhsT=sbias[k2][:, :], rhs=ident[:, :], start=True, stop=True, skip=True)
                    for r in range(4):
                        S.copy(biasT4[k2][:, r * 128:(r + 1) * 128], tp[0:64, 0:128], eng="scalar" if r % 2 == 0 else "vector")
                    self.run_units([mk("s", kt) for kt in range(qt + 1)], fill=fill, nfill=NFILL)
                    s3 = accS[:, 0:260].rearrange("p (r e) -> p r e", e=65)
                    w3 = accW[:, 0:260].rearrange("p (r e) -> p r e", e=65)
                    S.ts(sums[k2][:, :, 1:2], s3[:, :, 64:65], 1e-30, None, ALU.max)
                    S.ts(sums[k2][:, :, 2:3], w3[:, :, 64:65], 1e-30, None, ALU.max)
                    S.recip(rcp[k2][:, :, 1:3], sums[k2][:, :, 1:3])
                    S.tt(coef[k2][:, :, :], gts[k2][:, :, :], rcp[k2][:, :, :], ALU.mult)
                    for r in range(4):
                        o0 = (r % 2) * 129
                        o = oo[r % 2]
                        S.ts(o[:, :], accC[r // 2][:, o0:o0 + 64], coef[k2][:, r, 0:1], None, ALU.mult)
                        S.stt(o[:, :], accS[:, r * 65:r * 65 + 64], coef[k2][:, r, 1:2], o[:, :], ALU.mult, ALU.add)
                        S.stt(mo[k2][:, r * 64:(r + 1) * 64], accW[:, r * 65:r * 65 + 64], coef[k2][:, r, 2:3], o[:, :],
                              ALU.mult, ALU.add)
                    S.dma(mix_d.at(("d", g, qt))[qs, 512 + g * 256:512 + (g + 1) * 256], mo[k2][:, :])
            S.flush()

    def phase_moe(self, L):
        S, d = self.S, self.d
        i = L // 2
        NF = F_EXPERT // 128
        final = L == DEPTH - 1
        dst = d["out"] if final else d["xres"]
        with ExitStack() as ps:
            r = self.ln_alloc(ps, L, "ffn", nbuf=1)
            xTc = [self.sb(ps, f"xTc{k}", [128, 8, 512], BF16) for k in range(2)]
            Gc = [self.sb(ps, f"Gc{k}", [128, 4, 8], F32) for k in range(2)]
            Wg = [self.sb(ps, f"Wg{k}", [128, 8, 256], BF16) for k in range(2)]
            Wu = [self.sb(ps, f"Wu{k}", [128, 8, 256], BF16) for k in range(2)]
            Wdh = [self.sb(ps, f"Wdh{k}", [128, NF, 512], BF16) for k in range(2)]
            hT = self.sb(ps, "hT", [128, NF, 512], BF16)
            sg = [self.sb(ps, f"sg{k}", [128, 512], F32) for k in range(2)]
            acc = self.sb(ps, "acc", [128, 4, D], F32)
            bk = self.banks(ps, 8)
            g_all = d[f"b_mg{i}"]
            u_all = d[f"b_mu{i}"]
            d_all = d[f"b_md{i}"]
            xT_v = d["xT"][:, :].rearrange("(c p) t -> p c t", p=128)
            u = 0
            w = 0
            wd = 0
            for tc in range(8):
                xc = xTc[tc % 2]
                gc = Gc[tc % 2]
                S.dma(xc[:, :, :], View(d["xT"].at(("c", tc)), xT_v.ap[:, :, tc * 512:(tc + 1) * 512]))
                S.dma(gc[:, :, :], d["moeg"][tc * 512:(tc + 1) * 512, :].rearrange("(t p) e -> p t e", p=128))
                for e in range(N_EXPERTS):
                    g_v = None
                    u_v = None
                    d_v = d_all[e * F_EXPERT:(e + 1) * F_EXPERT, :].rearrange("(f p) n -> p f n", p=128)
                    for fg in range(NF // 2):
                        wg, wu = Wg[w % 2], Wu[w % 2]
                        w += 1
                        S.dma(wg[:, :, :], g_all[e * (NF // 2) + fg].rearrange("p (c j) -> p c j", c=8))
                        S.dma(wu[:, :, :], u_all[e * (NF // 2) + fg].rearrange("p (c j) -> p c j", c=8))
                        for f2 in range(2):
                            ft = fg * 2 + f2
                            pg, pu = bk[(2 * u) % 4], bk[(2 * u + 1) % 4]
                            for c in range(8):
                                S.matmul(pg[:, :], lhsT=wg[:, c, f2 * 128:(f2 + 1) * 128], rhs=xc[:, c, :], start=(c == 0), stop=(c == 7))
                            for c in range(8):
                                S.matmul(pu[:, :], lhsT=wu[:, c, f2 * 128:(f2 + 1) * 128], rhs=xc[:, c, :], start=(c == 0), stop=(c == 7))
                            S.act(sg[u % 2][:, :], pg[:, :], AF.Silu)
                            S.tt(View(hT.at(ft), hT.t[:, ft, :]), sg[u % 2][:, :], pu[:, :], ALU.mult)
                            u += 1
                    for hh in range(2):
                        wdh = Wdh[wd % 2]
                        wd += 1
                        for f0 in range(0, NF, 7):
                            S.dma(View(wdh.at(f0), wdh.t[:, f0:f0 + 7, :]), d_v[:, f0:f0 + 7, hh * 512:(hh + 1) * 512])
                        for t4 in range(4):
                            py = bk[4 + (t4 % 2)]
                            for ft in range(NF):
                                S.matmul(py[:, :], lhsT=View(hT.at(ft), hT.t[:, ft, t4 * 128:(t4 + 1) * 128]),
                                         rhs=View(wdh.at((ft // 7) * 7), wdh.t[:, ft, :]), start=(ft == 0), stop=(ft == NF - 1))
                            av = View(acc.at((t4, hh)), acc.t[:, t4, hh * 512:(hh + 1) * 512])
                            if e == 0:
                                S.ts(av, py[:, :], gc[:, t4, e:e + 1], None, ALU.mult)
                            else:
                                S.stt(av, py[:, :], gc[:, t4, e:e + 1], av, ALU.mult, ALU.add)
                for t4 in range(4):
                    t = tc * 4 + t4
                    yv = [View(acc.at((t4, hh)), acc.t[:, t4, hh * 512:(hh + 1) * 512]) for hh in range(2)]
                    self.ln_tile(r, t, yv, d["xres"], dst, bk[6:8], want_xT=not final)
            S.flush()

    def phase_moe_routed(self, L):
        S, d = self.S, self.d
        i = L // 2
        NF = F_EXPERT // 128
        CAP = MOE_CAP
        NCH = CAP // 512
        ROWS = 8 * CAP
        U32 = mybir.dt.uint32
        final = L == DEPTH - 1
        dst = d["out"] if final else d["xres"]
        xs_d, ys_d = d["xs"], d["ys"]
        with ExitStack() as ps:
            r = self.ln_alloc(ps, L, "ffn", nbuf=1)
            ident = self.const(ps, "c_ident", [128, 128], BF16)
            tri = self.const(ps, "c_tri", [128, 128], BF16)
            ones = self.const(ps, "c_ones", [128, 128], BF16)
            ebase = self.const(ps, "c_ebase", [128, 32, 8], F32)
            bk = self.banks(ps, 8)
            m1 = self.sb(ps, "m1", [128, 32, 8], F32)
            m2 = self.sb(ps, "m2", [128, 32, 8], F32)
            gv = self.sb(ps, "gv", [128, 32, 2], F32)
            S.dma(m1[:, :, :], d["moem1"][:, :].rearrange("(t p) e -> p t e", p=128))
            S.dma(m2[:, :, :], d["moem2"][:, :].rearrange("(t p) e -> p t e", p=128))
            S.dma(gv[:, :, :], d["moegv"][:, :].rearrange("(t p) e -> p t e", p=128))
            selb = self.sb(ps, "selb", [128, 256], BF16)
            S.tt(selb[:, :].rearrange("p (t e) -> p t e", e=8), m1[:, :, :], m2[:, :, :], ALU.add)
            S.matmul(bk[0][:, 0:256], lhsT=tri[:, :], rhs=selb[:, :], start=True, stop=True)
            S.matmul(bk[1][:, 0:256], lhsT=ones[:, :], rhs=selb[:, :], start=True, stop=True)
            tot = self.sb(ps, "tot", [128, 32, 8], F32)
            S.copy(tot[:, :, :], bk[1][:, 0:256].rearrange("p (t e) -> p t e", e=8))
            off = self.sb(ps, "off", [128, 32, 8], F32)
            S.memset(off[:, 0, :], 0.0)
            for t in range(1, 32):
                S.tt(off[:, t, :], off[:, t - 1, :], tot[:, t - 1, :], ALU.add)
            pos = self.sb(ps, "pos", [128, 32, 8], F32)
            S.tt(pos[:, :, :], off[:, :, :], bk[0][:, 0:256].rearrange("p (t e) -> p t e", e=8), ALU.add)
            slot = self.sb(ps, "slot", [128, 32, 8], F32)
            S.tt(slot[:, :, :], pos[:, :, :], ebase[:, :, :], ALU.add)
            S.ts(pos[:, :, :], pos[:, :, :], float(CAP), 1.0e6, ALU.is_ge, ALU.mult)
            S.tt(slot[:, :, :], slot[:, :, :], pos[:, :, :], ALU.add)
            sl_f = self.sb(ps, "sl_f", [128, 2, 32], F32)
            tmp = self.sb(ps, "rtmp", [128, 32, 8], F32)
            S.tt(tmp[:, :, :], slot[:, :, :], m1[:, :, :], ALU.mult)
            S.reduce(sl_f[:, 0, :], tmp[:, :, :], ALU.add)
            S.tt(tmp[:, :, :], slot[:, :, :], m2[:, :, :], ALU.mult)
            S.reduce(sl_f[:, 1, :], tmp[:, :, :], ALU.add)
            sl_i = self.sb(ps, "sl_i", [128, 2, 32], U32)
            S.copy(sl_i[:, :, :], sl_f[:, :, :])
            okf = self.sb(ps, "okf", [128, 2, 32], F32)
            S.ts(okf[:, :, :], sl_f[:, :, :], 1.0e5, None, ALU.is_lt)
            geff = self.sb(ps, "geff", [128, 2, 32], F32)
            S.tt(geff[:, 0, :], gv[:, :, 0], okf[:, 0, :], ALU.mult)
            S.tt(geff[:, 1, :], gv[:, :, 1], okf[:, 1, :], ALU.mult)
            zt = self.sb(ps, "zt", [128, D], BF16)
            S.memset(zt[:, :], 0.0)
            for r0 in range(0, ROWS, 128):
                S.dma(xs_d.at(("z", r0))[r0:r0 + 128, :], zt[:, :])
            S.flush()
            KB._uid += 1
            breg = ps.enter_context(self.nc.gpsimd.register(f"moe_bound{KB._uid}"))
            S.op("gpsimd", lambda e: e.reg_mov(breg, ROWS - 1))
            xf = [self.sb(ps, f"xf{k}", [128, D], F32) for k in range(2)]
            xb = [self.sb(ps, f"xb{k}", [128, D], BF16) for k in range(2)]
            for t in range(NT):
                k = t % 2
                tk = slice(t * 128, (t + 1) * 128)
                S.dma(xf[k][:, :], View(d["xres"].at(t), d["xres"].t[tk, :]))
                S.copy(xb[k][:, :], xf[k][:, :], eng="scalar")
                for j in range(2):
                    idx_ap = sl_i.t[:, j, t:t + 1]
                    o_ap, i_ap = xs_d.t[:, :], xb[k].t[:, :]
                    S.dma_op("gpsimd",
                             lambda e, o_ap=o_ap, i_ap=i_ap, idx_ap=idx_ap: e.indirect_dma_start(
                                 out=o_ap, out_offset=bass.IndirectOffsetOnAxis(ap=idx_ap, axis=0), in_=i_ap, in_offset=None,
                                 bounds_check=breg, oob_is_err=False),
                             reads=[xb[k], sl_i], writes=[xs_d.at(("sc", t, j))])
            S.flush()
            xtm = self.sb(ps, "xtm", [128, 4, D], BF16)
            xTc = self.sb(ps, "xTc", [128, 8, 512], BF16)
            Wg = [self.sb(ps, f"Wg{k}", [128, 8, 256], BF16) for k in range(2)]
            Wu = [self.sb(ps, f"Wu{k}", [128, 8, 256], BF16) for k in range(2)]
            Wdh = [self.sb(ps, f"Wdh{k}", [128, NF, 512], BF16) for k in range(2)]
            hT = self.sb(ps, "hT", [128, NF, 512], BF16)
            sg = [self.sb(ps, f"sg{k}", [128, 512], F32) for k in range(2)]
            yo = [self.sb(ps, f"yo{k}", [128, 512], F32) for k in range(2)]
            g_all, u_all, d_all = d[f"b_mg{i}"], d[f"b_mu{i}"], d[f"b_md{i}"]
            u = 0
            w = 0
            wd = 0
            yy = 0
            for e in range(N_EXPERTS):
                g_v = None
                u_v = None
                d_v = d_all[e * F_EXPERT:(e + 1) * F_EXPERT, :].rearrange("(f p) n -> p f n", p=128)
                for c in range(NCH):
                    r0 = e * CAP + c * 512
                    S.dma(xtm[:, :, :], View(xs_d.at(("ld", r0)), xs_d.t[r0:r0 + 512, :].rearrange("(s p) n -> p s n", p=128)))
                    for s4 in range(4):
                        for hb in range(2):
                            tb = bk[6 + hb]
                            for cc in range(4):
                                dc = hb * 4 + cc
                                S.matmul(tb[:, cc * 128:(cc + 1) * 128], lhsT=xtm[:, s4, dc * 128:(dc + 1) * 128], rhs=ident[:, :],
                                         start=True, stop=True, skip=True)
                            S.copy(xTc[:, hb * 4:(hb + 1) * 4, s4 * 128:(s4 + 1) * 128],
                                   tb[:, :].rearrange("p (c t) -> p c t", c=4), eng="scalar" if hb == 0 else "vector")
                    for fg in range(NF // 2):
                        wg, wu = Wg[w % 2], Wu[w % 2]
                        w += 1
                        S.dma(wg[:, :, :], g_all[e * (NF // 2) + fg].rearrange("p (c j) -> p c j", c=8))
                        S.dma(wu[:, :, :], u_all[e * (NF // 2) + fg].rearrange("p (c j) -> p c j", c=8))
                        for f2 in range(2):
                            ft = fg * 2 + f2
                            pg, pu = bk[(2 * u) % 4], bk[(2 * u + 1) % 4]
                            for cI in range(8):
                                S.matmul(pg[:, :], lhsT=wg[:, cI, f2 * 128:(f2 + 1) * 128], rhs=xTc[:, cI, :], start=(cI == 0), stop=(cI == 7))
                            for cI in range(8):
                                S.matmul(pu[:, :], lhsT=wu[:, cI, f2 * 128:(f2 + 1) * 128], rhs=xTc[:, cI, :], start=(cI == 0), stop=(cI == 7))
                            S.act(sg[u % 2][:, :], pg[:, :], AF.Silu)
                            S.tt(View(hT.at(ft), hT.t[:, ft, :]), sg[u % 2][:, :], pu[:, :], ALU.mult)
                            u += 1
                    for hh in range(2):
                        wdh = Wdh[wd % 2]
                        wd += 1
                        for f0 in range(0, NF, 7):
                            S.dma(View(wdh.at(f0), wdh.t[:, f0:f0 + 7, :]), d_v[:, f0:f0 + 7, hh * 512:(hh + 1) * 512])
                        for t4 in range(4):
                            py = bk[4 + (t4 % 2)]
                            for ft in range(NF):
                                S.matmul(py[:, :], lhsT=View(hT.at(ft), hT.t[:, ft, t4 * 128:(t4 + 1) * 128]),
                                         rhs=View(wdh.at((ft // 7) * 7), wdh.t[:, ft, :]), start=(ft == 0), stop=(ft == NF - 1))
                            y = yo[yy % 2]
                            yy += 1
                            S.copy(y[:, :], py[:, :], eng="scalar" if t4 % 2 == 0 else "vector")
                            rr0 = r0 + t4 * 128
                            S.dma(View(ys_d.at((rr0, hh)), ys_d.t[rr0:rr0 + 128, hh * 512:(hh + 1) * 512]), y[:, :])
            S.flush()
            y1 = self.sb(ps, "y1", [128, D], F32)
            y2 = self.sb(ps, "y2", [128, D], F32)
            acc = self.sb(ps, "acc", [128, D], F32)
            S.memset(y1[:, :], 0.0)
            S.memset(y2[:, :], 0.0)
            S.op("gpsimd", lambda e: e.reg_mov(breg, ROWS - 1))
            for t in range(NT):
                for j, yb in enumerate((y1, y2)):
                    idx_ap = sl_i.t[:, j, t:t + 1]
                    o_ap, i_ap = yb.t[:, :], ys_d.t[:, :]
                    S.dma_op("gpsimd",
                             lambda e, o_ap=o_ap, i_ap=i_ap, idx_ap=idx_ap: e.indirect_dma_start(
                                 out=o_ap, out_offset=None, in_=i_ap, in_offset=bass.IndirectOffsetOnAxis(ap=idx_ap, axis=0),
                                 bounds_check=breg, oob_is_err=False),
                             reads=[sl_i], writes=[yb])
                S.ts(acc[:, :], y1[:, :], geff[:, 0, t:t + 1], None, ALU.mult)
                S.stt(acc[:, :], y2[:, :], geff[:, 1, t:t + 1], acc[:, :], ALU.mult, ALU.add)
                self.ln_tile(r, t, [acc[:, 0:512], acc[:, 512:1024]], d["xres"], dst, bk[6:8], want_xT=not final)
            S.flush()


CONST_SPECS = dict(
    c_cos=([128, SEQ], F32), c_sin=([128, SEQ], F32), c_ident=([128, 128], BF16), c_ident4=([128, 512], BF16),
    c_identf=([128, 128], F32), c_causal=([128, 128], BF16), c_causalf=([128, 128], F32), c_winfar=([128, 128], BF16),
    c_e16=([16, 16, 128], BF16), c_e64=([64, 32, 128], BF16), c_cmpb=([32, 128, 256], BF16),
    c_seladd=([32, 128, 64], F32), c_c2s=([256, 64], BF16), c_pow2=([128, 24], F32),
    c_tri=([128, 128], BF16), c_ones=([128, 128], BF16), c_ebase=([128, 32, 8], F32),
)

PARAM_SPECS = dict(
    ev_w_out=[2, 1024, 1024], dif_lambda=[2, 4, 64], dif_subln=[2, 128],
    ffd_w_gate=[2, 1024, F_DENSE], ffd_w_up=[2, 1024, F_DENSE], ffd_w_down=[2, F_DENSE, 1024],
    od_w_out=[2, 1024, 1024], nsa_gate_b=[2, 24], nsa_phi_w1=[2, 2, 2048, 256], nsa_phi_w2=[2, 2, 256, 64],
    moe_w_router=[2, 1024, 8], moe_b_router=[2, 8],
    moe_w_gate=[2, 8, 1024, F_EXPERT], moe_w_up=[2, 8, 1024, F_EXPERT], moe_w_down=[2, 8, F_EXPERT, 1024],
    ln_mix_g=[4, 1024], ln_mix_b=[4, 1024], ln_ffn_g=[4, 1024], ln_ffn_b=[4, 1024],
)
LAYOUT_SPECS = {}
for _i in range(2):
    LAYOUT_SPECS[f"ev_fm{_i}"] = [1024, EV_FM]
    LAYOUT_SPECS[f"ev_fmp{_i}"] = [1024, EV_FM]
    LAYOUT_SPECS[f"ev_tm{_i}"] = [1024, EV_TM]
    LAYOUT_SPECS[f"od_fm{_i}"] = [1024, OD_FM]
    LAYOUT_SPECS[f"od_fmp{_i}"] = [1024, OD_FMR]
    LAYOUT_SPECS[f"od_tm{_i}"] = [1024, OD_TM]
    LAYOUT_SPECS[f"nsa_peT{_i}"] = [2, 64, 32]


def build_program(dbg=(), phases=None):
    nc = bass.Bass("TRN2", target_bir_lowering=False)
    st = ExitStack()
    kb = KB(nc, st, dbg)
    d = kb.d
    kb.din("x", [SEQ, D], F32)
    for k, (shape, dt) in CONST_SPECS.items():
        kb.din(k, shape, dt)
    for k, shape in PARAM_SPECS.items():
        kb.din(k, shape, F32)
    for k, shape in LAYOUT_SPECS.items():
        kb.din(k, shape, F32)
    kb.dscr("out", [SEQ, D], F32, out=True)
    kb.dscr("xres", [SEQ, D], F32)
    kb.dscr("xT", [D, SEQ], BF16)
    kb.dscr("qk", [2048, SEQ], BF16)
    kb.dscr("qraw", [512, SEQ], BF16)
    kb.dscr("vt", [SEQ, 768], BF16)
    kb.dscr("iw", [SEQ, 4], F32)
    kb.dscr("gates", [SEQ, 24], F32)
    kb.dscr("kmean", [512, 16], BF16)
    kb.dscr("mix", [SEQ, D], BF16)
    kb.dscr("moeg", [SEQ, 8], F32)
    kb.dscr("moem1", [SEQ, 8], F32)
    kb.dscr("moem2", [SEQ, 8], F32)
    kb.dscr("moegv", [SEQ, 2], F32)
    kb.dscr("xs", [8 * MOE_CAP, D], BF16)
    kb.dscr("ys", [8 * MOE_CAP, D], F32)
    if "dbg_lo" in kb.dbg:
        kb.dscr("dbg_lo", [SEQ, 4], F32)
    kb.dscr("kcmpT", [2, 64, 256], BF16)
    kb.dscr("vcmp", [2, 256, 64], BF16)
    for i in range(2):
        for k in ("ev_fm", "ev_fmp", "ev_tm", "od_fm", "od_fmp", "od_tm"):
            kb.dscr(f"b_{k}{i}", LAYOUT_SPECS[f"{k}{i}"], BF16)
        kb.dscr(f"b_ev_wout{i}", [1024, 1024], BF16)
        kb.dscr(f"b_od_wout{i}", [1024, 1024], BF16)
        kb.dscr(f"b_ffg{i}", [F_DENSE // 256, 128, 8 * 256], BF16)
        kb.dscr(f"b_ffu{i}", [F_DENSE // 256, 128, 8 * 256], BF16)
        kb.dscr(f"b_ffd{i}", [F_DENSE, 1024], BF16)
        kb.dscr(f"b_phi1_{i}", [2 * 2048, 256], BF16)
        kb.dscr(f"b_phi2_{i}", [2 * 256, 64], BF16)
        kb.dscr(f"b_mg{i}", [8 * (F_EXPERT // 256), 128, 8 * 256], BF16)
        kb.dscr(f"b_mu{i}", [8 * (F_EXPERT // 256), 128, 8 * 256], BF16)
        kb.dscr(f"b_md{i}", [8 * F_EXPERT, 1024], BF16)

    def want(name):
        return phases is None or name in phases

    S = kb.S
    for L in range(DEPTH):
        i = L // 2
        if L % 2 == 0:
            for k, cols in (("ev_fm", EV_FM), ("ev_fmp", EV_FM), ("ev_tm", EV_TM)):
                kb.conv_add(f"b_{k}{i}", d[f"{k}{i}"][:, :], 1024, cols)
            kb.conv_add(f"b_ev_wout{i}", d["ev_w_out"][i], 1024, 1024)
            kb.conv_add(f"b_ffg{i}", d["ffd_w_gate"][i], 1024, F_DENSE, tiled=True, dst_view=d[f"b_ffg{i}"])
            kb.conv_add(f"b_ffu{i}", d["ffd_w_up"][i], 1024, F_DENSE, tiled=True, dst_view=d[f"b_ffu{i}"])
            kb.conv_add(f"b_ffd{i}", d["ffd_w_down"][i], F_DENSE, 1024)
        else:
            for k, cols in (("od_fm", OD_FM), ("od_fmp", OD_FMR), ("od_tm", OD_TM)):
                kb.conv_add(f"b_{k}{i}", d[f"{k}{i}"][:, :], 1024, cols)
            kb.conv_add(f"b_phi1_{i}", d["nsa_phi_w1"][i].rearrange("a r c -> (a r) c"), 4096, 256)
            kb.conv_add(f"b_phi2_{i}", d["nsa_phi_w2"][i].rearrange("a r c -> (a r) c"), 512, 64)
            kb.conv_add(f"b_od_wout{i}", d["od_w_out"][i], 1024, 1024)
            NG_E = F_EXPERT // 256
            for e in range(8):
                for nm, src in (("b_mg", "moe_w_gate"), ("b_mu", "moe_w_up")):
                    sub = Buf(d[f"{nm}{i}"].t[e * NG_E:(e + 1) * NG_E], f"{nm}{i}_e{e}")
                    kb.conv_add(f"{nm}{i}" if e == 7 else f"{nm}{i}_part{e}", d[src][i][e], 1024, F_EXPERT,
                                tiled=True, dst_view=sub)
            kb.conv_add(f"b_md{i}", d["moe_w_down"][i].rearrange("e r c -> (e r) c"), 8 * F_EXPERT, 1024)
    if phases is not None:
        kb.conv_budget(1 << 60)
        S.flush()
    MB = 1 << 20
    NEED = {
        "proj_e": lambda i: [f"b_ev_fm{i}", f"b_ev_fmp{i}", f"b_ev_tm{i}"],
        "proj_o": lambda i: [f"b_od_fm{i}", f"b_od_fmp{i}", f"b_od_tm{i}"],
    }
    def run_phase(name, fn, budget_mb, needs=()):
        if not want(name):
            return
        kb.conv_ensure(needs)
        kb.conv_budget(budget_mb * MB)
        fn()

    if phases is None:
        kb.conv_ensure(NEED["proj_e"](0))
    run_phase("prologue", lambda: kb.phase_prologue(d["x"], d["xT"]), 120)
    for L in range(DEPTH):
        i = L // 2
        if L % 2 == 0:
            run_phase(f"proj{L}", lambda: kb.phase_proj(L), 0, NEED["proj_e"](i))
            run_phase(f"diff{L}", lambda: kb.phase_diff(L), 0)
            run_phase(f"dsa{L}", lambda: kb.phase_dsa(L), 450)
            run_phase(f"outproj{L}", lambda: kb.phase_outproj(L), 0, [f"b_ev_wout{i}"])
            run_phase(f"ffn{L}", lambda: kb.phase_ffn_dense(L), 0, [f"b_ffg{i}", f"b_ffu{i}", f"b_ffd{i}"])
        else:
            run_phase(f"proj{L}", lambda: kb.phase_proj(L), 0, NEED["proj_o"](i))
            run_phase(f"cmp{L}", lambda: kb.phase_cmp(L), 0, [f"b_phi1_{i}", f"b_phi2_{i}"])
            run_phase(f"moba{L}", lambda: kb.phase_moba(L), 0)
            run_phase(f"nsa{L}", lambda: kb.phase_nsa(L), 0)
            run_phase(f"outproj{L}", lambda: kb.phase_outproj(L), 0, [f"b_od_wout{i}"])
            moe_fn = (lambda: kb.phase_moe_routed(L)) if MOE_ROUTED else (lambda: kb.phase_moe(L))
            run_phase(f"moe{L}", moe_fn, 0, [f"b_mg{i}", f"b_mu{i}", f"b_md{i}"])
    st.close()
    return nc, kb


_PROGRAM = None


def kernel(**inputs):
    global _PROGRAM
    inp = {k: np.asarray(v) for k, v in inputs.items()}
    B = inp["x"].shape[0]
    assert inp["x"].shape == (8, SEQ, D)
    if _PROGRAM is None:
        _PROGRAM = build_program()[0]
    nc = _PROGRAM
    shared = {}
    shared.update(make_constants())
    for k, shape in PARAM_SPECS.items():
        shared[k] = np.ascontiguousarray(inp[k], dtype=np.float32).reshape(shape)
    shared.update(layout_weights(inp))
    in_maps = []
    for b in range(B):
        m = dict(shared)
        m["x"] = np.ascontiguousarray(inp["x"][b], dtype=np.float32)
        in_maps.append(m)
    res = run_bass_kernel_spmd(nc, in_maps, core_ids=list(range(B)))
    out = np.stack([np.asarray(r["out"], dtype=np.float32) for r in res.results], axis=0)
    return out
```
